# Optimizing a Trainium2 kernel written in Bass

```python
import math
import jax, jax.numpy as jnp
from jax import lax
import numpy as np

D_MODEL = 1024
BATCH = 8
SEQ = 4096
DEPTH = 4

N_MEM = 256
N_Q_HEADS = 8
N_KV_HEADS = 2
HEAD_DIM = 64
Q_GROUP = N_Q_HEADS // N_KV_HEADS
WINDOW = 128
BLOCK = 128
ROT_DIM = HEAD_DIM // 4
ROPE_THETA = 500000.0
SSM_WIDTH = D_MODEL // 2
SSM_GROUP = 16
SSM_GROUPS = SSM_WIDTH // SSM_GROUP
SSM_STATE = 64
POOL_WINDOWS = (2, 4, 8, 16)
POOL_WIDTH = D_MODEL // 2
POOL_GROUP = POOL_WIDTH // len(POOL_WINDOWS)
X_HEADS = 4
X_HEAD_DIM = D_MODEL // X_HEADS
D_FF = 2816
N_EXPERTS = 8
TOP_K = 2
D_FF_EXPERT = 3584
N_BRANCHES = 3
Q_WIDTH = N_Q_HEADS * HEAD_DIM
KV_WIDTH = N_KV_HEADS * HEAD_DIM
ATTN_WIDTH = Q_WIDTH
SPLITS = (Q_WIDTH, Q_WIDTH + KV_WIDTH, Q_WIDTH + 2 * KV_WIDTH, Q_WIDTH + 2 * KV_WIDTH + SSM_WIDTH, Q_WIDTH + 2 * KV_WIDTH + SSM_WIDTH + POOL_WIDTH)
IN_WIDTH = SPLITS[-1] + N_BRANCHES * D_MODEL
DN_ALPHA = (2.0 * DEPTH) ** 0.25
DN_BETA = (8.0 * DEPTH) ** -0.25
N_DENSE = (DEPTH + 1) // 2
N_MOE = DEPTH // 2
LN_EPS = 1e-5
NEG_INF = -1e30

kernel_name = 'hybrid_gated_swa_s5_pool_moe_block'


def layer_norm(x, g, b):
    xf = x.astype(jnp.float32)
    mu = jnp.mean(xf, axis=-1, keepdims=True)
    var = jnp.mean(jnp.square(xf - mu), axis=-1, keepdims=True)
    y = (xf - mu) * lax.rsqrt(var + LN_EPS)
    return (y * g.astype(jnp.float32) + b.astype(jnp.float32)).astype(x.dtype)


def partial_rotary(t, pos):
    half = ROT_DIM // 2
    inv_freq = jnp.power(jnp.float32(ROPE_THETA), -jnp.arange(half, dtype=jnp.float32) / half)
    ang = pos.astype(jnp.float32)[:, None] * inv_freq[None, :]
    cos = jnp.cos(ang)[None, :, None, :]
    sin = jnp.sin(ang)[None, :, None, :]
    tr = t[..., :ROT_DIM].astype(jnp.float32)
    t1, t2 = tr[..., :half], tr[..., half:]
    rot = jnp.concatenate([t1 * cos - t2 * sin, t2 * cos + t1 * sin], axis=-1)
    return jnp.concatenate([rot.astype(t.dtype), t[..., ROT_DIM:]], axis=-1)


def sliding_window_attention(q, k, v, sinks):
    bsz, l = q.shape[0], q.shape[1]
    nb = l // BLOCK
    qb = q.reshape(bsz, nb, BLOCK, N_KV_HEADS, Q_GROUP, HEAD_DIM)
    kb = k.reshape(bsz, nb, BLOCK, N_KV_HEADS, HEAD_DIM)
    vb = v.reshape(bsz, nb, BLOCK, N_KV_HEADS, HEAD_DIM)

    def with_prev_block(t):
        prev = jnp.pad(t, ((0, 0), (1, 0), (0, 0), (0, 0), (0, 0)))[:, :nb]
        return jnp.concatenate([prev, t], axis=2)

    kw, vw = with_prev_block(kb), with_prev_block(vb)
    s = jnp.einsum('bnqkgd,bnskd->bnkgqs', qb, kw).astype(jnp.float32) * (HEAD_DIM ** -0.5)
    qi = jnp.arange(BLOCK)[:, None]
    si = jnp.arange(2 * BLOCK)[None, :]
    rel = qi + BLOCK - si
    band = (rel >= 0) & (rel < WINDOW)
    has_prev = (jnp.arange(nb)[:, None] > 0) | (si >= BLOCK)
    mask = band[None, :, :] & has_prev[:, None, :]
    s = jnp.where(mask[None, :, None, None], s, NEG_INF)
    sink = sinks.astype(jnp.float32).reshape(N_KV_HEADS, Q_GROUP)[None, None, :, :, None, None]
    m = jnp.maximum(jnp.max(s, axis=-1, keepdims=True), sink)
    p = jnp.exp(s - m)
    p = p / (jnp.sum(p, axis=-1, keepdims=True) + jnp.exp(sink - m))
    o = jnp.einsum('bnkgqs,bnskd->bnqkgd', p.astype(v.dtype), vw)
    return o.reshape(bsz, l, N_Q_HEADS * HEAD_DIM)


def _complex_linear_combine(e1, e2):
    a1r, a1i, b1r, b1i = e1
    a2r, a2i, b2r, b2i = e2
    return (a2r * a1r - a2i * a1i, a2r * a1i + a2i * a1r,
            a2r * b1r - a2i * b1i + b2r, a2r * b1i + a2i * b1r + b2i)


def s5_ssm(u, a_re, a_im, log_dt, b_re, b_im, c_re, c_im, d_skip):
    bsz, l = u.shape[0], u.shape[1]
    f32 = jnp.float32
    uf = u.astype(f32).reshape(bsz, l, SSM_GROUPS, SSM_GROUP)
    ar, ai = a_re.astype(f32), a_im.astype(f32)
    dt = jnp.exp(log_dt.astype(f32))[:, None]
    decay = jnp.exp(dt * ar)
    abar_re, abar_im = decay * jnp.cos(dt * ai), decay * jnp.sin(dt * ai)
    inv_abs2 = 1.0 / (ar * ar + ai * ai)
    num_re, num_im = abar_re - 1.0, abar_im
    f_re = (num_re * ar + num_im * ai) * inv_abs2
    f_im = (num_im * ar - num_re * ai) * inv_abs2
    br, bi = b_re.astype(f32), b_im.astype(f32)
    bbar_re = f_re[..., None] * br - f_im[..., None] * bi
    bbar_im = f_re[..., None] * bi + f_im[..., None] * br
    bu_re = jnp.einsum('blgp,gnp->blgn', uf, bbar_re)
    bu_im = jnp.einsum('blgp,gnp->blgn', uf, bbar_im)
    a_seq_re = jnp.broadcast_to(abar_re[None, None], (1, l, SSM_GROUPS, SSM_STATE))
    a_seq_im = jnp.broadcast_to(abar_im[None, None], (1, l, SSM_GROUPS, SSM_STATE))
    _, _, s_re, s_im = lax.associative_scan(_complex_linear_combine, (a_seq_re, a_seq_im, bu_re, bu_im), axis=1)
    y = jnp.einsum('blgn,gpn->blgp', s_re, c_re.astype(f32)) - jnp.einsum('blgn,gpn->blgp', s_im, c_im.astype(f32))
    y = y + d_skip.astype(f32).reshape(SSM_GROUPS, SSM_GROUP) * uf
    return y.reshape(bsz, l, SSM_WIDTH)


def glu_after_gelu(y, w_glu, dtype):
    h = jax.nn.gelu(y).astype(dtype)
    hv, hg = jnp.split(h @ w_glu, 2, axis=-1)
    return hv * jax.nn.sigmoid(hg)


def multiscale_pool(u, pool_w, pool_scale):
    bsz, l = u.shape[0], u.shape[1]
    uf = u.astype(jnp.float32).reshape(bsz, l, len(POOL_WINDOWS), POOL_GROUP)
    cs = jnp.cumsum(uf, axis=1)
    pos1 = jnp.arange(1, l + 1, dtype=jnp.int32)
    outs = []
    for gi, w in enumerate(POOL_WINDOWS):
        csg = cs[:, :, gi]
        prev = jnp.pad(csg[:, :l - w], ((0, 0), (w, 0), (0, 0)))
        count = jnp.minimum(pos1, w).astype(jnp.float32)[None, :, None]
        outs.append((csg - prev) / count - uf[:, :, gi])
    pooled = jnp.stack(outs, axis=2)
    mixed = jnp.einsum('blgc,gcd->blgd', pooled.astype(u.dtype), pool_w)
    return mixed.reshape(bsz, l, POOL_WIDTH) * pool_scale


def hybrid_mixer(x, pos, w_in, b_gate, sinks, a_re, a_im, log_dt, b_re, b_im, c_re, c_im, d_skip, w_glu,
                 pool_w, pool_scale, w_br_attn, w_br_ssm, w_br_pool, w_o):
    bsz, l = x.shape[0], x.shape[1]
    proj = x @ w_in
    q, k, v, u_ssm, u_pool, gate_logits = jnp.split(proj, list(SPLITS), axis=-1)
    q = partial_rotary(q.reshape(bsz, l, N_Q_HEADS, HEAD_DIM), pos)
    k = partial_rotary(k.reshape(bsz, l, N_KV_HEADS, HEAD_DIM), pos)
    v = v.reshape(bsz, l, N_KV_HEADS, HEAD_DIM)
    attn_out = sliding_window_attention(q, k, v, sinks) @ w_br_attn
    ssm_out = glu_after_gelu(s5_ssm(u_ssm, a_re, a_im, log_dt, b_re, b_im, c_re, c_im, d_skip), w_glu, x.dtype) @ w_br_ssm
    pool_out = multiscale_pool(u_pool, pool_w, pool_scale) @ w_br_pool
    gates = jax.nn.sigmoid(gate_logits + b_gate).reshape(bsz, l, N_BRANCHES, D_MODEL)
    merged = gates[:, :, 0] * attn_out + gates[:, :, 1] * ssm_out + gates[:, :, 2] * pool_out
    return merged @ w_o


def memory_cross_attention(x, mem, wq, wkv, wo):
    bsz, l = x.shape[0], x.shape[1]
    m = mem.shape[1]
    q = (x @ wq).reshape(bsz, l, X_HEADS, X_HEAD_DIM)
    kk, vv = jnp.split(mem @ wkv, 2, axis=-1)
    kk = kk.reshape(bsz, m, X_HEADS, X_HEAD_DIM)
    vv = vv.reshape(bsz, m, X_HEADS, X_HEAD_DIM)
    s = jnp.einsum('blhd,bmhd->bhlm', q, kk).astype(jnp.float32) * (X_HEAD_DIM ** -0.5)
    p = jax.nn.softmax(s, axis=-1).astype(x.dtype)
    o = jnp.einsum('bhlm,bmhd->blhd', p, vv).reshape(bsz, l, D_MODEL)
    return o @ wo


def swiglu(x, w_gu, w_down):
    gt, up = jnp.split(x @ w_gu, 2, axis=-1)
    return (jax.nn.silu(gt) * up) @ w_down


def moe_swiglu(x, w_router, b_router, w_gu, w_down):
    logits = (x @ w_router).astype(jnp.float32) + b_router.astype(jnp.float32)
    top_vals, top_idx = lax.top_k(logits, TOP_K)
    top_w = jax.nn.softmax(top_vals, axis=-1)
    combine = jnp.sum(jax.nn.one_hot(top_idx, N_EXPERTS, dtype=jnp.float32) * top_w[..., None], axis=-2)
    out = jnp.zeros_like(x)
    for e in range(N_EXPERTS):
        out = out + combine[..., e:e + 1].astype(x.dtype) * swiglu(x, w_gu[e], w_down[e])
    return out


def setup_inputs(seed: int = 0) -> dict:
    key = jax.random.key(seed)
    ks = jax.random.split(key, 40)

    def nrm(i, shape, scale):
        return jax.random.normal(ks[i], shape, jnp.float32) * scale

    L = DEPTH
    G, N, P = SSM_GROUPS, SSM_STATE, SSM_GROUP
    a_im_base = jnp.pi * jnp.arange(N, dtype=jnp.float32)
    return {
        'x': nrm(0, (BATCH, SEQ, D_MODEL), 1.0),
        'mem': nrm(1, (BATCH, N_MEM, D_MODEL), 1.0),
        'w_in': nrm(2, (L, D_MODEL, IN_WIDTH), D_MODEL ** -0.5),
        'b_gate': nrm(3, (L, N_BRANCHES * D_MODEL), 0.02),
        'attn_sinks': nrm(4, (L, N_Q_HEADS), 0.5),
        'ssm_a_re': -0.5 + nrm(5, (L, G, N), 0.02),
        'ssm_a_im': a_im_base + nrm(6, (L, G, N), 0.02),
        'ssm_log_dt': jax.random.uniform(ks[7], (L, G), jnp.float32, math.log(1e-3), math.log(1e-1)),
        'ssm_b_re': nrm(8, (L, G, N, P), (2.0 * P) ** -0.5),
        'ssm_b_im': nrm(9, (L, G, N, P), (2.0 * P) ** -0.5),
        'ssm_c_re': nrm(10, (L, G, P, N), N ** -0.5),
        'ssm_c_im': nrm(11, (L, G, P, N), N ** -0.5),
        'ssm_d': nrm(12, (L, SSM_WIDTH), 0.5),
        'ssm_w_glu': nrm(13, (L, SSM_WIDTH, 2 * SSM_WIDTH), SSM_WIDTH ** -0.5),
        'pool_w': nrm(14, (L, len(POOL_WINDOWS), POOL_GROUP, POOL_GROUP), POOL_GROUP ** -0.5),
        'pool_scale': 1.0 + nrm(15, (L, POOL_WIDTH), 0.02),
        'w_br_attn': nrm(16, (L, ATTN_WIDTH, D_MODEL), ATTN_WIDTH ** -0.5),
        'w_br_ssm': nrm(17, (L, SSM_WIDTH, D_MODEL), SSM_WIDTH ** -0.5),
        'w_br_pool': nrm(18, (L, POOL_WIDTH, D_MODEL), POOL_WIDTH ** -0.5),
        'w_o': nrm(19, (L, D_MODEL, D_MODEL), D_MODEL ** -0.5 * DN_BETA),
        'ln1_g': 1.0 + nrm(20, (L, D_MODEL), 0.02),
        'ln1_b': nrm(21, (L, D_MODEL), 0.02),
        'xa_wq': nrm(22, (L, D_MODEL, D_MODEL), D_MODEL ** -0.5),
        'xa_wkv': nrm(23, (L, D_MODEL, 2 * D_MODEL), D_MODEL ** -0.5),
        'xa_wo': nrm(24, (L, D_MODEL, D_MODEL), D_MODEL ** -0.5 * DN_BETA),
        'ln2_g': 1.0 + nrm(25, (L, D_MODEL), 0.02),
        'ln2_b': nrm(26, (L, D_MODEL), 0.02),
        'ffn_w_gu': nrm(27, (N_DENSE, D_MODEL, 2 * D_FF), D_MODEL ** -0.5),
        'ffn_w_down': nrm(28, (N_DENSE, D_FF, D_MODEL), D_FF ** -0.5 * DN_BETA),
        'moe_w_router': nrm(29, (N_MOE, D_MODEL, N_EXPERTS), D_MODEL ** -0.5),
        'moe_b_router': nrm(30, (N_MOE, N_EXPERTS), 0.01),
        'moe_w_gu': nrm(31, (N_MOE, N_EXPERTS, D_MODEL, 2 * D_FF_EXPERT), D_MODEL ** -0.5),
        'moe_w_down': nrm(32, (N_MOE, N_EXPERTS, D_FF_EXPERT, D_MODEL), D_FF_EXPERT ** -0.5 * DN_BETA),
        'ln3_g': 1.0 + nrm(33, (L, D_MODEL), 0.02),
        'ln3_b': nrm(34, (L, D_MODEL), 0.02),
    }


def reference(x, mem, w_in, b_gate, attn_sinks, ssm_a_re, ssm_a_im, ssm_log_dt, ssm_b_re, ssm_b_im,
              ssm_c_re, ssm_c_im, ssm_d, ssm_w_glu, pool_w, pool_scale, w_br_attn, w_br_ssm, w_br_pool, w_o,
              ln1_g, ln1_b, xa_wq, xa_wkv, xa_wo, ln2_g, ln2_b, ffn_w_gu, ffn_w_down,
              moe_w_router, moe_b_router, moe_w_gu, moe_w_down, ln3_g, ln3_b):
    pos = jnp.arange(x.shape[1], dtype=jnp.int32)
    for i in range(DEPTH):
        h = hybrid_mixer(x, pos, w_in[i], b_gate[i], attn_sinks[i], ssm_a_re[i], ssm_a_im[i], ssm_log_dt[i],
                         ssm_b_re[i], ssm_b_im[i], ssm_c_re[i], ssm_c_im[i], ssm_d[i], ssm_w_glu[i],
                         pool_w[i], pool_scale[i], w_br_attn[i], w_br_ssm[i], w_br_pool[i], w_o[i])
        x = layer_norm(DN_ALPHA * x + h, ln1_g[i], ln1_b[i])
        c = memory_cross_attention(x, mem, xa_wq[i], xa_wkv[i], xa_wo[i])
        x = layer_norm(DN_ALPHA * x + c, ln2_g[i], ln2_b[i])
        j = i // 2
        if i % 2 == 0:
            f = swiglu(x, ffn_w_gu[j], ffn_w_down[j])
        else:
            f = moe_swiglu(x, moe_w_router[j], moe_b_router[j], moe_w_gu[j], moe_w_down[j])
        x = layer_norm(DN_ALPHA * x + f, ln3_g[i], ln3_b[i])
    return x
```

```python
import math
from contextlib import ExitStack

import ml_dtypes
import numpy as np

import concourse.bass as bass
import concourse.mybir as mybir
from concourse.bass_utils import run_bass_kernel_spmd

F32 = mybir.dt.float32
BF16 = mybir.dt.bfloat16
I32 = mybir.dt.int32
AF = mybir.ActivationFunctionType
ALU = mybir.AluOpType
AX = mybir.AxisListType

D = 1024
G = 1024
NT = G // 128
NMEM = 256
DFF = 2816
DFFE = 3584
NEXP = 8
DEPTH = 4
ALPHA = (2.0 * DEPTH) ** 0.25
LN_EPS = 1e-5
IN_W = 4864
C_Q, C_K, C_V, C_SSM, C_POOL, C_GATE = 0, 512, 640, 768, 1280, 1792
SLOT_B = 8192
NSLOT = 11
NRING = 6
TWO_PI = 2.0 * math.pi


class Buf:
    def __init__(self, name, t=None):
        self.name = name
        self.t = t
        self.w = {}
        self.r = {}
        self.sem = None
        self.dcnt = 0


class Sched:
    ENG = ("pe", "act", "dve", "pool", "sp")

    def __init__(self, nc, stack, same_eng_sync=("act", "dve", "pool")):
        self.nc = nc
        self.stack = stack
        self.same = set(same_eng_sync)
        self.ops = {e: [] for e in self.ENG}
        self.sem = {e: stack.enter_context(nc.semaphore("s_" + e)) for e in self.ENG}
        self.cnt = {e: 0 for e in self.ENG}
        self.waited = {e: {} for e in self.ENG}
        self.nsem = 5
        self.ninst = 0

    @staticmethod
    def _add(deps, k, h, v):
        if k not in deps or deps[k][1] < v:
            deps[k] = (h, v)

    def _deps(self, reads, writes, pwrites):
        deps = {}
        for b in reads:
            for k, (h, v, _p) in b.w.items():
                self._add(deps, k, h, v)
        for b in writes:
            for k, (h, v, _p) in b.w.items():
                self._add(deps, k, h, v)
            for k, (h, v) in b.r.items():
                self._add(deps, k, h, v)
        for b in pwrites:
            for k, (h, v, p) in b.w.items():
                if not p:
                    self._add(deps, k, h, v)
            for k, (h, v) in b.r.items():
                self._add(deps, k, h, v)
        return deps

    def _waits(self, eng, deps):
        for k, (h, v) in deps.items():
            if k == eng and eng not in self.same:
                continue
            if self.waited[eng].get(k, 0) >= v:
                continue
            self.waited[eng][k] = v
            self.ops[eng].append(lambda e, h=h, v=v: e.wait_ge(h, v))
            self.ninst += 1

    def _reg(self, t, reads, writes, pwrites):
        k, h, v = t
        for b in reads:
            if k not in b.r or b.r[k][1] < v:
                b.r[k] = (h, v)
        for b in writes:
            b.w = {k: (h, v, False)}
            b.r = {}
        for b in pwrites:
            if b.r:
                b.w = {k: (h, v, True)}
                b.r = {}
            else:
                if k not in b.w or b.w[k][1] < v:
                    b.w[k] = (h, v, True)

    def op(self, eng, fn, reads=(), writes=(), pwrites=(), inc=True):
        self._waits(eng, self._deps(reads, writes, pwrites))
        sem = self.sem[eng]
        if inc:
            self.cnt[eng] += 1
            n = self.cnt[eng]
            self.ops[eng].append(lambda e, fn=fn, sem=sem: fn(e).then_inc(sem, 1))
        else:
            n = self.cnt[eng] + 1
            self.ops[eng].append(lambda e, fn=fn: fn(e))
        self.ninst += 1
        self._reg((eng, sem, n), reads, writes, pwrites)

    def dma(self, q, out_ap, in_ap, reads=(), writes=(), pwrites=(), **kw):
        dst = (list(writes) + list(pwrites))[0]
        if dst.sem is None:
            dst.sem = self.stack.enter_context(self.nc.semaphore("d_" + dst.name))
            self.nsem += 1
        self._waits(q, self._deps(reads, writes, pwrites))
        dst.dcnt += 16
        sem = dst.sem
        self.ops[q].append(
            lambda e, o=out_ap, i=in_ap, sem=sem, kw=kw: e.dma_start(out=o, in_=i, **kw).then_inc(sem, 16)
        )
        self.ninst += 1
        self._reg(("d_" + dst.name, sem, dst.dcnt), reads, writes, pwrites)

    def mm(self, ps, out_ap, terms, reads):
        n = len(terms)
        for i, (l, r) in enumerate(terms):
            first, last = i == 0, i == n - 1
            self.op(
                "pe",
                lambda e, l=l, r=r, first=first, last=last, o=out_ap: e.matmul(o, l, r, start=first, stop=last),
                reads=reads if first else (),
                writes=[ps] if first else (),
                inc=last,
            )

    def finish(self, bufs):
        deps = {}
        for b in bufs:
            for k, (h, v, _p) in b.w.items():
                self._add(deps, k, h, v)
        self._waits("sp", deps)

    def emit(self):
        nc = self.nc
        ops = self.ops
        with nc.Block() as block:
            @block.tensor
            def _(e):
                for f in ops["pe"]:
                    f(e)

            @block.scalar
            def _(e):
                for f in ops["act"]:
                    f(e)

            @block.vector
            def _(e):
                for f in ops["dve"]:
                    f(e)

            @block.gpsimd
            def _(e):
                for f in ops["pool"]:
                    f(e)

            @block.sync
            def _(e):
                for f in ops["sp"]:
                    f(e)


class V:
    def __init__(self, ap, b):
        self.ap = ap
        self.b = b


def build(ng, depth, dbg=False, phases=None, same=("act", "dve", "pool")):
    nc = bass.Bass("TRN2", target_bir_lowering=False)
    SEQ = ng * G
    nden = (depth + 1) // 2
    nmoe = depth // 2

    def din(name, shape, dt=F32):
        return nc.dram_tensor(name, list(shape), dt, kind="ExternalInput").ap()

    x_d = din("x", [SEQ, D])
    mem_d = din("mem", [NMEM, D])
    w_in_d = din("w_in", [depth, D, IN_W])
    b_gate_d = din("b_gate", [depth, 3 * D])
    sinks_d = din("attn_sinks", [depth, 8])
    a_re_d = din("ssm_a_re", [depth, 32, 64])
    a_im_d = din("ssm_a_im", [depth, 32, 64])
    ldt_d = din("ssm_log_dt", [depth, 32])
    b_re_d = din("ssm_b_re", [depth, 32, 64, 16])
    b_im_d = din("ssm_b_im", [depth, 32, 64, 16])
    c_re_d = din("ssm_c_re", [depth, 32, 16, 64])
    c_im_d = din("ssm_c_im", [depth, 32, 16, 64])
    ssm_d_d = din("ssm_d", [depth, 512])
    w_glu_d = din("ssm_w_glu", [depth, 512, 1024])
    pool_w_d = din("pool_w", [depth, 4, 128, 128])
    pool_sc_d = din("pool_scale", [depth, 512])
    w_bra_d = din("w_br_attn", [depth, 512, D])
    w_brs_d = din("w_br_ssm", [depth, 512, D])
    w_brp_d = din("w_br_pool", [depth, 512, D])
    w_o_d = din("w_o", [depth, D, D])
    ln_g_d = [din(f"ln{i}_g", [depth, D]) for i in (1, 2, 3)]
    ln_b_d = [din(f"ln{i}_b", [depth, D]) for i in (1, 2, 3)]
    xa_wq_d = din("xa_wq", [depth, D, D])
    xa_wkv_d = din("xa_wkv", [depth, D, 2 * D])
    xa_wo_d = din("xa_wo", [depth, D, D])
    ffn_gu_d = din("ffn_w_gu", [nden, D, 2 * DFF])
    ffn_dn_d = din("ffn_w_down", [nden, DFF, D])
    if nmoe:
        moe_wr_d = din("moe_w_router", [nmoe, D, NEXP])
        moe_br_d = din("moe_b_router", [nmoe, NEXP])
        moe_gu_d = din("moe_w_gu", [nmoe, NEXP, D, 2 * DFFE])
        moe_dn_d = din("moe_w_down", [nmoe, NEXP, DFFE, D])
    c_bf_d = din("c_bf", [128, 1280], BF16)
    c_f_d = din("c_f", [128, 72])
    c_rope_d = din("c_rope", [32, 2, SEQ])
    out_d = nc.dram_tensor("out", [SEQ, D], F32, kind="ExternalOutput").ap()
    ssmt_d = nc.dram_tensor("ssmt", [depth, 16, 128, 2, G], F32).ap()
    ssmw_d = nc.dram_tensor("ssmw", [depth, 16, 128, 4, 128], BF16).ap()
    ssmd_d = nc.dram_tensor("ssmd", [depth, 128, 4, 128], BF16).ap()
    dbg_d = {}
    if dbg:
        for nm in ("d_x1", "d_x2", "d_x3"):
            dbg_d[nm] = nc.dram_tensor(nm, [G, D], F32, kind="ExternalOutput").ap()
        for nm in ("d_aot0", "d_aot1", "d_glut", "d_pmt", "d_mt0", "d_mt1"):
            dbg_d[nm] = nc.dram_tensor(nm, [128, SLOT_B // 2], BF16, kind="ExternalOutput").ap()

    with ExitStack() as st:
        S = Sched(nc, st, same_eng_sync=same)

        def sb(name, shape, dt):
            return Buf(name, st.enter_context(nc.sbuf_tensor(name, list(shape), dt)))

        X = [sb(f"X{i}", [128, D], F32) for i in range(NT)]
        XTt = st.enter_context(nc.sbuf_tensor("XT", [128, 8, G], BF16))
        XTb = [Buf("XT0"), Buf("XT1")]
        CB = sb("CB", [128, 1280], BF16)
        CF = sb("CF", [128, 72], F32)
        MEMT = sb("MEMT", [128, 8, NMEM], BF16)
        SSMC = sb("SSMC", [128, depth, 16, 2], F32)
        KTC = sb("KTC", [64, depth, 2, 128], BF16)
        VC = sb("VC", [128, depth, 128], BF16)
        UPC = sb("UPC", [128, depth, 4, 16], F32)
        SSMR = sb("SSMR", [128, depth, 16], F32)
        ESK = sb("ESK", [64, depth, 8], F32)
        BG = sb("BG", [128, depth, 24], F32)
        PSC = sb("PSC", [128, depth, 4], F32)
        LNG = sb("LNG", [128, 2, D], F32)
        SM = sb("SM", [128, 64], F32)
        SM2 = sb("SM2", [128, 32], F32)
        CW = sb("CW", [128, NT, NEXP], F32)
        RT = sb("RT", [128, 8, 64], F32)
        BR = sb("BRT", [128, NEXP], F32)
        EPS = sb("EPS", [128, 1], F32)
        RING = [sb(f"R{i}", [128, SLOT_B // 2], BF16) for i in range(NRING)]
        SLt = [st.enter_context(nc.sbuf_tensor(f"SL{i}", [128, SLOT_B // 2], BF16)) for i in range(NSLOT)]
        SLq = [[Buf(f"SL{i}q{j}") for j in range(4)] for i in range(NSLOT)]
        PSD = [st.enter_context(nc.psum_tensor(f"psd{i}", [128, 1024], F32)) for i in range(4)]
        PS = [Buf(f"ps{i}", PSD[i // 2][:, (i % 2) * 512:(i % 2 + 1) * 512]) for i in range(8)]
        B_ssmw = Buf("ssmw")
        B_ssmt = Buf("ssmt")
        B_ssmd = Buf("ssmd")
        B_out = Buf("outb")
        B_dbg = Buf("dbgb")
        state = {"ps": 0, "ring": 0}

        def ps_next():
            p = PS[state["ps"] % 8]
            state["ps"] += 1
            return p

        def ps_sub(lo, n, key):
            i = state.get(key, 0)
            state[key] = i + 1
            return PS[lo + i % n]

        def ring_next():
            r = RING[state["ring"] % NRING]
            state["ring"] += 1
            return r

        def sv(i, dt, off_b, shape):
            n = 1
            for s_ in shape:
                n *= s_
            esz = 4 if dt in (F32, I32) else 2
            nb = n * esz
            assert off_b + nb <= SLOT_B, (i, off_b, nb)
            a = SLt[i][:, off_b // 2: (off_b + nb) // 2]
            if dt != BF16:
                a = a.bitcast(dt)
            if len(shape) == 2:
                a = a.rearrange("p (a b) -> p a b", a=shape[0])
            elif len(shape) == 3:
                a = a.rearrange("p (a b c) -> p a b c", a=shape[0], b=shape[1])
            q0, q1 = off_b // 2048, (off_b + nb - 1) // 2048
            return V(a, [SLq[i][j] for j in range(q0, q1 + 1)])

        def rv(rbuf, shape):
            n = 1
            for s_ in shape:
                n *= s_
            a = rbuf.t[:, 0:n]
            if len(shape) == 2:
                a = a.rearrange("p (a b) -> p a b", a=shape[0])
            elif len(shape) == 3:
                a = a.rearrange("p (a b c) -> p a b c", a=shape[0], b=shape[1])
            return a

        ident = CB.t[:, 0:128]
        ones = CB.t[:, 128:256]
        mcur = CB.t[:, 256:768].rearrange("p (h q) -> p h q", h=4)
        mprev = CB.t[:, 768:1280].rearrange("p (h q) -> p h q", h=4)
        INVC = CF.t[:, 0:64].rearrange("p (a b) -> p a b", a=4)
        ROWM = CF.t[:, 64:68]
        EVENM = CF.t[:, 68:69]
        ODDM = CF.t[:, 69:70]

        def act(out, in_, func, reads, writes=(), pw=(), bias=None, scale=None):
            kw = {}
            if bias is not None:
                kw["bias"] = bias
            if scale is not None:
                kw["scale"] = scale
            S.op("act", lambda e: e.activation(out, in_, func, **kw), reads=reads, writes=writes, pwrites=pw)

        def tt(eng, out, in0, in1, op, reads, writes=(), pw=()):
            S.op(eng, lambda e: e.tensor_tensor(out, in0, in1, op), reads=reads, writes=writes, pwrites=pw)

        def ts(eng, out, in0, s1, s2, op0, op1, reads, writes=(), pw=()):
            if s2 is None:
                S.op(eng, lambda e: e.tensor_scalar(out, in0, s1, None, op0), reads=reads, writes=writes, pwrites=pw)
            else:
                S.op(eng, lambda e: e.tensor_scalar(out, in0, s1, s2, op0, op1), reads=reads, writes=writes, pwrites=pw)

        def stt(out, in0, sc, in1, op0, op1, reads, writes=(), pw=()):
            S.op("dve", lambda e: e.scalar_tensor_tensor(out, in0, sc, in1, op0, op1), reads=reads, writes=writes, pwrites=pw)

        def cp(eng, out, in_, reads, writes=(), pw=()):
            if eng == "act":
                S.op("act", lambda e: e.activation(out, in_, AF.Copy), reads=reads, writes=writes, pwrites=pw)
            else:
                S.op(eng, lambda e: e.tensor_copy(out, in_), reads=reads, writes=writes, pwrites=pw)

        def memset(eng, ap, val, writes=(), pw=()):
            S.op(eng, lambda e: e.memset(ap, val), writes=writes, pwrites=pw)

        def wload(dst_buf, dst_ap, src_ap, partial=False):
            if partial:
                S.dma("pool", dst_ap, src_ap, pwrites=[dst_buf])
            else:
                S.dma("pool", dst_ap, src_ap, writes=[dst_buf])

        def kview(w2d):
            return w2d.rearrange("(k p) c -> p k c", p=128)

        def xt(k, c0, c1):
            return XTt[:, k, c0:c1]

        S.dma("sp", CB.t[:, :], c_bf_d[:, :], writes=[CB])
        S.dma("sp", CF.t[:, :], c_f_d[:, :], writes=[CF])
        memset("dve", EPS.t[:, :], LN_EPS, writes=[EPS])
        memset("dve", SSMC.t[:, :, :, :], 0.0, writes=[SSMC])
        memset("dve", UPC.t[:, :, :, :], 0.0, writes=[UPC])
        memset("dve", KTC.t[:, :, :, :], 0.0, writes=[KTC])
        memset("dve", VC.t[:, :, :], 0.0, writes=[VC])
        NCD = dict(allow_slow_non_contiguous=True)
        for l in range(depth):
            S.dma("sp", BG.t[:, l, :], b_gate_d[l].rearrange("(c p) -> p c", p=128), pwrites=[BG], **NCD)
            S.dma("sp", PSC.t[:, l, :], pool_sc_d[l].rearrange("(c p) -> p c", p=128), pwrites=[PSC], **NCD)
            S.dma("sp", ESK.t[:, l, :], sinks_d[l].partition_broadcast(64), pwrites=[ESK], **NCD)
        act(ESK.t[:, :, :], ESK.t[:, :, :], AF.Exp, reads=[ESK], writes=[ESK])

        for mt in range(2):
            mf = sv(0, F32, 0, [D])
            S.dma("sp", mf.ap, mem_d[mt * 128:(mt + 1) * 128, :], writes=mf.b)
            mb = sv(1, BF16, 0, [D])
            cp("act", mb.ap, mf.ap, reads=mf.b, writes=mb.b)
            p = ps_next()
            pb = p.t[:, :].bitcast(BF16)
            for kc in range(8):
                S.op("pe", lambda e, kc=kc, pb=pb, mb=mb: e.transpose(pb[:, kc * 128:(kc + 1) * 128], mb.ap[:, kc * 128:(kc + 1) * 128], ident),
                     reads=mb.b + [CB] if kc == 0 else (), writes=[p] if kc == 0 else (), inc=(kc == 7))
            cp("dve", MEMT.t[:, :, mt * 128:(mt + 1) * 128], pb.rearrange("p (k t) -> p k t", k=8), reads=[p], pw=[MEMT])

        def ssm_prologue(l):
            PAv = sv(0, F32, 0, [2048])
            PA = PAv.b
            pa = PAv.ap

            def pv(i):
                return pa[:, i * 16:(i + 1) * 16]
            are, aim, dtv, th, rr, cs, sn, tA, fre, fim, nre, inv, tB = [pv(i) for i in range(13)]
            tI = pa[:, 13 * 16:14 * 16].bitcast(I32)
            dcol = pa[:, 14 * 16:14 * 16 + 4]
            S.dma("sp", are, a_re_d[l].rearrange("(q g) n -> (g n) q", g=2), writes=PA, **NCD)
            S.dma("sp", aim, a_im_d[l].rearrange("(q g) n -> (g n) q", g=2), pwrites=PA, **NCD)
            for g2 in range(2):
                S.dma("sp", dtv[g2 * 64:(g2 + 1) * 64, :],
                      ldt_d[l].rearrange("(q g) -> g q", g=2)[g2].partition_broadcast(64), pwrites=PA, **NCD)
            S.dma("sp", dcol, ssm_d_d[l].rearrange("(j p) -> p j", p=128), pwrites=PA, **NCD)

            def d(fn_out, *a, **k):
                pass
            act(dtv, dtv, AF.Exp, reads=PA, writes=PA)
            tt("dve", rr, dtv, are, ALU.mult, reads=PA, writes=PA)
            act(rr, rr, AF.Exp, reads=PA, writes=PA)
            cp("dve", SSMR.t[:, l, :], rr, reads=PA, pw=[SSMR])
            tt("dve", th, dtv, aim, ALU.mult, reads=PA, writes=PA)
            for dst, shift in ((sn, 0.0), (cs, 0.25)):
                ts("dve", tA, th, 1.0 / TWO_PI, shift, ALU.mult, ALU.add, reads=PA, writes=PA)
                cp("dve", tI, tA, reads=PA, writes=PA)
                cp("dve", dst, tI, reads=PA, writes=PA)
                tt("dve", tA, tA, dst, ALU.subtract, reads=PA, writes=PA)
                act(dst, tA, AF.Sin, reads=PA, writes=PA, scale=TWO_PI)
            tt("dve", nre, rr, cs, ALU.mult, reads=PA, writes=PA)
            ts("dve", nre, nre, -1.0, None, ALU.add, None, reads=PA, writes=PA)
            tt("dve", tA, rr, sn, ALU.mult, reads=PA, writes=PA)
            tt("dve", inv, are, are, ALU.mult, reads=PA, writes=PA)
            tt("dve", tB, aim, aim, ALU.mult, reads=PA, writes=PA)
            tt("dve", inv, inv, tB, ALU.add, reads=PA, writes=PA)
            S.op("dve", lambda e: e.reciprocal(inv, inv), reads=PA, writes=PA)
            tt("dve", fre, nre, are, ALU.mult, reads=PA, writes=PA)
            tt("dve", tB, tA, aim, ALU.mult, reads=PA, writes=PA)
            tt("dve", fre, fre, tB, ALU.add, reads=PA, writes=PA)
            tt("dve", fre, fre, inv, ALU.mult, reads=PA, writes=PA)
            tt("dve", fim, tA, are, ALU.mult, reads=PA, writes=PA)
            tt("dve", tB, nre, aim, ALU.mult, reads=PA, writes=PA)
            tt("dve", fim, fim, tB, ALU.subtract, reads=PA, writes=PA)
            tt("dve", fim, fim, inv, ALU.mult, reads=PA, writes=PA)
            STG = [sv(6, BF16, 0, [8, 4, 128]), sv(7, BF16, 0, [8, 4, 128])]
            for s_ in STG:
                memset("dve", s_.ap, 0.0, writes=s_.b)
            bre = sv(1, F32, 0, [16, 16]); bim = sv(1, F32, 1024, [16, 16])
            bbr = sv(2, F32, 0, [16, 16]); bbi = sv(2, F32, 1024, [16, 16]); btm = sv(2, F32, 2048, [16, 16])
            S.dma("sp", bre.ap, b_re_d[l].rearrange("(q g) n p -> (g n) q p", g=2), writes=bre.b)
            S.dma("sp", bim.ap, b_im_d[l].rearrange("(q g) n p -> (g n) q p", g=2), pwrites=bim.b)
            freb = fre.unsqueeze(2).to_broadcast([128, 16, 16])
            fimb = fim.unsqueeze(2).to_broadcast([128, 16, 16])
            R_ = PA + bre.b
            BBb = [SLq[2][0], SLq[2][1]]
            tt("dve", bbr.ap, bre.ap, freb, ALU.mult, reads=R_, writes=BBb)
            tt("dve", btm.ap, bim.ap, fimb, ALU.mult, reads=R_, writes=BBb)
            tt("dve", bbr.ap, bbr.ap, btm.ap, ALU.subtract, reads=BBb, writes=BBb)
            tt("dve", bbi.ap, bim.ap, freb, ALU.mult, reads=R_, writes=BBb)
            tt("dve", btm.ap, bre.ap, fimb, ALU.mult, reads=R_, writes=BBb)
            tt("dve", bbi.ap, bbi.ap, btm.ap, ALU.add, reads=BBb, writes=BBb)
            SRC = sv(3, BF16, 0, [128])
            for comp, bsrc in ((0, bbr), (1, bbi)):
                for j in range(4):
                    memset("dve", SRC.ap, 0.0, writes=SRC.b)
                    srcv = SRC.ap.rearrange("p (qq g p2) -> p qq g p2", qq=4, g=2)
                    for g2 in range(2):
                        cp("dve", srcv[g2 * 64:(g2 + 1) * 64, :, g2, :], bsrc.ap[g2 * 64:(g2 + 1) * 64, 4 * j:4 * j + 4, :],
                           reads=BBb, pw=SRC.b)
                    p = ps_next()
                    pb = p.t[:, :].bitcast(BF16)
                    S.op("pe", lambda e, pb=pb: e.transpose(pb[:, 0:128], SRC.ap, ident), reads=SRC.b + [CB], writes=[p])
                    for qq in range(4):
                        q = 4 * j + qq
                        s_ = STG[q // 8]
                        ts("dve", s_.ap[:, q % 8, comp, :], pb[:, 0:128], ROWM[:, qq:qq + 1], None, ALU.mult, None,
                           reads=[p, CF], pw=s_.b)
            cn = sv(1, F32, 0, [4, 64])
            for comp, csrc_d, sgn in ((2, c_re_d, 1.0), (3, c_im_d, -1.0)):
                S.dma("sp", cn.ap, csrc_d[l].rearrange("(j g) p n -> (g p) j n", g=8), writes=cn.b)
                for j in range(4):
                    ts("dve", SRC.ap[:, 0:64], cn.ap[:, j, :], EVENM, sgn, ALU.mult, ALU.mult, reads=cn.b + [CF], writes=SRC.b)
                    ts("dve", SRC.ap[:, 64:128], cn.ap[:, j, :], ODDM, sgn, ALU.mult, ALU.mult, reads=cn.b + [CF], pw=SRC.b)
                    p = ps_next()
                    pb = p.t[:, :].bitcast(BF16)
                    S.op("pe", lambda e, pb=pb: e.transpose(pb[:, 0:128], SRC.ap, ident), reads=SRC.b + [CB], writes=[p])
                    for qq in range(4):
                        q = 4 * j + qq
                        s_ = STG[q // 8]
                        cp("dve", s_.ap[:, q % 8, comp, qq * 32:(qq + 1) * 32], pb[:, qq * 32:(qq + 1) * 32], reads=[p], pw=s_.b)
            for hf in range(2):
                S.dma("sp", ssmw_d[l, hf * 8:(hf + 1) * 8].rearrange("q p c m -> p q c m"), STG[hf].ap, reads=STG[hf].b, pwrites=[B_ssmw])
            dk = sv(3, BF16, 2048, [4, 128])
            for j in range(4):
                ts("dve", dk.ap[:, j, :], ident, dcol[:, j:j + 1], None, ALU.mult, None, reads=[CB] + PA, pw=dk.b)
            S.dma("sp", ssmd_d[l], dk.ap, reads=dk.b, pwrites=[B_ssmd])
            for qb in range(8):
                par = qb % 2
                tr = sv(8, F32, 0, [2, G]) if par == 0 else sv(4, F32, 0, [2, G])
                ti = sv(9, F32, 0, [2, G]) if par == 0 else sv(5, F32, 0, [2, G])
                tm1 = sv(10, F32, 0, [2, 512])
                tm2 = sv(10, F32, 4096, [2, 512])
                q0 = 2 * qb
                cp("dve", tr.ap[:, :, 0:1], cs[:, q0:q0 + 2].unsqueeze(2), reads=PA, writes=tr.b)
                cp("dve", ti.ap[:, :, 0:1], sn[:, q0:q0 + 2].unsqueeze(2), reads=PA, writes=ti.b)
                m = 1
                while m < G:
                    cm = tr.ap[:, :, m - 1:m].to_broadcast([128, 2, m])
                    sm = ti.ap[:, :, m - 1:m].to_broadcast([128, 2, m])
                    a_ = tm1.ap[:, :, 0:m]
                    b_ = tm2.ap[:, :, 0:m]
                    RB = tr.b + ti.b
                    tt("dve", a_, tr.ap[:, :, 0:m], cm, ALU.mult, reads=RB, writes=tm1.b)
                    tt("dve", b_, ti.ap[:, :, 0:m], sm, ALU.mult, reads=RB, writes=tm2.b)
                    tt("dve", tr.ap[:, :, m:2 * m], a_, b_, ALU.subtract, reads=tm1.b + tm2.b, writes=tr.b)
                    tt("dve", a_, tr.ap[:, :, 0:m], sm, ALU.mult, reads=RB, writes=tm1.b)
                    tt("dve", b_, ti.ap[:, :, 0:m], cm, ALU.mult, reads=RB, writes=tm2.b)
                    tt("dve", ti.ap[:, :, m:2 * m], a_, b_, ALU.add, reads=tm1.b + tm2.b, writes=ti.b)
                    m *= 2
                for qq in range(2):
                    S.dma("sp", ssmt_d[l, q0 + qq, :, 0, :], tr.ap[:, qq, :], reads=tr.b, pwrites=[B_ssmt])
                    S.dma("sp", ssmt_d[l, q0 + qq, :, 1, :], ti.ap[:, qq, :], reads=ti.b, pwrites=[B_ssmt])

        if phases is None or "pro" in phases:
            for l in range(depth):
                ssm_prologue(l)

        def dbg_dump(name, ap, bufs):
            if dbg and name in dbg_d:
                S.dma("sp", dbg_d[name], ap, reads=bufs, pwrites=[B_dbg])

        def make_xt(t):
            xb = sv(9, BF16, (t % 4) * 2048, [D])
            cp("act", xb.ap, X[t].t[:, :], reads=[X[t]], writes=xb.b)
            p = ps_next()
            pb = p.t[:, :].bitcast(BF16)
            for kc in range(8):
                S.op("pe", lambda e, kc=kc, pb=pb, xb=xb: e.transpose(pb[:, kc * 128:(kc + 1) * 128], xb.ap[:, kc * 128:(kc + 1) * 128], ident),
                     reads=xb.b + [CB] if kc == 0 else (), writes=[p] if kc == 0 else (), inc=(kc == 7))
            cp("act", XTt[:, :, t * 128:(t + 1) * 128], pb.rearrange("p (k t) -> p k t", k=8), reads=[p], pw=[XTb[t // 4]])

        def layernorm(li, l, g, last):
            S.dma("sp", LNG.t[:, 0, :], ln_g_d[li][l].partition_broadcast(128), writes=[LNG])
            S.dma("sp", LNG.t[:, 1, :], ln_b_d[li][l].partition_broadcast(128), pwrites=[LNG])
            smv = SM.t[:, 0:48].rearrange("p (a b c) -> p a b c", a=4, b=2)
            mv = SM2.t[:, 0:8].rearrange("p (a b) -> p a b", a=4)
            sd = SM2.t[:, 8:12]
            rs = SM2.t[:, 12:16]
            for hf in range(2):
                for t4 in range(4):
                    t = hf * 4 + t4
                    for c in range(2):
                        S.op("dve", lambda e, t=t, t4=t4, c=c: e.bn_stats(smv[:, t4, c, :], X[t].t[:, c * 512:(c + 1) * 512]),
                             reads=[X[t]], writes=[SM] if (t4 == 0 and c == 0) else (), pwrites=() if (t4 == 0 and c == 0) else [SM])
                    S.op("dve", lambda e, t4=t4: e.bn_aggr(mv[:, t4, :], smv[:, t4, :, :]), reads=[SM], writes=[SM2] if t4 == 0 else (), pwrites=() if t4 == 0 else [SM2])
                act(sd, mv[:, :, 1], AF.Sqrt, reads=[SM2, EPS], pw=[SM2], bias=EPS.t[:, 0:1])
                S.op("dve", lambda e: e.reciprocal(rs, sd), reads=[SM2], pwrites=[SM2])
                for t4 in range(4):
                    t = hf * 4 + t4
                    xa = X[t].t[:, :]
                    ts("dve", xa, xa, mv[:, t4, 0:1], rs[:, t4:t4 + 1], ALU.subtract, ALU.mult, reads=[X[t], SM2], writes=[X[t]])
                    tt("dve", xa, xa, LNG.t[:, 0, :], ALU.mult, reads=[X[t], LNG], writes=[X[t]])
                    tt("dve", xa, xa, LNG.t[:, 1, :], ALU.add, reads=[X[t], LNG], writes=[X[t]])
                    if last:
                        S.dma("sp", out_d[g * G + t * 128: g * G + (t + 1) * 128, :], xa, reads=[X[t]], pwrites=[B_out])
                    else:
                        make_xt(t)
                    if dbg and g == 0 and l == 0:
                        S.dma("sp", dbg_d[f"d_x{li + 1}"][t * 128:(t + 1) * 128, :], xa, reads=[X[t]], pwrites=[B_dbg])

        def out_proj(src_of_k, src_bufs, w_d2, l, li, g, last):
            WU = []
            for hf in range(2):
                r = ring_next()
                wload(r, rv(r, [8, 512]), kview(w_d2)[:, :, hf * 512:(hf + 1) * 512])
                WU.append(r)
            for t in range(NT):
                for hf in range(2):
                    p = ps_next()
                    w = rv(WU[hf], [8, 512])
                    S.mm(p, p.t[:, :], [(src_of_k(k)[:, t * 128:(t + 1) * 128], w[:, k, :]) for k in range(8)],
                         reads=src_bufs + [WU[hf]])
                    xa = X[t].t[:, hf * 512:(hf + 1) * 512]
                    stt(xa, xa, ALPHA, p.t[:, :], ALU.mult, ALU.add, reads=[X[t], p], pw=[X[t]])
            layernorm(li, l, g, last)

        def phase_attn(g, l):
            Wq = ring_next()
            wq = rv(Wq, [8, 512])
            wload(Wq, wq, kview(w_in_d[l])[:, :, C_Q:C_Q + 512])
            Wkv = ring_next()
            wkv = rv(Wkv, [8, 256])
            wload(Wkv, wkv, kview(w_in_d[l])[:, :, C_K:C_K + 256])
            rope = sv(5, F32, 0, [2, G])
            S.dma("sp", rope.ap[0:32], c_rope_d[:, :, g * G:(g + 1) * G], writes=rope.b)
            wrot = sv(6, BF16, 0, [8, 10, 32])
            memset("dve", wrot.ap, 0.0, writes=wrot.b)
            wq4 = wq.rearrange("p k (h d) -> p k h d", h=8)
            wk4 = wkv[:, :, 0:128].rearrange("p k (h d) -> p k h d", h=2)
            cp("dve", wrot.ap[:, :, 0:8, 0:8], wq4[:, :, :, 8:16], reads=[Wq], pw=wrot.b)
            cp("dve", wrot.ap[:, :, 0:8, 8:16], wq4[:, :, :, 0:8], reads=[Wq], pw=wrot.b)
            cp("dve", wrot.ap[:, :, 8:10, 0:8], wk4[:, :, :, 8:16], reads=[Wkv], pw=wrot.b)
            cp("dve", wrot.ap[:, :, 8:10, 8:16], wk4[:, :, :, 0:8], reads=[Wkv], pw=wrot.b)
            QT = [sv(0, BF16, 0, [4, G]), sv(1, BF16, 0, [4, G])]
            AOT = [sv(2, BF16, 0, [4, G]), sv(3, BF16, 0, [4, G])]
            KT = sv(4, BF16, 0, [2, 1152])
            Vv = sv(4, BF16, 4608, [9, 128])
            cp("dve", KT.ap[0:64, :, 0:128], KTC.t[:, l, :, :], reads=[KTC], writes=KT.b)
            cp("dve", Vv.ap[:, 0, :], VC.t[:, l, :], reads=[VC], pw=Vv.b)
            for h in range(10):
                for s_ in range(2):
                    pq = ps_next()
                    pr = ps_next()
                    if h < 8:
                        lq = [wq[:, k, h * 64:(h + 1) * 64] for k in range(8)]
                        wb = Wq
                    else:
                        lq = [wkv[:, k, (h - 8) * 64:(h - 7) * 64] for k in range(8)]
                        wb = Wkv
                    S.mm(pq, pq.t[0:64, :], [(lq[k], xt(k, s_ * 512, (s_ + 1) * 512)) for k in range(8)], reads=[wb, XTb[s_]])
                    S.mm(pr, pr.t[0:32, :], [(wrot.ap[:, k, h, :], xt(k, s_ * 512, (s_ + 1) * 512)) for k in range(8)],
                         reads=wrot.b + [XTb[s_]])
                    par = (h * 2 + s_) % 2
                    T1 = sv(7, F32, par * 4096, [512])
                    T2 = sv(7, F32, par * 4096 + 2048, [512])
                    cs_ = rope.ap[0:32, 0, s_ * 512:(s_ + 1) * 512]
                    sn_ = rope.ap[0:32, 1, s_ * 512:(s_ + 1) * 512]
                    tt("dve", T1.ap[0:32], pq.t[0:32, :], cs_, ALU.mult, reads=[pq] + rope.b, writes=T1.b)
                    tt("dve", T2.ap[0:32], pr.t[0:32, :], sn_, ALU.mult, reads=[pr] + rope.b, writes=T2.b)
                    if h < 8:
                        dst = QT[h // 4].ap[:, h % 4, s_ * 512:(s_ + 1) * 512]
                        db = [QT[h // 4].b[h % 4]]
                    else:
                        dst = KT.ap[:, h - 8, 128 + s_ * 512:128 + (s_ + 1) * 512]
                        db = KT.b
                    tt("dve", dst[0:32], T1.ap[0:32], T2.ap[0:32], ALU.add, reads=T1.b + T2.b, pw=db)
                    cp("act", dst[32:64], pq.t[32:64, :], reads=[pq], pw=db)
            for t in range(NT):
                pv_ = ps_next()
                S.mm(pv_, pv_.t[:, 0:128], [(xt(k, t * 128, (t + 1) * 128), wkv[:, k, 128:256]) for k in range(8)],
                     reads=[XTb[t // 4], Wkv])
                cp("act", Vv.ap[:, 1 + t, :], pv_.t[:, 0:128], reads=[pv_], pw=Vv.b)
            cp("dve", KTC.t[:, l, :, :], KT.ap[0:64, :, G:G + 128], reads=KT.b, writes=[KTC])
            cp("dve", VC.t[:, l, :], Vv.ap[:, NT, :], reads=Vv.b, writes=[VC])
            it = 0
            for i in range(NT):
                for kv in range(2):
                    has_prev = not (g == 0 and i == 0)
                    qr = QT[kv].ap[0:64, :, i * 128:(i + 1) * 128]
                    PTc = sv(8, BF16, (it % 2) * 2048, [512])
                    PTp = sv(8, BF16, (it % 2) * 2048 + 1024, [512])
                    d1 = sv(8, F32, 4096 + (it % 2) * 2048, [512])
                    it += 1
                    pc = ps_next()
                    o3 = pc.t[:, :].rearrange("p (h q) -> p h q", h=4)
                    S.mm(pc, o3, [(KT.ap[0:64, kv, 128 + i * 128:128 + (i + 1) * 128], qr), (ident, mcur)],
                         reads=KT.b + QT[kv].b + [CB])
                    act(PTc.ap, pc.t[:, :], AF.Exp, reads=[pc], writes=PTc.b, scale=0.125)
                    if has_prev:
                        pp = ps_next()
                        o3p = pp.t[:, :].rearrange("p (h q) -> p h q", h=4)
                        S.mm(pp, o3p, [(KT.ap[0:64, kv, i * 128:(i + 1) * 128], qr), (ident, mprev)],
                             reads=KT.b + QT[kv].b + [CB])
                        act(PTp.ap, pp.t[:, :], AF.Exp, reads=[pp], pw=PTp.b, scale=0.125)
                    po = ps_next()
                    pd = ps_next()
                    tv = [(Vv.ap[:, 1 + i, kv * 64:(kv + 1) * 64], PTc.ap)]
                    td = [(ones[:, 0:64], PTc.ap)]
                    if has_prev:
                        tv.append((Vv.ap[:, i, kv * 64:(kv + 1) * 64], PTp.ap))
                        td.append((ones[:, 0:64], PTp.ap))
                    S.mm(po, po.t[0:64, :], tv, reads=Vv.b + PTc.b)
                    S.mm(pd, pd.t[0:64, :], td, reads=PTc.b + [CB])
                    esk = ESK.t[:, l, 4 * kv:4 * kv + 4].unsqueeze(2).to_broadcast([64, 4, 128])
                    d13 = d1.ap[0:64].rearrange("p (h q) -> p h q", h=4)
                    tt("dve", d13, pd.t[0:64, :].rearrange("p (h q) -> p h q", h=4), esk, ALU.add, reads=[pd, ESK], writes=d1.b)
                    S.op("dve", lambda e, d13=d13: e.reciprocal(d13, d13), reads=d1.b, writes=d1.b)
                    tt("dve", AOT[kv].ap[0:64, :, i * 128:(i + 1) * 128], po.t[0:64, :].rearrange("p (h q) -> p h q", h=4), d13,
                       ALU.mult, reads=[po] + d1.b, pw=AOT[kv].b)
            if g == 0 and l == 0:
                dbg_dump("d_aot0", SLt[2][:, :], AOT[0].b)
                dbg_dump("d_aot1", SLt[3][:, :], AOT[1].b)
            return AOT

        def phase_ssm(g, l):
            Wss = ring_next()
            wss = rv(Wss, [8, 512])
            wload(Wss, wss, kview(w_in_d[l])[:, :, C_SSM:C_SSM + 512])
            UT = sv(0, BF16, 0, [4, G])
            YG = UT
            GLUT = sv(10, BF16, 0, [4, G])
            dsk = sv(6, BF16, 4096, [4, 128])
            S.dma("sp", dsk.ap, ssmd_d[l], reads=[B_ssmd], writes=dsk.b)
            for j in range(4):
                for s_ in range(2):
                    p = ps_next()
                    S.mm(p, p.t[:, :], [(wss[:, k, j * 128:(j + 1) * 128], xt(k, s_ * 512, (s_ + 1) * 512)) for k in range(8)],
                         reads=[Wss, XTb[s_]])
                    cp("act", UT.ap[:, j, s_ * 512:(s_ + 1) * 512], p.t[:, :], reads=[p], pw=[UT.b[j]])
            t1 = sv(7, F32, 0, [G]); t2 = sv(7, F32, 4096, [G])
            t3 = sv(8, F32, 0, [G]); t4 = sv(8, F32, 4096, [G])
            gre = sv(9, F32, 0, [G]); gim = sv(9, F32, 4096, [G])
            pr = [sv(1, BF16, i * 2048, [G]) for i in range(4)]
            BRb = [PS[4], PS[5]]
            BIb = [PS[6], PS[7]]
            bre_ps = PSD[2][:, :]
            bim_ps = PSD[3][:, :]
            for j in range(4):
                yp = [PS[(j % 2) * 2], PS[(j % 2) * 2 + 1]]
                for s_ in range(2):
                    S.op("pe", lambda e, j=j, s_=s_, yp=yp: e.matmul(yp[s_].t[:, :], dsk.ap[:, j, :], UT.ap[:, j, s_ * 512:(s_ + 1) * 512], start=True, stop=False),
                         reads=dsk.b + [UT.b[j]], writes=[yp[s_]], inc=False)
                for qq in range(4):
                    q = 4 * j + qq
                    tb = sv(4 + q % 2, F32, 0, [2, G])
                    tbw = sv(6, BF16, (q % 2) * 2048, [4, 128])
                    S.dma("sp", tb.ap, ssmt_d[l, q], reads=[B_ssmt], writes=tb.b)
                    S.dma("sp", tbw.ap, ssmw_d[l, q], reads=[B_ssmw], writes=tbw.b)
                    rb = SSMR.t[:, l, q:q + 1].to_broadcast([128, G])
                    for s_ in range(2):
                        c0, c1 = s_ * 512, (s_ + 1) * 512
                        S.mm(BRb[s_], BRb[s_].t[:, :], [(tbw.ap[:, 0, :], UT.ap[:, j, c0:c1])], reads=tbw.b + [UT.b[j]])
                        S.mm(BIb[s_], BIb[s_].t[:, :], [(tbw.ap[:, 1, :], UT.ap[:, j, c0:c1])], reads=tbw.b + [UT.b[j]])
                    Tc = tb.ap[:, 0, :]
                    Ts = tb.ap[:, 1, :]
                    tt("dve", t1.ap, bre_ps, Tc, ALU.mult, reads=BRb + tb.b, writes=t1.b)
                    tt("dve", t2.ap, bim_ps, Ts, ALU.mult, reads=BIb + tb.b, writes=t2.b)
                    tt("dve", t3.ap, bim_ps, Tc, ALU.mult, reads=BIb + tb.b, writes=t3.b)
                    tt("dve", t4.ap, bre_ps, Ts, ALU.mult, reads=BRb + tb.b, writes=t4.b)
                    tt("dve", t1.ap, t1.ap, t2.ap, ALU.add, reads=t1.b + t2.b, writes=t1.b)
                    tt("dve", t3.ap, t3.ap, t4.ap, ALU.subtract, reads=t3.b + t4.b, writes=t3.b)
                    S.op("dve", lambda e, rb=rb, q=q: e.tensor_tensor_scan(gre.ap, rb, t1.ap, SSMC.t[:, l, q, 0:1], ALU.mult, ALU.add),
                         reads=t1.b + [SSMR, SSMC], writes=gre.b)
                    S.op("dve", lambda e, rb=rb, q=q: e.tensor_tensor_scan(gim.ap, rb, t3.ap, SSMC.t[:, l, q, 1:2], ALU.mult, ALU.add),
                         reads=t3.b + [SSMR, SSMC], writes=gim.b)
                    GB = gre.b + gim.b
                    tt("dve", pr[0].ap, gre.ap, Tc, ALU.mult, reads=GB + tb.b, writes=pr[0].b)
                    stt(pr[1].ap, gim.ap, -1.0, Ts, ALU.mult, ALU.mult, reads=GB + tb.b, writes=pr[1].b)
                    tt("dve", pr[2].ap, gre.ap, Ts, ALU.mult, reads=GB + tb.b, writes=pr[2].b)
                    tt("dve", pr[3].ap, gim.ap, Tc, ALU.mult, reads=GB + tb.b, writes=pr[3].b)
                    lastq = qq == 3
                    for s_ in range(2):
                        c0, c1 = s_ * 512, (s_ + 1) * 512
                        for i in range(4):
                            fin = lastq and i == 3
                            S.op("pe", lambda e, s_=s_, yp=yp, tbw=tbw, i=i, c0=c0, c1=c1, fin=fin:
                                 e.matmul(yp[s_].t[:, :], tbw.ap[:, 2 + i // 2, :], pr[i].ap[:, c0:c1], start=False, stop=fin),
                                 reads=tbw.b + pr[i].b, pwrites=[yp[s_]], inc=(i == 3))
                    cz = SM2.t[:, 16:17]
                    Tcl = tb.ap[:, 0, G - 1:G]
                    Tsl = tb.ap[:, 1, G - 1:G]
                    GBt = GB + tb.b
                    ts("dve", cz, gim.ap[:, G - 1:G], Tsl, None, ALU.mult, None, reads=GBt, pw=[SM2])
                    stt(SSMC.t[:, l, q, 0:1], gre.ap[:, G - 1:G], Tcl, cz, ALU.mult, ALU.subtract, reads=GBt + [SM2], pw=[SSMC])
                    ts("dve", cz, gim.ap[:, G - 1:G], Tcl, None, ALU.mult, None, reads=GBt, pw=[SM2])
                    stt(SSMC.t[:, l, q, 1:2], gre.ap[:, G - 1:G], Tsl, cz, ALU.mult, ALU.add, reads=GBt + [SM2], pw=[SSMC])
                for s_ in range(2):
                    act(YG.ap[:, j, s_ * 512:(s_ + 1) * 512], yp[s_].t[:, :], AF.Gelu_apprx_tanh, reads=[yp[s_]], pw=[YG.b[j]])
            Wgl = ring_next()
            wgl = rv(Wgl, [4, 1024])
            wload(Wgl, wgl, kview(w_glu_d[l]))
            for c in range(4):
                for s_ in range(2):
                    phv = ps_next()
                    phg = ps_next()
                    S.mm(phv, phv.t[:, :], [(wgl[:, k, c * 128:(c + 1) * 128], YG.ap[:, k, s_ * 512:(s_ + 1) * 512]) for k in range(4)],
                         reads=[Wgl] + YG.b)
                    S.mm(phg, phg.t[:, :], [(wgl[:, k, 512 + c * 128:512 + (c + 1) * 128], YG.ap[:, k, s_ * 512:(s_ + 1) * 512]) for k in range(4)],
                         reads=[Wgl] + YG.b)
                    sg = sv(7, F32, ((c * 2 + s_) % 2) * 2048, [512])
                    act(sg.ap, phg.t[:, :], AF.Sigmoid, reads=[phg], writes=sg.b)
                    tt("dve", GLUT.ap[:, c, s_ * 512:(s_ + 1) * 512], phv.t[:, :], sg.ap, ALU.mult, reads=[phv] + sg.b, pw=[GLUT.b[c]])
            if g == 0 and l == 0:
                dbg_dump("d_glut", SLt[10][:, :], GLUT.b)
            return GLUT

        def phase_pool(g, l):
            Wpo = ring_next()
            wpo = rv(Wpo, [8, 512])
            wload(Wpo, wpo, kview(w_in_d[l])[:, :, C_POOL:C_POOL + 512])
            Wpw = ring_next()
            wpw = rv(Wpw, [4, 128])
            wload(Wpw, wpw, pool_w_d[l].rearrange("g c d -> c g d"))
            PL = sv(5, BF16, 0, [4, G])
            PMT = sv(6, BF16, 0, [4, G])
            L = G + 16
            for j in range(4):
                UP = sv(0, F32, 0, [L])
                SA = sv(1, F32, 0, [L])
                SB_ = sv(4, F32, 0, [L])
                cp("dve", UP.ap[:, 0:16], UPC.t[:, l, j, :], reads=[UPC], writes=UP.b)
                for s_ in range(2):
                    p = ps_next()
                    S.mm(p, p.t[:, :], [(wpo[:, k, j * 128:(j + 1) * 128], xt(k, s_ * 512, (s_ + 1) * 512)) for k in range(8)],
                         reads=[Wpo, XTb[s_]])
                    cp("act", UP.ap[:, 16 + s_ * 512:16 + (s_ + 1) * 512], p.t[:, :], reads=[p], pw=UP.b)
                cp("dve", UPC.t[:, l, j, :], UP.ap[:, G:G + 16], reads=UP.b, writes=[UPC])
                cur, nxt, other = UP, SA, SB_
                for s in range(1, j + 2):
                    sh = 1 << (s - 1)
                    lo = (1 << s) - 1
                    tt("dve", nxt.ap[:, lo:L], cur.ap[:, lo:L], cur.ap[:, lo - sh:L - sh], ALU.add, reads=cur.b, writes=nxt.b)
                    cur, nxt = nxt, (other if nxt is SA else SA)
                w = 1 << (j + 1)
                stt(PL.ap[:, j, :], cur.ap[:, 16:L], 1.0 / w, UP.ap[:, 16:L], ALU.mult, ALU.subtract, reads=cur.b + UP.b, writes=[PL.b[j]])
                if g == 0:
                    tmp = SM.t[:, 48:64]
                    tt("dve", tmp, cur.ap[:, 16:32], INVC[:, j, :], ALU.mult, reads=cur.b + [CF], pw=[SM])
                    tt("dve", PL.ap[:, j, 0:16], tmp, UP.ap[:, 16:32], ALU.subtract, reads=[SM] + UP.b, pw=[PL.b[j]])
                for s_ in range(2):
                    pm = ps_next()
                    S.mm(pm, pm.t[:, :], [(wpw[:, j, :], PL.ap[:, j, s_ * 512:(s_ + 1) * 512])], reads=[Wpw, PL.b[j]])
                    act(PMT.ap[:, j, s_ * 512:(s_ + 1) * 512], pm.t[:, :], AF.Identity, reads=[pm, PSC], pw=[PMT.b[j]],
                        scale=PSC.t[:, l, j:j + 1])
            if g == 0 and l == 0:
                dbg_dump("d_pmt", SLt[6][:, :], PMT.b)
            return PMT

        def phase_merge(g, l, AOT, GLUT, PMT):
            WA = [sv(4, BF16, 0, [4, D]), sv(5, BF16, 0, [4, D])]
            for hf in range(2):
                S.dma("pool", WA[hf].ap[0:64], w_bra_d[l].rearrange("(h p) d -> p h d", p=64)[:, hf * 4:(hf + 1) * 4, :], writes=WA[hf].b)
            WSs = sv(9, BF16, 0, [4, D])
            S.dma("pool", WSs.ap, kview(w_brs_d[l]), writes=WSs.b)
            r = [ring_next() for _ in range(4)]
            wbp = rv(r[0], [4, D])
            wload(r[0], wbp, kview(w_brp_d[l]))
            MT = [sv(7, BF16, 0, [4, G]), sv(8, BF16, 0, [4, G])]
            for hf in range(2):
                gw = []
                for i in range(3):
                    gv = rv(r[1 + i], [8, 512])
                    c0 = C_GATE + i * D + hf * 512
                    wload(r[1 + i], gv, kview(w_in_d[l])[:, :, c0:c0 + 512])
                    gw.append(gv)
                for d4 in range(4):
                    dc = hf * 4 + d4
                    for s_ in range(2):
                        c0, c1 = s_ * 512, (s_ + 1) * 512
                        pg = [ps_next() for _ in range(3)]
                        po = [ps_next() for _ in range(3)]
                        for i in range(3):
                            S.mm(pg[i], pg[i].t[:, :], [(gw[i][:, k, d4 * 128:(d4 + 1) * 128], xt(k, c0, c1)) for k in range(8)],
                                 reads=[r[1 + i], XTb[s_]])
                        S.mm(po[0], po[0].t[:, :],
                             [(WA[h // 4].ap[0:64, h % 4, dc * 128:(dc + 1) * 128], AOT[h // 4].ap[0:64, h % 4, c0:c1]) for h in range(8)],
                             reads=WA[0].b + WA[1].b + AOT[0].b + AOT[1].b)
                        S.mm(po[1], po[1].t[:, :], [(WSs.ap[:, k, dc * 128:(dc + 1) * 128], GLUT.ap[:, k, c0:c1]) for k in range(4)],
                             reads=WSs.b + GLUT.b)
                        S.mm(po[2], po[2].t[:, :], [(wbp[:, k, dc * 128:(dc + 1) * 128], PMT.ap[:, k, c0:c1]) for k in range(4)],
                             reads=[r[0]] + PMT.b)
                        sg = [sv(0, F32, i * 2048, [512]) for i in range(3)]
                        for i in range(3):
                            act(sg[i].ap, pg[i].t[:, :], AF.Sigmoid, reads=[pg[i], BG], writes=sg[i].b,
                                bias=BG.t[:, l, i * 8 + dc:i * 8 + dc + 1])
                        m1 = sv(1, F32, 0, [512])
                        m2 = sv(1, F32, 2048, [512])
                        tt("dve", m1.ap, po[0].t[:, :], sg[0].ap, ALU.mult, reads=[po[0]] + sg[0].b, writes=m1.b)
                        tt("dve", m2.ap, po[1].t[:, :], sg[1].ap, ALU.mult, reads=[po[1]] + sg[1].b, writes=m2.b)
                        tt("dve", m1.ap, m1.ap, m2.ap, ALU.add, reads=m1.b + m2.b, writes=m1.b)
                        tt("dve", m2.ap, po[2].t[:, :], sg[2].ap, ALU.mult, reads=[po[2]] + sg[2].b, writes=m2.b)
                        tt("dve", MT[hf].ap[:, d4, c0:c1], m1.ap, m2.ap, ALU.add, reads=m1.b + m2.b, pw=[MT[hf].b[d4]])
            if g == 0 and l == 0:
                dbg_dump("d_mt0", SLt[7][:, :], MT[0].b)
                dbg_dump("d_mt1", SLt[8][:, :], MT[1].b)
            return MT

        def phase_xattn(g, l):
            KKT = sv(0, BF16, 0, [8, NMEM])
            VV = sv(0, BF16, 4096, [2, D])
            QXT = [sv(1, BF16, 0, [4, G]), sv(2, BF16, 0, [4, G])]
            OXT = [sv(4, BF16, 0, [4, G]), sv(5, BF16, 0, [4, G])]
            WK = []
            for u in range(4):
                rr_ = ring_next()
                wload(rr_, rv(rr_, [8, 512]), kview(xa_wkv_d[l])[:, :, u * 512:(u + 1) * 512])
                WK.append(rr_)
            for c in range(8):
                p = ps_next()
                w = rv(WK[c // 4], [8, 512])
                S.mm(p, p.t[:, 0:NMEM], [(w[:, k, (c % 4) * 128:(c % 4 + 1) * 128], MEMT.t[:, k, :]) for k in range(8)],
                     reads=[WK[c // 4], MEMT])
                cp("act", KKT.ap[:, c, :], p.t[:, 0:NMEM], reads=[p], pw=KKT.b)
            for mt in range(2):
                for hf in range(2):
                    p = ps_next()
                    w = rv(WK[2 + hf], [8, 512])
                    S.mm(p, p.t[:, :], [(MEMT.t[:, k, mt * 128:(mt + 1) * 128], w[:, k, :]) for k in range(8)], reads=[WK[2 + hf], MEMT])
                    cp("act", VV.ap[:, mt, hf * 512:(hf + 1) * 512], p.t[:, :], reads=[p], pw=VV.b)
            WQ = []
            for u in range(2):
                rr_ = ring_next()
                wload(rr_, rv(rr_, [8, 512]), kview(xa_wq_d[l])[:, :, u * 512:(u + 1) * 512])
                WQ.append(rr_)
            for c in range(8):
                for s_ in range(2):
                    p = ps_next()
                    w = rv(WQ[c // 4], [8, 512])
                    S.mm(p, p.t[:, :], [(w[:, k, (c % 4) * 128:(c % 4 + 1) * 128], xt(k, s_ * 512, (s_ + 1) * 512)) for k in range(8)],
                         reads=[WQ[c // 4], XTb[s_]])
                    cp("act", QXT[c // 4].ap[:, c % 4, s_ * 512:(s_ + 1) * 512], p.t[:, :], reads=[p], pw=[QXT[c // 4].b[c % 4]])
            it = 0
            for h in range(4):
                for s_ in range(2):
                    c0, c1 = s_ * 512, (s_ + 1) * 512
                    PT = [sv(3, BF16, (it % 2) * 2048 + mt * 1024, [512]) for mt in range(2)]
                    rden = sv(3, F32, 4096 + (it % 2) * 2048, [512])
                    it += 1
                    for mt in range(2):
                        p = ps_next()
                        S.mm(p, p.t[:, :],
                             [(KKT.ap[:, 2 * h + dcc, mt * 128:(mt + 1) * 128], QXT[(2 * h + dcc) // 4].ap[:, (2 * h + dcc) % 4, c0:c1]) for dcc in range(2)],
                             reads=KKT.b + QXT[(2 * h) // 4].b)
                        if mt == 0:
                            act(PT[mt].ap, p.t[:, :], AF.Exp, reads=[p], writes=PT[mt].b, scale=1.0 / 16.0)
                        else:
                            act(PT[mt].ap, p.t[:, :], AF.Exp, reads=[p], pw=PT[mt].b, scale=1.0 / 16.0)
                    pd = ps_next()
                    S.mm(pd, pd.t[:, :], [(ones, PT[mt].ap) for mt in range(2)], reads=PT[0].b + [CB])
                    S.op("dve", lambda e, rden=rden, pd=pd: e.reciprocal(rden.ap, pd.t[:, :]), reads=[pd], writes=rden.b)
                    for dcc in range(2):
                        po = ps_next()
                        c = 2 * h + dcc
                        S.mm(po, po.t[:, :], [(VV.ap[:, mt, c * 128:(c + 1) * 128], PT[mt].ap) for mt in range(2)], reads=VV.b + PT[0].b)
                        tt("dve", OXT[c // 4].ap[:, c % 4, c0:c1], po.t[:, :], rden.ap, ALU.mult, reads=[po] + rden.b, pw=[OXT[c // 4].b[c % 4]])
            return OXT

        def phase_ffn(g, l):
            moe = (l % 2 == 1)
            jj = l // 2
            for t in range(NT):
                act(X[t].t[:, :], X[t].t[:, :], AF.Identity, reads=[X[t]], writes=[X[t]], scale=ALPHA)
            if moe:
                Wr = ring_next()
                wr = rv(Wr, [8, NEXP])
                S.dma("pool", wr, moe_wr_d[jj].rearrange("(k p) e -> p k e", p=128), writes=[Wr], **NCD)
                S.dma("sp", BR.t[:, :], moe_br_d[jj].partition_broadcast(128), writes=[BR], **NCD)
                pl = ps_next()
                for t in range(NT):
                    S.mm(pl, pl.t[:, t * 8:(t + 1) * 8], [(xt(k, t * 128, (t + 1) * 128), wr[:, k, :]) for k in range(8)],
                         reads=[Wr, XTb[t // 4]])
                lg = RT.t[:, :, 0:8]
                eq = RT.t[:, :, 8:16]
                l2 = RT.t[:, :, 16:24]
                ex = RT.t[:, :, 24:32]
                m1 = RT.t[:, :, 32:33]
                m2 = RT.t[:, :, 33:34]
                dn = RT.t[:, :, 34:35]
                R_ = [RT]
                tt("dve", lg, pl.t[:, 0:64].rearrange("p (t e) -> p t e", t=NT), BR.t[:, :].unsqueeze(1).to_broadcast([128, NT, NEXP]),
                   ALU.add, reads=[pl, BR], writes=R_)
                S.op("dve", lambda e: e.tensor_reduce(m1, lg, AX.X, ALU.max), reads=R_, writes=R_)
                tt("dve", eq, lg, m1.to_broadcast([128, NT, NEXP]), ALU.is_equal, reads=R_, writes=R_)
                S.op("dve", lambda e: e.scalar_tensor_tensor(l2, eq, -1e30, lg, ALU.mult, ALU.add), reads=R_, writes=R_)
                S.op("dve", lambda e: e.tensor_reduce(m2, l2, AX.X, ALU.max), reads=R_, writes=R_)
                tt("dve", eq, lg, m2.to_broadcast([128, NT, NEXP]), ALU.is_ge, reads=R_, writes=R_)
                tt("dve", l2, lg, m1.to_broadcast([128, NT, NEXP]), ALU.subtract, reads=R_, writes=R_)
                act(ex, l2, AF.Exp, reads=R_, writes=R_)
                tt("dve", ex, ex, eq, ALU.mult, reads=R_, writes=R_)
                S.op("dve", lambda e: e.tensor_reduce(dn, ex, AX.X, ALU.add), reads=R_, writes=R_)
                S.op("dve", lambda e: e.reciprocal(dn, dn), reads=R_, writes=R_)
                tt("dve", CW.t[:, :, :], ex, dn.to_broadcast([128, NT, NEXP]), ALU.mult, reads=R_, writes=[CW])
                nexp, F, gu_of, dn_of = NEXP, DFFE, (lambda e_: moe_gu_d[jj, e_]), (lambda e_: moe_dn_d[jj, e_])
            else:
                nexp, F, gu_of, dn_of = 1, DFF, (lambda e_: ffn_gu_d[jj]), (lambda e_: ffn_dn_d[jj])
            nch = F // 128
            fgs = []
            c = 0
            while c < nch:
                n = min(4, nch - c)
                fgs.append((c, n))
                c += n
            fi = 0
            for e_ in range(nexp):
                gu = kview(gu_of(e_))
                dnw = dn_of(e_)
                for (c0, n) in fgs:
                    Wg = ring_next(); Wu = ring_next(); Wd = ring_next()
                    wg = rv(Wg, [8, n * 128]); wu = rv(Wu, [8, n * 128]); wd = rv(Wd, [n, D])
                    wload(Wg, wg, gu[:, :, c0 * 128:(c0 + n) * 128])
                    wload(Wu, wu, gu[:, :, F + c0 * 128:F + (c0 + n) * 128])
                    wload(Wd, wd, dnw[c0 * 128:(c0 + n) * 128, :].rearrange("(c p) d -> p c d", p=128))
                    HT = sv(fi % 2, BF16, 0, [4, G])
                    fi += 1
                    for s_ in range(2):
                        a0, a1 = s_ * 512, (s_ + 1) * 512
                        for fc in range(n):
                            pg = ps_next()
                            pu = ps_next()
                            S.mm(pg, pg.t[:, :], [(wg[:, k, fc * 128:(fc + 1) * 128], xt(k, a0, a1)) for k in range(8)], reads=[Wg, XTb[s_]])
                            S.mm(pu, pu.t[:, :], [(wu[:, k, fc * 128:(fc + 1) * 128], xt(k, a0, a1)) for k in range(8)], reads=[Wu, XTb[s_]])
                            sg = sv(2, F32, ((fc + s_ * n) % 3) * 2048, [512])
                            act(sg.ap, pg.t[:, :], AF.Silu, reads=[pg], writes=sg.b)
                            tt("dve", HT.ap[:, fc, a0:a1], pu.t[:, :], sg.ap, ALU.mult, reads=[pu] + sg.b, pw=[HT.b[fc]])
                    for s_ in range(2):
                        for t4 in range(4):
                            t = s_ * 4 + t4
                            for hf in range(2):
                                p = ps_next()
                                S.mm(p, p.t[:, :], [(HT.ap[:, fc, t * 128:(t + 1) * 128], wd[:, fc, hf * 512:(hf + 1) * 512]) for fc in range(n)],
                                     reads=HT.b[0:n] + [Wd])
                                xa = X[t].t[:, hf * 512:(hf + 1) * 512]
                                if moe:
                                    stt(xa, p.t[:, :], CW.t[:, t, e_:e_ + 1], xa, ALU.mult, ALU.add, reads=[p, CW, X[t]], pw=[X[t]])
                                else:
                                    tt("dve", xa, xa, p.t[:, :], ALU.add, reads=[p, X[t]], pw=[X[t]])

        for g in range(ng):
            for t in range(NT):
                S.dma("sp", X[t].t[:, :], x_d[g * G + t * 128:g * G + (t + 1) * 128, :], writes=[X[t]])
                make_xt(t)
            PH = phases if phases is not None else {"attn", "ssm", "pool", "merge", "wo", "xattn", "xo", "ffn"}
            for l in range(depth):
                AOT = [sv(2, BF16, 0, [4, G]), sv(3, BF16, 0, [4, G])]
                GLUT = sv(10, BF16, 0, [4, G])
                PMT = sv(6, BF16, 0, [4, G])
                MT = [sv(7, BF16, 0, [4, G]), sv(8, BF16, 0, [4, G])]
                OXT = [sv(4, BF16, 0, [4, G]), sv(5, BF16, 0, [4, G])]
                if "attn" in PH:
                    AOT = phase_attn(g, l)
                if "ssm" in PH:
                    GLUT = phase_ssm(g, l)
                if "pool" in PH:
                    PMT = phase_pool(g, l)
                if "merge" in PH:
                    MT = phase_merge(g, l, AOT, GLUT, PMT)
                if "wo" in PH:
                    out_proj(lambda k: MT[k // 4].ap[:, k % 4, :], MT[0].b + MT[1].b, w_o_d[l], l, 0, g, False)
                if "xattn" in PH:
                    OXT = phase_xattn(g, l)
                if "xo" in PH:
                    out_proj(lambda k: OXT[k // 4].ap[:, k % 4, :], OXT[0].b + OXT[1].b, xa_wo_d[l], l, 1, g, False)
                if "ffn" in PH:
                    phase_ffn(g, l)
                layernorm(2, l, g, l == depth - 1)
        fin = [B_out] + ([B_dbg] if dbg else [])
        S.finish(fin)
        S.emit()
        build.stats = (S.ninst, S.nsem, {e: len(v) for e, v in S.ops.items()})
    return nc


def make_consts(seq):
    bf = ml_dtypes.bfloat16
    c_bf = np.zeros((128, 1280), np.float32)
    c_bf[:, 0:128] = np.eye(128)
    c_bf[:, 128:256] = 1.0
    k = np.arange(128)[:, None]
    q = np.arange(128)[None, :]
    mc = np.where(k <= q, 0.0, -30000.0)
    mp = np.where(k > q, 0.0, -30000.0)
    c_bf[:, 256:768] = np.tile(mc, (1, 4))
    c_bf[:, 768:1280] = np.tile(mp, (1, 4))
    c_f = np.zeros((128, 72), np.float32)
    for j, w in enumerate((2, 4, 8, 16)):
        c_f[:, j * 16:(j + 1) * 16] = 1.0 / np.minimum(np.arange(1, 17), w)
    p = np.arange(128)
    for qq in range(4):
        c_f[:, 64 + qq] = (p // 32 == qq)
    c_f[:, 68] = ((p // 16) % 2 == 0)
    c_f[:, 69] = ((p // 16) % 2 == 1)
    half = 8
    inv_freq = np.power(np.float32(500000.0), -np.arange(half, dtype=np.float32) / half).astype(np.float32)
    ang = np.arange(seq, dtype=np.float32)[None, :] * inv_freq[:, None]
    c_rope = np.zeros((32, 2, seq), np.float32)
    c_rope[:, 0, :] = 1.0
    c_rope[0:8, 0, :] = np.cos(ang)
    c_rope[8:16, 0, :] = np.cos(ang)
    c_rope[0:8, 1, :] = -np.sin(ang)
    c_rope[8:16, 1, :] = np.sin(ang)
    return c_bf.astype(bf), c_f, c_rope


_NC_CACHE = {}


def kernel(**inputs):
    x = np.asarray(inputs["x"], np.float32)
    B, SEQ, _ = x.shape
    ng = SEQ // G
    key = (ng, DEPTH)
    if key not in _NC_CACHE:
        _NC_CACHE[key] = build(ng, DEPTH)
    nc = _NC_CACHE[key]
    c_bf, c_f, c_rope = make_consts(SEQ)
    shared = {k: np.ascontiguousarray(np.asarray(v, np.float32)) for k, v in inputs.items() if k not in ("x", "mem")}
    shared.update(c_bf=c_bf, c_f=c_f, c_rope=c_rope)
    in_maps = []
    for b in range(B):
        m = dict(shared)
        m["x"] = np.ascontiguousarray(x[b])
        m["mem"] = np.ascontiguousarray(np.asarray(inputs["mem"], np.float32)[b])
        in_maps.append(m)
    res = run_bass_kernel_spmd(nc, in_maps, core_ids=list(range(B)))
    return np.stack([np.asarray(r["out"], np.float32) for r in res.results], axis=0)
```

```python
import math
from contextlib import ExitStack

import ml_dtypes
import numpy as np

import concourse.bass as bass
import concourse.mybir as mybir
from concourse.bass_utils import run_bass_kernel_spmd

F32 = mybir.dt.float32
BF16 = mybir.dt.bfloat16
I32 = mybir.dt.int32
AF = mybir.ActivationFunctionType
ALU = mybir.AluOpType
AX = mybir.AxisListType

D = 1024
G = 1024
NT = G // 128
NMEM = 256
DFF = 2816
DFFE = 3584
NEXP = 8
DEPTH = 4
ALPHA = (2.0 * DEPTH) ** 0.25
LN_EPS = 1e-5
IN_W = 4864
C_Q, C_K, C_V, C_SSM, C_POOL, C_GATE = 0, 512, 640, 768, 1280, 1792
SLOT_B = 8192
NSLOT = 11
NRING = 6
TWO_PI = 2.0 * math.pi


class Buf:
    def __init__(self, name, t=None):
        self.name = name
        self.t = t
        self.w = {}
        self.r = {}
        self.sem = None
        self.dcnt = 0


class Sched:
    ENG = ("pe", "act", "dve", "pool", "sp")

    def __init__(self, nc, stack, same_eng_sync=("act", "dve", "pool")):
        self.nc = nc
        self.stack = stack
        self.same = set(same_eng_sync)
        self.ops = {e: [] for e in self.ENG}
        self.sem = {e: stack.enter_context(nc.semaphore("s_" + e)) for e in self.ENG}
        self.cnt = {e: 0 for e in self.ENG}
        self.waited = {e: {} for e in self.ENG}
        self.nsem = 5
        self.ninst = 0

    @staticmethod
    def _add(deps, k, h, v):
        if k not in deps or deps[k][1] < v:
            deps[k] = (h, v)

    def _deps(self, reads, writes, pwrites):
        deps = {}
        for b in reads:
            for k, (h, v, _p) in b.w.items():
                self._add(deps, k, h, v)
        for b in writes:
            for k, (h, v, _p) in b.w.items():
                self._add(deps, k, h, v)
            for k, (h, v) in b.r.items():
                self._add(deps, k, h, v)
        for b in pwrites:
            for k, (h, v, p) in b.w.items():
                if not p:
                    self._add(deps, k, h, v)
            for k, (h, v) in b.r.items():
                self._add(deps, k, h, v)
        return deps

    def _waits(self, eng, deps):
        for k, (h, v) in deps.items():
            if k == eng and eng not in self.same:
                continue
            if self.waited[eng].get(k, 0) >= v:
                continue
            self.waited[eng][k] = v
            self.ops[eng].append(lambda e, h=h, v=v: e.wait_ge(h, v))
            self.ninst += 1

    def _reg(self, t, reads, writes, pwrites):
        k, h, v = t
        for b in reads:
            if k not in b.r or b.r[k][1] < v:
                b.r[k] = (h, v)
        for b in writes:
            b.w = {k: (h, v, False)}
            b.r = {}
        for b in pwrites:
            if b.r:
                b.w = {k: (h, v, True)}
                b.r = {}
            else:
                if k not in b.w or b.w[k][1] < v:
                    b.w[k] = (h, v, True)

    def op(self, eng, fn, reads=(), writes=(), pwrites=(), inc=True):
        self._waits(eng, self._deps(reads, writes, pwrites))
        sem = self.sem[eng]
        if inc:
            self.cnt[eng] += 1
            n = self.cnt[eng]
            self.ops[eng].append(lambda e, fn=fn, sem=sem: fn(e).then_inc(sem, 1))
        else:
            n = self.cnt[eng] + 1
            self.ops[eng].append(lambda e, fn=fn: fn(e))
        self.ninst += 1
        self._reg((eng, sem, n), reads, writes, pwrites)

    def dma(self, q, out_ap, in_ap, reads=(), writes=(), pwrites=(), **kw):
        dst = (list(writes) + list(pwrites))[0]
        if dst.sem is None:
            dst.sem = self.stack.enter_context(self.nc.semaphore("d_" + dst.name))
            self.nsem += 1
        self._waits(q, self._deps(reads, writes, pwrites))
        dst.dcnt += 16
        sem = dst.sem
        self.ops[q].append(
            lambda e, o=out_ap, i=in_ap, sem=sem, kw=kw: e.dma_start(out=o, in_=i, **kw).then_inc(sem, 16)
        )
        self.ninst += 1
        self._reg(("d_" + dst.name, sem, dst.dcnt), reads, writes, pwrites)

    def mm(self, ps, out_ap, terms, reads):
        n = len(terms)
        for i, (l, r) in enumerate(terms):
            first, last = i == 0, i == n - 1
            self.op(
                "pe",
                lambda e, l=l, r=r, first=first, last=last, o=out_ap: e.matmul(o, l, r, start=first, stop=last),
                reads=reads if first else (),
                writes=[ps] if first else (),
                inc=last,
            )

    def finish(self, bufs):
        deps = {}
        for b in bufs:
            for k, (h, v, _p) in b.w.items():
                self._add(deps, k, h, v)
        self._waits("sp", deps)

    def emit(self):
        nc = self.nc
        ops = self.ops
        with nc.Block() as block:
            @block.tensor
            def _(e):
                for f in ops["pe"]:
                    f(e)

            @block.scalar
            def _(e):
                for f in ops["act"]:
                    f(e)

            @block.vector
            def _(e):
                for f in ops["dve"]:
                    f(e)

            @block.gpsimd
            def _(e):
                for f in ops["pool"]:
                    f(e)

            @block.sync
            def _(e):
                for f in ops["sp"]:
                    f(e)


class V:
    def __init__(self, ap, b):
        self.ap = ap
        self.b = b


def build(ng, depth, dbg=False, phases=None, same=("act", "dve", "pool")):
    nc = bass.Bass("TRN2", target_bir_lowering=False)
    SEQ = ng * G
    nden = (depth + 1) // 2
    nmoe = depth // 2

    def din(name, shape, dt=F32):
        return nc.dram_tensor(name, list(shape), dt, kind="ExternalInput").ap()

    x_d = din("x", [SEQ, D])
    mem_d = din("mem", [NMEM, D])
    w_in_d = din("w_in", [depth, D, IN_W])
    b_gate_d = din("b_gate", [depth, 3 * D])
    sinks_d = din("attn_sinks", [depth, 8])
    a_re_d = din("ssm_a_re", [depth, 32, 64])
    a_im_d = din("ssm_a_im", [depth, 32, 64])
    ldt_d = din("ssm_log_dt", [depth, 32])
    b_re_d = din("ssm_b_re", [depth, 32, 64, 16])
    b_im_d = din("ssm_b_im", [depth, 32, 64, 16])
    c_re_d = din("ssm_c_re", [depth, 32, 16, 64])
    c_im_d = din("ssm_c_im", [depth, 32, 16, 64])
    ssm_d_d = din("ssm_d", [depth, 512])
    w_glu_d = din("ssm_w_glu", [depth, 512, 1024])
    pool_w_d = din("pool_w", [depth, 4, 128, 128])
    pool_sc_d = din("pool_scale", [depth, 512])
    w_bra_d = din("w_br_attn", [depth, 512, D])
    w_brs_d = din("w_br_ssm", [depth, 512, D])
    w_brp_d = din("w_br_pool", [depth, 512, D])
    w_o_d = din("w_o", [depth, D, D])
    ln_g_d = [din(f"ln{i}_g", [depth, D]) for i in (1, 2, 3)]
    ln_b_d = [din(f"ln{i}_b", [depth, D]) for i in (1, 2, 3)]
    xa_wq_d = din("xa_wq", [depth, D, D])
    xa_wkv_d = din("xa_wkv", [depth, D, 2 * D])
    xa_wo_d = din("xa_wo", [depth, D, D])
    ffn_gu_d = din("ffn_w_gu", [nden, D, 2 * DFF])
    ffn_dn_d = din("ffn_w_down", [nden, DFF, D])
    if nmoe:
        moe_wr_d = din("moe_w_router", [nmoe, D, NEXP])
        moe_br_d = din("moe_b_router", [nmoe, NEXP])
        moe_gu_d = din("moe_w_gu", [nmoe, NEXP, D, 2 * DFFE])
        moe_dn_d = din("moe_w_down", [nmoe, NEXP, DFFE, D])
    c_bf_d = din("c_bf", [128, 1280], BF16)
    c_f_d = din("c_f", [128, 72])
    c_rope_d = din("c_rope", [32, 2, SEQ])
    out_d = nc.dram_tensor("out", [SEQ, D], F32, kind="ExternalOutput").ap()
    ssmt_d = nc.dram_tensor("ssmt", [depth, 16, 128, 2, G], F32).ap()
    ssmw_d = nc.dram_tensor("ssmw", [depth, 16, 128, 4, 128], BF16).ap()
    ssmd_d = nc.dram_tensor("ssmd", [depth, 128, 4, 128], BF16).ap()
    dbg_d = {}
    if dbg:
        for nm in ("d_x1", "d_x2", "d_x3"):
            dbg_d[nm] = nc.dram_tensor(nm, [G, D], F32, kind="ExternalOutput").ap()
        for nm in ("d_aot0", "d_aot1", "d_glut", "d_pmt", "d_mt0", "d_mt1"):
            dbg_d[nm] = nc.dram_tensor(nm, [128, SLOT_B // 2], BF16, kind="ExternalOutput").ap()

    with ExitStack() as st:
        S = Sched(nc, st, same_eng_sync=same)

        def sb(name, shape, dt):
            return Buf(name, st.enter_context(nc.sbuf_tensor(name, list(shape), dt)))

        X = [sb(f"X{i}", [128, D], F32) for i in range(NT)]
        XTt = st.enter_context(nc.sbuf_tensor("XT", [128, 8, G], BF16))
        XTb = [Buf("XT0"), Buf("XT1")]
        CB = sb("CB", [128, 1280], BF16)
        CF = sb("CF", [128, 72], F32)
        MEMT = sb("MEMT", [128, 8, NMEM], BF16)
        SSMC = sb("SSMC", [128, depth, 16, 2], F32)
        KTC = sb("KTC", [64, depth, 2, 128], BF16)
        VC = sb("VC", [128, depth, 128], BF16)
        UPC = sb("UPC", [128, depth, 4, 16], F32)
        SSMR = sb("SSMR", [128, depth, 16], F32)
        ESK = sb("ESK", [64, depth, 8], F32)
        BG = sb("BG", [128, depth, 24], F32)
        PSC = sb("PSC", [128, depth, 4], F32)
        LNG = sb("LNG", [128, 2, D], F32)
        SM = sb("SM", [128, 64], F32)
        SM2 = sb("SM2", [128, 32], F32)
        CW = sb("CW", [128, NT, NEXP], F32)
        RT = sb("RT", [128, 8, 64], F32)
        BR = sb("BRT", [128, NEXP], F32)
        EPS = sb("EPS", [128, 1], F32)
        RING = [sb(f"R{i}", [128, SLOT_B // 2], BF16) for i in range(NRING)]
        SLt = [st.enter_context(nc.sbuf_tensor(f"SL{i}", [128, SLOT_B // 2], BF16)) for i in range(NSLOT)]
        SLq = [[Buf(f"SL{i}q{j}") for j in range(4)] for i in range(NSLOT)]
        PSD = [st.enter_context(nc.psum_tensor(f"psd{i}", [128, 1024], F32)) for i in range(4)]
        PS = [Buf(f"ps{i}", PSD[i // 2][:, (i % 2) * 512:(i % 2 + 1) * 512]) for i in range(8)]
        B_ssmw = Buf("ssmw")
        B_ssmt = Buf("ssmt")
        B_ssmd = Buf("ssmd")
        B_out = Buf("outb")
        B_dbg = Buf("dbgb")
        state = {"ps": 0, "ring": 0}

        def ps_next():
            p = PS[state["ps"] % 8]
            state["ps"] += 1
            return p

        def ps_sub(lo, n, key):
            i = state.get(key, 0)
            state[key] = i + 1
            return PS[lo + i % n]

        def ring_next():
            r = RING[state["ring"] % NRING]
            state["ring"] += 1
            return r

        def sv(i, dt, off_b, shape):
            n = 1
            for s_ in shape:
                n *= s_
            esz = 4 if dt in (F32, I32) else 2
            nb = n * esz
            assert off_b + nb <= SLOT_B, (i, off_b, nb)
            a = SLt[i][:, off_b // 2: (off_b + nb) // 2]
            if dt != BF16:
                a = a.bitcast(dt)
            if len(shape) == 2:
                a = a.rearrange("p (a b) -> p a b", a=shape[0])
            elif len(shape) == 3:
                a = a.rearrange("p (a b c) -> p a b c", a=shape[0], b=shape[1])
            q0, q1 = off_b // 2048, (off_b + nb - 1) // 2048
            return V(a, [SLq[i][j] for j in range(q0, q1 + 1)])

        def rv(rbuf, shape):
            n = 1
            for s_ in shape:
                n *= s_
            a = rbuf.t[:, 0:n]
            if len(shape) == 2:
                a = a.rearrange("p (a b) -> p a b", a=shape[0])
            elif len(shape) == 3:
                a = a.rearrange("p (a b c) -> p a b c", a=shape[0], b=shape[1])
            return a

        ident = CB.t[:, 0:128]
        ones = CB.t[:, 128:256]
        mcur = CB.t[:, 256:768].rearrange("p (h q) -> p h q", h=4)
        mprev = CB.t[:, 768:1280].rearrange("p (h q) -> p h q", h=4)
        INVC = CF.t[:, 0:64].rearrange("p (a b) -> p a b", a=4)
        ROWM = CF.t[:, 64:68]
        EVENM = CF.t[:, 68:69]
        ODDM = CF.t[:, 69:70]

        def act(out, in_, func, reads, writes=(), pw=(), bias=None, scale=None):
            kw = {}
            if bias is not None:
                kw["bias"] = bias
            if scale is not None:
                kw["scale"] = scale
            S.op("act", lambda e: e.activation(out, in_, func, **kw), reads=reads, writes=writes, pwrites=pw)

        def tt(eng, out, in0, in1, op, reads, writes=(), pw=()):
            S.op(eng, lambda e: e.tensor_tensor(out, in0, in1, op), reads=reads, writes=writes, pwrites=pw)

        def ts(eng, out, in0, s1, s2, op0, op1, reads, writes=(), pw=()):
            if s2 is None:
                S.op(eng, lambda e: e.tensor_scalar(out, in0, s1, None, op0), reads=reads, writes=writes, pwrites=pw)
            else:
                S.op(eng, lambda e: e.tensor_scalar(out, in0, s1, s2, op0, op1), reads=reads, writes=writes, pwrites=pw)

        def stt(out, in0, sc, in1, op0, op1, reads, writes=(), pw=()):
            S.op("dve", lambda e: e.scalar_tensor_tensor(out, in0, sc, in1, op0, op1), reads=reads, writes=writes, pwrites=pw)

        def cp(eng, out, in_, reads, writes=(), pw=()):
            if eng == "act":
                S.op("act", lambda e: e.activation(out, in_, AF.Copy), reads=reads, writes=writes, pwrites=pw)
            else:
                S.op(eng, lambda e: e.tensor_copy(out, in_), reads=reads, writes=writes, pwrites=pw)

        def memset(eng, ap, val, writes=(), pw=()):
            S.op(eng, lambda e: e.memset(ap, val), writes=writes, pwrites=pw)

        def wload(dst_buf, dst_ap, src_ap, partial=False):
            if partial:
                S.dma("pool", dst_ap, src_ap, pwrites=[dst_buf])
            else:
                S.dma("pool", dst_ap, src_ap, writes=[dst_buf])

        def kview(w2d):
            return w2d.rearrange("(k p) c -> p k c", p=128)

        def xt(k, c0, c1):
            return XTt[:, k, c0:c1]

        S.dma("sp", CB.t[:, :], c_bf_d[:, :], writes=[CB])
        S.dma("sp", CF.t[:, :], c_f_d[:, :], writes=[CF])
        memset("dve", EPS.t[:, :], LN_EPS, writes=[EPS])
        memset("dve", SSMC.t[:, :, :, :], 0.0, writes=[SSMC])
        memset("dve", UPC.t[:, :, :, :], 0.0, writes=[UPC])
        memset("dve", KTC.t[:, :, :, :], 0.0, writes=[KTC])
        memset("dve", VC.t[:, :, :], 0.0, writes=[VC])
        NCD = dict(allow_slow_non_contiguous=True)
        for l in range(depth):
            S.dma("sp", BG.t[:, l, :], b_gate_d[l].rearrange("(c p) -> p c", p=128), pwrites=[BG], **NCD)
            S.dma("sp", PSC.t[:, l, :], pool_sc_d[l].rearrange("(c p) -> p c", p=128), pwrites=[PSC], **NCD)
            S.dma("sp", ESK.t[:, l, :], sinks_d[l].partition_broadcast(64), pwrites=[ESK], **NCD)
        act(ESK.t[:, :, :], ESK.t[:, :, :], AF.Exp, reads=[ESK], writes=[ESK])

        for mt in range(2):
            mf = sv(0, F32, 0, [D])
            S.dma("sp", mf.ap, mem_d[mt * 128:(mt + 1) * 128, :], writes=mf.b)
            mb = sv(1, BF16, 0, [D])
            cp("act", mb.ap, mf.ap, reads=mf.b, writes=mb.b)
            p = ps_next()
            pb = p.t[:, :].bitcast(BF16)
            for kc in range(8):
                S.op("pe", lambda e, kc=kc, pb=pb, mb=mb: e.transpose(pb[:, kc * 128:(kc + 1) * 128], mb.ap[:, kc * 128:(kc + 1) * 128], ident),
                     reads=mb.b + [CB] if kc == 0 else (), writes=[p] if kc == 0 else (), inc=(kc == 7))
            cp("dve", MEMT.t[:, :, mt * 128:(mt + 1) * 128], pb.rearrange("p (k t) -> p k t", k=8), reads=[p], pw=[MEMT])

        def ssm_prologue(l):
            PAv = sv(0, F32, 0, [2048])
            PA = PAv.b
            pa = PAv.ap

            def pv(i):
                return pa[:, i * 16:(i + 1) * 16]
            are, aim, dtv, th, rr, cs, sn, tA, fre, fim, nre, inv, tB = [pv(i) for i in range(13)]
            tI = pa[:, 13 * 16:14 * 16].bitcast(I32)
            dcol = pa[:, 14 * 16:14 * 16 + 4]
            S.dma("sp", are, a_re_d[l].rearrange("(q g) n -> (g n) q", g=2), writes=PA, **NCD)
            S.dma("sp", aim, a_im_d[l].rearrange("(q g) n -> (g n) q", g=2), pwrites=PA, **NCD)
            for g2 in range(2):
                S.dma("sp", dtv[g2 * 64:(g2 + 1) * 64, :],
                      ldt_d[l].rearrange("(q g) -> g q", g=2)[g2].partition_broadcast(64), pwrites=PA, **NCD)
            S.dma("sp", dcol, ssm_d_d[l].rearrange("(j p) -> p j", p=128), pwrites=PA, **NCD)

            def d(fn_out, *a, **k):
                pass
            act(dtv, dtv, AF.Exp, reads=PA, writes=PA)
            tt("dve", rr, dtv, are, ALU.mult, reads=PA, writes=PA)
            act(rr, rr, AF.Exp, reads=PA, writes=PA)
            cp("dve", SSMR.t[:, l, :], rr, reads=PA, pw=[SSMR])
            tt("dve", th, dtv, aim, ALU.mult, reads=PA, writes=PA)
            for dst, shift in ((sn, 0.0), (cs, 0.25)):
                ts("dve", tA, th, 1.0 / TWO_PI, shift, ALU.mult, ALU.add, reads=PA, writes=PA)
                cp("dve", tI, tA, reads=PA, writes=PA)
                cp("dve", dst, tI, reads=PA, writes=PA)
                tt("dve", tA, tA, dst, ALU.subtract, reads=PA, writes=PA)
                act(dst, tA, AF.Sin, reads=PA, writes=PA, scale=TWO_PI)
            tt("dve", nre, rr, cs, ALU.mult, reads=PA, writes=PA)
            ts("dve", nre, nre, -1.0, None, ALU.add, None, reads=PA, writes=PA)
            tt("dve", tA, rr, sn, ALU.mult, reads=PA, writes=PA)
            tt("dve", inv, are, are, ALU.mult, reads=PA, writes=PA)
            tt("dve", tB, aim, aim, ALU.mult, reads=PA, writes=PA)
            tt("dve", inv, inv, tB, ALU.add, reads=PA, writes=PA)
            S.op("dve", lambda e: e.reciprocal(inv, inv), reads=PA, writes=PA)
            tt("dve", fre, nre, are, ALU.mult, reads=PA, writes=PA)
            tt("dve", tB, tA, aim, ALU.mult, reads=PA, writes=PA)
            tt("dve", fre, fre, tB, ALU.add, reads=PA, writes=PA)
            tt("dve", fre, fre, inv, ALU.mult, reads=PA, writes=PA)
            tt("dve", fim, tA, are, ALU.mult, reads=PA, writes=PA)
            tt("dve", tB, nre, aim, ALU.mult, reads=PA, writes=PA)
            tt("dve", fim, fim, tB, ALU.subtract, reads=PA, writes=PA)
            tt("dve", fim, fim, inv, ALU.mult, reads=PA, writes=PA)
            STG = [sv(6, BF16, 0, [8, 4, 128]), sv(7, BF16, 0, [8, 4, 128])]
            for s_ in STG:
                memset("dve", s_.ap, 0.0, writes=s_.b)
            bre = sv(1, F32, 0, [16, 16]); bim = sv(1, F32, 1024, [16, 16])
            bbr = sv(2, F32, 0, [16, 16]); bbi = sv(2, F32, 1024, [16, 16]); btm = sv(2, F32, 2048, [16, 16])
            S.dma("sp", bre.ap, b_re_d[l].rearrange("(q g) n p -> (g n) q p", g=2), writes=bre.b)
            S.dma("sp", bim.ap, b_im_d[l].rearrange("(q g) n p -> (g n) q p", g=2), pwrites=bim.b)
            freb = fre.unsqueeze(2).to_broadcast([128, 16, 16])
            fimb = fim.unsqueeze(2).to_broadcast([128, 16, 16])
            R_ = PA + bre.b
            BBb = [SLq[2][0], SLq[2][1]]
            tt("dve", bbr.ap, bre.ap, freb, ALU.mult, reads=R_, writes=BBb)
            tt("dve", btm.ap, bim.ap, fimb, ALU.mult, reads=R_, writes=BBb)
            tt("dve", bbr.ap, bbr.ap, btm.ap, ALU.subtract, reads=BBb, writes=BBb)
            tt("dve", bbi.ap, bim.ap, freb, ALU.mult, reads=R_, writes=BBb)
            tt("dve", btm.ap, bre.ap, fimb, ALU.mult, reads=R_, writes=BBb)
            tt("dve", bbi.ap, bbi.ap, btm.ap, ALU.add, reads=BBb, writes=BBb)
            SRC = sv(3, BF16, 0, [128])
            for comp, bsrc in ((0, bbr), (1, bbi)):
                for j in range(4):
                    memset("dve", SRC.ap, 0.0, writes=SRC.b)
                    srcv = SRC.ap.rearrange("p (qq g p2) -> p qq g p2", qq=4, g=2)
                    for g2 in range(2):
                        cp("dve", srcv[g2 * 64:(g2 + 1) * 64, :, g2, :], bsrc.ap[g2 * 64:(g2 + 1) * 64, 4 * j:4 * j + 4, :],
                           reads=BBb, pw=SRC.b)
                    p = ps_next()
                    pb = p.t[:, :].bitcast(BF16)
                    S.op("pe", lambda e, pb=pb: e.transpose(pb[:, 0:128], SRC.ap, ident), reads=SRC.b + [CB], writes=[p])
                    for qq in range(4):
                        q = 4 * j + qq
                        s_ = STG[q // 8]
                        ts("dve", s_.ap[:, q % 8, comp, :], pb[:, 0:128], ROWM[:, qq:qq + 1], None, ALU.mult, None,
                           reads=[p, CF], pw=s_.b)
            cn = sv(1, F32, 0, [4, 64])
            for comp, csrc_d, sgn in ((2, c_re_d, 1.0), (3, c_im_d, -1.0)):
                S.dma("sp", cn.ap, csrc_d[l].rearrange("(j g) p n -> (g p) j n", g=8), writes=cn.b)
                for j in range(4):
                    ts("dve", SRC.ap[:, 0:64], cn.ap[:, j, :], EVENM, sgn, ALU.mult, ALU.mult, reads=cn.b + [CF], writes=SRC.b)
                    ts("dve", SRC.ap[:, 64:128], cn.ap[:, j, :], ODDM, sgn, ALU.mult, ALU.mult, reads=cn.b + [CF], pw=SRC.b)
                    p = ps_next()
                    pb = p.t[:, :].bitcast(BF16)
                    S.op("pe", lambda e, pb=pb: e.transpose(pb[:, 0:128], SRC.ap, ident), reads=SRC.b + [CB], writes=[p])
                    for qq in range(4):
                        q = 4 * j + qq
                        s_ = STG[q // 8]
                        cp("dve", s_.ap[:, q % 8, comp, qq * 32:(qq + 1) * 32], pb[:, qq * 32:(qq + 1) * 32], reads=[p], pw=s_.b)
            for hf in range(2):
                S.dma("sp", ssmw_d[l, hf * 8:(hf + 1) * 8].rearrange("q p c m -> p q c m"), STG[hf].ap, reads=STG[hf].b, pwrites=[B_ssmw])
            dk = sv(3, BF16, 2048, [4, 128])
            for j in range(4):
                ts("dve", dk.ap[:, j, :], ident, dcol[:, j:j + 1], None, ALU.mult, None, reads=[CB] + PA, pw=dk.b)
            S.dma("sp", ssmd_d[l], dk.ap, reads=dk.b, pwrites=[B_ssmd])
            for qb in range(8):
                par = qb % 2
                tr = sv(8, F32, 0, [2, G]) if par == 0 else sv(4, F32, 0, [2, G])
                ti = sv(9, F32, 0, [2, G]) if par == 0 else sv(5, F32, 0, [2, G])
                tm1 = sv(10, F32, 0, [2, 512])
                tm2 = sv(10, F32, 4096, [2, 512])
                q0 = 2 * qb
                cp("dve", tr.ap[:, :, 0:1], cs[:, q0:q0 + 2].unsqueeze(2), reads=PA, writes=tr.b)
                cp("dve", ti.ap[:, :, 0:1], sn[:, q0:q0 + 2].unsqueeze(2), reads=PA, writes=ti.b)
                m = 1
                while m < G:
                    cm = tr.ap[:, :, m - 1:m].to_broadcast([128, 2, m])
                    sm = ti.ap[:, :, m - 1:m].to_broadcast([128, 2, m])
                    a_ = tm1.ap[:, :, 0:m]
                    b_ = tm2.ap[:, :, 0:m]
                    RB = tr.b + ti.b
                    tt("dve", a_, tr.ap[:, :, 0:m], cm, ALU.mult, reads=RB, writes=tm1.b)
                    tt("dve", b_, ti.ap[:, :, 0:m], sm, ALU.mult, reads=RB, writes=tm2.b)
                    tt("dve", tr.ap[:, :, m:2 * m], a_, b_, ALU.subtract, reads=tm1.b + tm2.b, writes=tr.b)
                    tt("dve", a_, tr.ap[:, :, 0:m], sm, ALU.mult, reads=RB, writes=tm1.b)
                    tt("dve", b_, ti.ap[:, :, 0:m], cm, ALU.mult, reads=RB, writes=tm2.b)
                    tt("dve", ti.ap[:, :, m:2 * m], a_, b_, ALU.add, reads=tm1.b + tm2.b, writes=ti.b)
                    m *= 2
                for qq in range(2):
                    S.dma("sp", ssmt_d[l, q0 + qq, :, 0, :], tr.ap[:, qq, :], reads=tr.b, pwrites=[B_ssmt])
                    S.dma("sp", ssmt_d[l, q0 + qq, :, 1, :], ti.ap[:, qq, :], reads=ti.b, pwrites=[B_ssmt])

        if phases is None or "pro" in phases:
            for l in range(depth):
                ssm_prologue(l)

        def dbg_dump(name, ap, bufs):
            if dbg and name in dbg_d:
                S.dma("sp", dbg_d[name], ap, reads=bufs, pwrites=[B_dbg])

        def make_xt(t):
            xb = sv(9, BF16, (t % 4) * 2048, [D])
            cp("act", xb.ap, X[t].t[:, :], reads=[X[t]], writes=xb.b)
            p = ps_next()
            pb = p.t[:, :].bitcast(BF16)
            for kc in range(8):
                S.op("pe", lambda e, kc=kc, pb=pb, xb=xb: e.transpose(pb[:, kc * 128:(kc + 1) * 128], xb.ap[:, kc * 128:(kc + 1) * 128], ident),
                     reads=xb.b + [CB] if kc == 0 else (), writes=[p] if kc == 0 else (), inc=(kc == 7))
            cp("act", XTt[:, :, t * 128:(t + 1) * 128], pb.rearrange("p (k t) -> p k t", k=8), reads=[p], pw=[XTb[t // 4]])

        def layernorm(li, l, g, last):
            S.dma("sp", LNG.t[:, 0, :], ln_g_d[li][l].partition_broadcast(128), writes=[LNG])
            S.dma("sp", LNG.t[:, 1, :], ln_b_d[li][l].partition_broadcast(128), pwrites=[LNG])
            smv = SM.t[:, 0:48].rearrange("p (a b c) -> p a b c", a=4, b=2)
            mv = SM2.t[:, 0:8].rearrange("p (a b) -> p a b", a=4)
            sd = SM2.t[:, 8:12]
            rs = SM2.t[:, 12:16]
            for hf in range(2):
                for t4 in range(4):
                    t = hf * 4 + t4
                    for c in range(2):
                        S.op("dve", lambda e, t=t, t4=t4, c=c: e.bn_stats(smv[:, t4, c, :], X[t].t[:, c * 512:(c + 1) * 512]),
                             reads=[X[t]], writes=[SM] if (t4 == 0 and c == 0) else (), pwrites=() if (t4 == 0 and c == 0) else [SM])
                    S.op("dve", lambda e, t4=t4: e.bn_aggr(mv[:, t4, :], smv[:, t4, :, :]), reads=[SM], writes=[SM2] if t4 == 0 else (), pwrites=() if t4 == 0 else [SM2])
                act(sd, mv[:, :, 1], AF.Sqrt, reads=[SM2, EPS], pw=[SM2], bias=EPS.t[:, 0:1])
                S.op("dve", lambda e: e.reciprocal(rs, sd), reads=[SM2], pwrites=[SM2])
                for t4 in range(4):
                    t = hf * 4 + t4
                    xa = X[t].t[:, :]
                    stt(xa, xa, mv[:, t4, 0:1], LNG.t[:, 0, :], ALU.subtract, ALU.mult, reads=[X[t], SM2, LNG], writes=[X[t]])
                    stt(xa, xa, rs[:, t4:t4 + 1], LNG.t[:, 1, :], ALU.mult, ALU.add, reads=[X[t], SM2, LNG], writes=[X[t]])
                    if last:
                        S.dma("sp", out_d[g * G + t * 128: g * G + (t + 1) * 128, :], xa, reads=[X[t]], pwrites=[B_out])
                    else:
                        make_xt(t)
                    if dbg and g == 0 and l == 0:
                        S.dma("sp", dbg_d[f"d_x{li + 1}"][t * 128:(t + 1) * 128, :], xa, reads=[X[t]], pwrites=[B_dbg])

        def out_proj(src_of_k, src_bufs, w_d2, l, li, g, last, filler=None):
            WU = []
            for hf in range(2):
                r = ring_next()
                wload(r, rv(r, [8, 512]), kview(w_d2)[:, :, hf * 512:(hf + 1) * 512])
                WU.append(r)
            for t in range(NT):
                for hf in range(2):
                    p = ps_next()
                    w = rv(WU[hf], [8, 512])
                    S.mm(p, p.t[:, :], [(src_of_k(k)[:, t * 128:(t + 1) * 128], w[:, k, :]) for k in range(8)],
                         reads=src_bufs + [WU[hf]])
                    xa = X[t].t[:, hf * 512:(hf + 1) * 512]
                    stt(xa, xa, ALPHA, p.t[:, :], ALU.mult, ALU.add, reads=[X[t], p], pw=[X[t]])
            if filler is not None:
                filler()
            layernorm(li, l, g, last)

        def phase_attn(g, l):
            Wq = ring_next()
            wq = rv(Wq, [8, 512])
            wload(Wq, wq, kview(w_in_d[l])[:, :, C_Q:C_Q + 512])
            Wkv = ring_next()
            wkv = rv(Wkv, [8, 256])
            wload(Wkv, wkv, kview(w_in_d[l])[:, :, C_K:C_K + 256])
            rope = sv(5, F32, 0, [2, G])
            S.dma("sp", rope.ap[0:32], c_rope_d[:, :, g * G:(g + 1) * G], writes=rope.b)
            wrot = sv(6, BF16, 0, [8, 10, 32])
            memset("dve", wrot.ap, 0.0, writes=wrot.b)
            wq4 = wq.rearrange("p k (h d) -> p k h d", h=8)
            wk4 = wkv[:, :, 0:128].rearrange("p k (h d) -> p k h d", h=2)
            cp("dve", wrot.ap[:, :, 0:8, 0:8], wq4[:, :, :, 8:16], reads=[Wq], pw=wrot.b)
            cp("dve", wrot.ap[:, :, 0:8, 8:16], wq4[:, :, :, 0:8], reads=[Wq], pw=wrot.b)
            cp("dve", wrot.ap[:, :, 8:10, 0:8], wk4[:, :, :, 8:16], reads=[Wkv], pw=wrot.b)
            cp("dve", wrot.ap[:, :, 8:10, 8:16], wk4[:, :, :, 0:8], reads=[Wkv], pw=wrot.b)
            QT = [sv(0, BF16, 0, [4, G]), sv(1, BF16, 0, [4, G])]
            AOT = [sv(2, BF16, 0, [4, G]), sv(3, BF16, 0, [4, G])]
            KT = sv(4, BF16, 0, [2, 1152])
            Vv = sv(4, BF16, 4608, [9, 128])
            cp("dve", KT.ap[0:64, :, 0:128], KTC.t[:, l, :, :], reads=[KTC], writes=KT.b)
            cp("dve", Vv.ap[:, 0, :], VC.t[:, l, :], reads=[VC], pw=Vv.b)
            for h in range(10):
                for s_ in range(2):
                    pq = ps_next()
                    pr = ps_next()
                    if h < 8:
                        lq = [wq[:, k, h * 64:(h + 1) * 64] for k in range(8)]
                        wb = Wq
                    else:
                        lq = [wkv[:, k, (h - 8) * 64:(h - 7) * 64] for k in range(8)]
                        wb = Wkv
                    S.mm(pq, pq.t[0:64, :], [(lq[k], xt(k, s_ * 512, (s_ + 1) * 512)) for k in range(8)], reads=[wb, XTb[s_]])
                    S.mm(pr, pr.t[0:32, :], [(wrot.ap[:, k, h, :], xt(k, s_ * 512, (s_ + 1) * 512)) for k in range(8)],
                         reads=wrot.b + [XTb[s_]])
                    par = (h * 2 + s_) % 2
                    T1 = sv(7, F32, par * 4096, [512])
                    T2 = sv(7, F32, par * 4096 + 2048, [512])
                    cs_ = rope.ap[0:32, 0, s_ * 512:(s_ + 1) * 512]
                    sn_ = rope.ap[0:32, 1, s_ * 512:(s_ + 1) * 512]
                    tt("dve", T1.ap[0:32], pq.t[0:32, :], cs_, ALU.mult, reads=[pq] + rope.b, writes=T1.b)
                    tt("dve", T2.ap[0:32], pr.t[0:32, :], sn_, ALU.mult, reads=[pr] + rope.b, writes=T2.b)
                    if h < 8:
                        dst = QT[h // 4].ap[:, h % 4, s_ * 512:(s_ + 1) * 512]
                        db = [QT[h // 4].b[h % 4]]
                    else:
                        dst = KT.ap[:, h - 8, 128 + s_ * 512:128 + (s_ + 1) * 512]
                        db = KT.b
                    tt("dve", dst[0:32], T1.ap[0:32], T2.ap[0:32], ALU.add, reads=T1.b + T2.b, pw=db)
                    cp("act", dst[32:64], pq.t[32:64, :], reads=[pq], pw=db)
            for t in range(NT):
                pv_ = ps_next()
                S.mm(pv_, pv_.t[:, 0:128], [(xt(k, t * 128, (t + 1) * 128), wkv[:, k, 128:256]) for k in range(8)],
                     reads=[XTb[t // 4], Wkv])
                cp("act", Vv.ap[:, 1 + t, :], pv_.t[:, 0:128], reads=[pv_], pw=Vv.b)
            cp("dve", KTC.t[:, l, :, :], KT.ap[0:64, :, G:G + 128], reads=KT.b, writes=[KTC])
            cp("dve", VC.t[:, l, :], Vv.ap[:, NT, :], reads=Vv.b, writes=[VC])
            it = 0
            for i in range(NT):
                for kv in range(2):
                    has_prev = not (g == 0 and i == 0)
                    qr = QT[kv].ap[0:64, :, i * 128:(i + 1) * 128]
                    PTc = sv(8, BF16, (it % 2) * 2048, [512])
                    PTp = sv(8, BF16, (it % 2) * 2048 + 1024, [512])
                    d1 = sv(8, F32, 4096 + (it % 2) * 2048, [512])
                    it += 1
                    pc = ps_next()
                    o3 = pc.t[:, :].rearrange("p (h q) -> p h q", h=4)
                    S.mm(pc, o3, [(KT.ap[0:64, kv, 128 + i * 128:128 + (i + 1) * 128], qr), (ident, mcur)],
                         reads=KT.b + QT[kv].b + [CB])
                    act(PTc.ap, pc.t[:, :], AF.Exp, reads=[pc], writes=PTc.b, scale=0.125)
                    if has_prev:
                        pp = ps_next()
                        o3p = pp.t[:, :].rearrange("p (h q) -> p h q", h=4)
                        S.mm(pp, o3p, [(KT.ap[0:64, kv, i * 128:(i + 1) * 128], qr), (ident, mprev)],
                             reads=KT.b + QT[kv].b + [CB])
                        act(PTp.ap, pp.t[:, :], AF.Exp, reads=[pp], pw=PTp.b, scale=0.125)
                    po = ps_next()
                    pd = ps_next()
                    tv = [(Vv.ap[:, 1 + i, kv * 64:(kv + 1) * 64], PTc.ap)]
                    td = [(ones[:, 0:64], PTc.ap)]
                    if has_prev:
                        tv.append((Vv.ap[:, i, kv * 64:(kv + 1) * 64], PTp.ap))
                        td.append((ones[:, 0:64], PTp.ap))
                    S.mm(po, po.t[0:64, :], tv, reads=Vv.b + PTc.b)
                    S.mm(pd, pd.t[0:64, :], td, reads=PTc.b + [CB])
                    esk = ESK.t[:, l, 4 * kv:4 * kv + 4].unsqueeze(2).to_broadcast([64, 4, 128])
                    d13 = d1.ap[0:64].rearrange("p (h q) -> p h q", h=4)
                    tt("dve", d13, pd.t[0:64, :].rearrange("p (h q) -> p h q", h=4), esk, ALU.add, reads=[pd, ESK], writes=d1.b)
                    S.op("dve", lambda e, d13=d13: e.reciprocal(d13, d13), reads=d1.b, writes=d1.b)
                    tt("dve", AOT[kv].ap[0:64, :, i * 128:(i + 1) * 128], po.t[0:64, :].rearrange("p (h q) -> p h q", h=4), d13,
                       ALU.mult, reads=[po] + d1.b, pw=AOT[kv].b)
            if g == 0 and l == 0:
                dbg_dump("d_aot0", SLt[2][:, :], AOT[0].b)
                dbg_dump("d_aot1", SLt[3][:, :], AOT[1].b)
            return AOT

        def phase_ssm(g, l):
            Wss = ring_next()
            wss = rv(Wss, [8, 512])
            wload(Wss, wss, kview(w_in_d[l])[:, :, C_SSM:C_SSM + 512])
            UT = sv(0, BF16, 0, [4, G])
            YG = UT
            GLUT = sv(10, BF16, 0, [4, G])
            dsk = sv(6, BF16, 4096, [4, 128])
            S.dma("sp", dsk.ap, ssmd_d[l], reads=[B_ssmd], writes=dsk.b)
            for j in range(4):
                for s_ in range(2):
                    p = ps_next()
                    S.mm(p, p.t[:, :], [(wss[:, k, j * 128:(j + 1) * 128], xt(k, s_ * 512, (s_ + 1) * 512)) for k in range(8)],
                         reads=[Wss, XTb[s_]])
                    cp("act", UT.ap[:, j, s_ * 512:(s_ + 1) * 512], p.t[:, :], reads=[p], pw=[UT.b[j]])
            t1 = sv(7, F32, 0, [G]); t2 = sv(7, F32, 4096, [G])
            t3 = sv(8, F32, 0, [G]); t4 = sv(8, F32, 4096, [G])
            gre = sv(9, F32, 0, [G]); gim = sv(9, F32, 4096, [G])
            pr = [sv(1, BF16, i * 2048, [G]) for i in range(4)]
            BRb = [PS[4], PS[5]]
            BIb = [PS[6], PS[7]]
            bre_ps = PSD[2][:, :]
            bim_ps = PSD[3][:, :]
            for j in range(4):
                yp = [PS[(j % 2) * 2], PS[(j % 2) * 2 + 1]]
                for s_ in range(2):
                    S.op("pe", lambda e, j=j, s_=s_, yp=yp: e.matmul(yp[s_].t[:, :], dsk.ap[:, j, :], UT.ap[:, j, s_ * 512:(s_ + 1) * 512], start=True, stop=False),
                         reads=dsk.b + [UT.b[j]], writes=[yp[s_]], inc=False)
                for qq in range(4):
                    q = 4 * j + qq
                    tb = sv(4 + q % 2, F32, 0, [2, G])
                    tbw = sv(6, BF16, (q % 2) * 2048, [4, 128])
                    S.dma("sp", tb.ap, ssmt_d[l, q], reads=[B_ssmt], writes=tb.b)
                    S.dma("sp", tbw.ap, ssmw_d[l, q], reads=[B_ssmw], writes=tbw.b)
                    rb = SSMR.t[:, l, q:q + 1].to_broadcast([128, G])
                    for s_ in range(2):
                        c0, c1 = s_ * 512, (s_ + 1) * 512
                        S.mm(BRb[s_], BRb[s_].t[:, :], [(tbw.ap[:, 0, :], UT.ap[:, j, c0:c1])], reads=tbw.b + [UT.b[j]])
                        S.mm(BIb[s_], BIb[s_].t[:, :], [(tbw.ap[:, 1, :], UT.ap[:, j, c0:c1])], reads=tbw.b + [UT.b[j]])
                    Tc = tb.ap[:, 0, :]
                    Ts = tb.ap[:, 1, :]
                    tt("dve", t1.ap, bre_ps, Tc, ALU.mult, reads=BRb + tb.b, writes=t1.b)
                    tt("dve", t2.ap, bim_ps, Ts, ALU.mult, reads=BIb + tb.b, writes=t2.b)
                    tt("dve", t3.ap, bim_ps, Tc, ALU.mult, reads=BIb + tb.b, writes=t3.b)
                    tt("dve", t4.ap, bre_ps, Ts, ALU.mult, reads=BRb + tb.b, writes=t4.b)
                    tt("dve", t1.ap, t1.ap, t2.ap, ALU.add, reads=t1.b + t2.b, writes=t1.b)
                    tt("dve", t3.ap, t3.ap, t4.ap, ALU.subtract, reads=t3.b + t4.b, writes=t3.b)
                    S.op("dve", lambda e, rb=rb, q=q: e.tensor_tensor_scan(gre.ap, rb, t1.ap, SSMC.t[:, l, q, 0:1], ALU.mult, ALU.add),
                         reads=t1.b + [SSMR, SSMC], writes=gre.b)
                    S.op("dve", lambda e, rb=rb, q=q: e.tensor_tensor_scan(gim.ap, rb, t3.ap, SSMC.t[:, l, q, 1:2], ALU.mult, ALU.add),
                         reads=t3.b + [SSMR, SSMC], writes=gim.b)
                    GB = gre.b + gim.b
                    tt("dve", pr[0].ap, gre.ap, Tc, ALU.mult, reads=GB + tb.b, writes=pr[0].b)
                    stt(pr[1].ap, gim.ap, -1.0, Ts, ALU.mult, ALU.mult, reads=GB + tb.b, writes=pr[1].b)
                    tt("dve", pr[2].ap, gre.ap, Ts, ALU.mult, reads=GB + tb.b, writes=pr[2].b)
                    tt("dve", pr[3].ap, gim.ap, Tc, ALU.mult, reads=GB + tb.b, writes=pr[3].b)
                    lastq = qq == 3
                    for s_ in range(2):
                        c0, c1 = s_ * 512, (s_ + 1) * 512
                        for i in range(4):
                            fin = lastq and i == 3
                            S.op("pe", lambda e, s_=s_, yp=yp, tbw=tbw, i=i, c0=c0, c1=c1, fin=fin:
                                 e.matmul(yp[s_].t[:, :], tbw.ap[:, 2 + i // 2, :], pr[i].ap[:, c0:c1], start=False, stop=fin),
                                 reads=tbw.b + pr[i].b, pwrites=[yp[s_]], inc=(i == 3))
                    cz = SM2.t[:, 16:17]
                    Tcl = tb.ap[:, 0, G - 1:G]
                    Tsl = tb.ap[:, 1, G - 1:G]
                    GBt = GB + tb.b
                    ts("dve", cz, gim.ap[:, G - 1:G], Tsl, None, ALU.mult, None, reads=GBt, pw=[SM2])
                    stt(SSMC.t[:, l, q, 0:1], gre.ap[:, G - 1:G], Tcl, cz, ALU.mult, ALU.subtract, reads=GBt + [SM2], pw=[SSMC])
                    ts("dve", cz, gim.ap[:, G - 1:G], Tcl, None, ALU.mult, None, reads=GBt, pw=[SM2])
                    stt(SSMC.t[:, l, q, 1:2], gre.ap[:, G - 1:G], Tsl, cz, ALU.mult, ALU.add, reads=GBt + [SM2], pw=[SSMC])
                for s_ in range(2):
                    act(YG.ap[:, j, s_ * 512:(s_ + 1) * 512], yp[s_].t[:, :], AF.Gelu_apprx_tanh, reads=[yp[s_]], pw=[YG.b[j]])
            Wgl = ring_next()
            wgl = rv(Wgl, [4, 1024])
            wload(Wgl, wgl, kview(w_glu_d[l]))
            for c in range(4):
                for s_ in range(2):
                    phv = ps_next()
                    phg = ps_next()
                    S.mm(phv, phv.t[:, :], [(wgl[:, k, c * 128:(c + 1) * 128], YG.ap[:, k, s_ * 512:(s_ + 1) * 512]) for k in range(4)],
                         reads=[Wgl] + YG.b)
                    S.mm(phg, phg.t[:, :], [(wgl[:, k, 512 + c * 128:512 + (c + 1) * 128], YG.ap[:, k, s_ * 512:(s_ + 1) * 512]) for k in range(4)],
                         reads=[Wgl] + YG.b)
                    sg = sv(7, F32, ((c * 2 + s_) % 2) * 2048, [512])
                    act(sg.ap, phg.t[:, :], AF.Sigmoid, reads=[phg], writes=sg.b)
                    tt("dve", GLUT.ap[:, c, s_ * 512:(s_ + 1) * 512], phv.t[:, :], sg.ap, ALU.mult, reads=[phv] + sg.b, pw=[GLUT.b[c]])
            if g == 0 and l == 0:
                dbg_dump("d_glut", SLt[10][:, :], GLUT.b)
            return GLUT

        def phase_pool(g, l):
            Wpo = ring_next()
            wpo = rv(Wpo, [8, 512])
            wload(Wpo, wpo, kview(w_in_d[l])[:, :, C_POOL:C_POOL + 512])
            Wpw = ring_next()
            wpw = rv(Wpw, [4, 128])
            wload(Wpw, wpw, pool_w_d[l].rearrange("g c d -> c g d"))
            PL = sv(5, BF16, 0, [4, G])
            PMT = sv(6, BF16, 0, [4, G])
            L = G + 16
            for j in range(4):
                UP = sv(0, F32, 0, [L])
                SA = sv(1, F32, 0, [L])
                SB_ = sv(4, F32, 0, [L])
                cp("dve", UP.ap[:, 0:16], UPC.t[:, l, j, :], reads=[UPC], writes=UP.b)
                for s_ in range(2):
                    p = ps_next()
                    S.mm(p, p.t[:, :], [(wpo[:, k, j * 128:(j + 1) * 128], xt(k, s_ * 512, (s_ + 1) * 512)) for k in range(8)],
                         reads=[Wpo, XTb[s_]])
                    cp("act", UP.ap[:, 16 + s_ * 512:16 + (s_ + 1) * 512], p.t[:, :], reads=[p], pw=UP.b)
                cp("dve", UPC.t[:, l, j, :], UP.ap[:, G:G + 16], reads=UP.b, writes=[UPC])
                cur, nxt, other = UP, SA, SB_
                for s in range(1, j + 2):
                    sh = 1 << (s - 1)
                    lo = (1 << s) - 1
                    tt("dve", nxt.ap[:, lo:L], cur.ap[:, lo:L], cur.ap[:, lo - sh:L - sh], ALU.add, reads=cur.b, writes=nxt.b)
                    cur, nxt = nxt, (other if nxt is SA else SA)
                w = 1 << (j + 1)
                stt(PL.ap[:, j, :], cur.ap[:, 16:L], 1.0 / w, UP.ap[:, 16:L], ALU.mult, ALU.subtract, reads=cur.b + UP.b, writes=[PL.b[j]])
                if g == 0:
                    tmp = SM.t[:, 48:64]
                    tt("dve", tmp, cur.ap[:, 16:32], INVC[:, j, :], ALU.mult, reads=cur.b + [CF], pw=[SM])
                    tt("dve", PL.ap[:, j, 0:16], tmp, UP.ap[:, 16:32], ALU.subtract, reads=[SM] + UP.b, pw=[PL.b[j]])
                for s_ in range(2):
                    pm = ps_next()
                    S.mm(pm, pm.t[:, :], [(wpw[:, j, :], PL.ap[:, j, s_ * 512:(s_ + 1) * 512])], reads=[Wpw, PL.b[j]])
                    act(PMT.ap[:, j, s_ * 512:(s_ + 1) * 512], pm.t[:, :], AF.Identity, reads=[pm, PSC], pw=[PMT.b[j]],
                        scale=PSC.t[:, l, j:j + 1])
            if g == 0 and l == 0:
                dbg_dump("d_pmt", SLt[6][:, :], PMT.b)
            return PMT

        def phase_merge(g, l, AOT, GLUT, PMT):
            WA = [sv(4, BF16, 0, [4, D]), sv(5, BF16, 0, [4, D])]
            for hf in range(2):
                S.dma("pool", WA[hf].ap[0:64], w_bra_d[l].rearrange("(h p) d -> p h d", p=64)[:, hf * 4:(hf + 1) * 4, :], writes=WA[hf].b)
            WSs = sv(9, BF16, 0, [4, D])
            S.dma("pool", WSs.ap, kview(w_brs_d[l]), writes=WSs.b)
            r = [ring_next() for _ in range(4)]
            wbp = rv(r[0], [4, D])
            wload(r[0], wbp, kview(w_brp_d[l]))
            MT = [sv(7, BF16, 0, [4, G]), sv(8, BF16, 0, [4, G])]
            for hf in range(2):
                gw = []
                for i in range(3):
                    gv = rv(r[1 + i], [8, 512])
                    c0 = C_GATE + i * D + hf * 512
                    wload(r[1 + i], gv, kview(w_in_d[l])[:, :, c0:c0 + 512])
                    gw.append(gv)
                for d4 in range(4):
                    dc = hf * 4 + d4
                    for s_ in range(2):
                        c0, c1 = s_ * 512, (s_ + 1) * 512
                        pg = [ps_next() for _ in range(3)]
                        po = [ps_next() for _ in range(3)]
                        for i in range(3):
                            S.mm(pg[i], pg[i].t[:, :], [(gw[i][:, k, d4 * 128:(d4 + 1) * 128], xt(k, c0, c1)) for k in range(8)],
                                 reads=[r[1 + i], XTb[s_]])
                        S.mm(po[0], po[0].t[:, :],
                             [(WA[h // 4].ap[0:64, h % 4, dc * 128:(dc + 1) * 128], AOT[h // 4].ap[0:64, h % 4, c0:c1]) for h in range(8)],
                             reads=WA[0].b + WA[1].b + AOT[0].b + AOT[1].b)
                        S.mm(po[1], po[1].t[:, :], [(WSs.ap[:, k, dc * 128:(dc + 1) * 128], GLUT.ap[:, k, c0:c1]) for k in range(4)],
                             reads=WSs.b + GLUT.b)
                        S.mm(po[2], po[2].t[:, :], [(wbp[:, k, dc * 128:(dc + 1) * 128], PMT.ap[:, k, c0:c1]) for k in range(4)],
                             reads=[r[0]] + PMT.b)
                        sg = [sv(0, F32, i * 2048, [512]) for i in range(3)]
                        for i in range(3):
                            act(sg[i].ap, pg[i].t[:, :], AF.Sigmoid, reads=[pg[i], BG], writes=sg[i].b,
                                bias=BG.t[:, l, i * 8 + dc:i * 8 + dc + 1])
                        m1 = sv(1, F32, 0, [512])
                        m2 = sv(1, F32, 2048, [512])
                        tt("dve", m1.ap, po[0].t[:, :], sg[0].ap, ALU.mult, reads=[po[0]] + sg[0].b, writes=m1.b)
                        tt("dve", m2.ap, po[1].t[:, :], sg[1].ap, ALU.mult, reads=[po[1]] + sg[1].b, writes=m2.b)
                        tt("dve", m1.ap, m1.ap, m2.ap, ALU.add, reads=m1.b + m2.b, writes=m1.b)
                        tt("dve", m2.ap, po[2].t[:, :], sg[2].ap, ALU.mult, reads=[po[2]] + sg[2].b, writes=m2.b)
                        tt("dve", MT[hf].ap[:, d4, c0:c1], m1.ap, m2.ap, ALU.add, reads=m1.b + m2.b, pw=[MT[hf].b[d4]])
            if g == 0 and l == 0:
                dbg_dump("d_mt0", SLt[7][:, :], MT[0].b)
                dbg_dump("d_mt1", SLt[8][:, :], MT[1].b)
            return MT

        def phase_xattn_kv(g, l):
            KKT = sv(0, BF16, 0, [8, NMEM])
            VV = sv(0, BF16, 4096, [2, D])
            WK = []
            for u in range(4):
                rr_ = ring_next()
                wload(rr_, rv(rr_, [8, 512]), kview(xa_wkv_d[l])[:, :, u * 512:(u + 1) * 512])
                WK.append(rr_)
            for c in range(8):
                p = ps_next()
                w = rv(WK[c // 4], [8, 512])
                S.mm(p, p.t[:, 0:NMEM], [(w[:, k, (c % 4) * 128:(c % 4 + 1) * 128], MEMT.t[:, k, :]) for k in range(8)],
                     reads=[WK[c // 4], MEMT])
                cp("act", KKT.ap[:, c, :], p.t[:, 0:NMEM], reads=[p], pw=KKT.b)
            for mt in range(2):
                for hf in range(2):
                    p = ps_next()
                    w = rv(WK[2 + hf], [8, 512])
                    S.mm(p, p.t[:, :], [(MEMT.t[:, k, mt * 128:(mt + 1) * 128], w[:, k, :]) for k in range(8)], reads=[WK[2 + hf], MEMT])
                    cp("act", VV.ap[:, mt, hf * 512:(hf + 1) * 512], p.t[:, :], reads=[p], pw=VV.b)

        def phase_xattn(g, l):
            KKT = sv(0, BF16, 0, [8, NMEM])
            VV = sv(0, BF16, 4096, [2, D])
            QXT = [sv(1, BF16, 0, [4, G]), sv(2, BF16, 0, [4, G])]
            OXT = [sv(4, BF16, 0, [4, G]), sv(5, BF16, 0, [4, G])]
            WQ = []
            for u in range(2):
                rr_ = ring_next()
                wload(rr_, rv(rr_, [8, 512]), kview(xa_wq_d[l])[:, :, u * 512:(u + 1) * 512])
                WQ.append(rr_)
            for c in range(8):
                for s_ in range(2):
                    p = ps_next()
                    w = rv(WQ[c // 4], [8, 512])
                    S.mm(p, p.t[:, :], [(w[:, k, (c % 4) * 128:(c % 4 + 1) * 128], xt(k, s_ * 512, (s_ + 1) * 512)) for k in range(8)],
                         reads=[WQ[c // 4], XTb[s_]])
                    cp("act", QXT[c // 4].ap[:, c % 4, s_ * 512:(s_ + 1) * 512], p.t[:, :], reads=[p], pw=[QXT[c // 4].b[c % 4]])
            it = 0
            for h in range(4):
                for s_ in range(2):
                    c0, c1 = s_ * 512, (s_ + 1) * 512
                    PT = [sv(3, BF16, (it % 2) * 2048 + mt * 1024, [512]) for mt in range(2)]
                    rden = sv(3, F32, 4096 + (it % 2) * 2048, [512])
                    it += 1
                    for mt in range(2):
                        p = ps_next()
                        S.mm(p, p.t[:, :],
                             [(KKT.ap[:, 2 * h + dcc, mt * 128:(mt + 1) * 128], QXT[(2 * h + dcc) // 4].ap[:, (2 * h + dcc) % 4, c0:c1]) for dcc in range(2)],
                             reads=KKT.b + QXT[(2 * h) // 4].b)
                        if mt == 0:
                            act(PT[mt].ap, p.t[:, :], AF.Exp, reads=[p], writes=PT[mt].b, scale=1.0 / 16.0)
                        else:
                            act(PT[mt].ap, p.t[:, :], AF.Exp, reads=[p], pw=PT[mt].b, scale=1.0 / 16.0)
                    pd = ps_next()
                    S.mm(pd, pd.t[:, :], [(ones, PT[mt].ap) for mt in range(2)], reads=PT[0].b + [CB])
                    S.op("dve", lambda e, rden=rden, pd=pd: e.reciprocal(rden.ap, pd.t[:, :]), reads=[pd], writes=rden.b)
                    for dcc in range(2):
                        po = ps_next()
                        c = 2 * h + dcc
                        S.mm(po, po.t[:, :], [(VV.ap[:, mt, c * 128:(c + 1) * 128], PT[mt].ap) for mt in range(2)], reads=VV.b + PT[0].b)
                        tt("dve", OXT[c // 4].ap[:, c % 4, c0:c1], po.t[:, :], rden.ap, ALU.mult, reads=[po] + rden.b, pw=[OXT[c // 4].b[c % 4]])
            return OXT

        def phase_ffn(g, l):
            moe = (l % 2 == 1)
            jj = l // 2
            for t in range(NT):
                act(X[t].t[:, :], X[t].t[:, :], AF.Identity, reads=[X[t]], writes=[X[t]], scale=ALPHA)
            if moe:
                Wr = ring_next()
                wr = rv(Wr, [8, NEXP])
                S.dma("pool", wr, moe_wr_d[jj].rearrange("(k p) e -> p k e", p=128), writes=[Wr], **NCD)
                S.dma("sp", BR.t[:, :], moe_br_d[jj].partition_broadcast(128), writes=[BR], **NCD)
                pl = ps_next()
                for t in range(NT):
                    S.mm(pl, pl.t[:, t * 8:(t + 1) * 8], [(xt(k, t * 128, (t + 1) * 128), wr[:, k, :]) for k in range(8)],
                         reads=[Wr, XTb[t // 4]])
                lg = RT.t[:, :, 0:8]
                eq = RT.t[:, :, 8:16]
                l2 = RT.t[:, :, 16:24]
                ex = RT.t[:, :, 24:32]
                m1 = RT.t[:, :, 32:33]
                m2 = RT.t[:, :, 33:34]
                dn = RT.t[:, :, 34:35]
                R_ = [RT]
                tt("dve", lg, pl.t[:, 0:64].rearrange("p (t e) -> p t e", t=NT), BR.t[:, :].unsqueeze(1).to_broadcast([128, NT, NEXP]),
                   ALU.add, reads=[pl, BR], writes=R_)
                S.op("dve", lambda e: e.tensor_reduce(m1, lg, AX.X, ALU.max), reads=R_, writes=R_)
                tt("dve", eq, lg, m1.to_broadcast([128, NT, NEXP]), ALU.is_equal, reads=R_, writes=R_)
                S.op("dve", lambda e: e.scalar_tensor_tensor(l2, eq, -1e30, lg, ALU.mult, ALU.add), reads=R_, writes=R_)
                S.op("dve", lambda e: e.tensor_reduce(m2, l2, AX.X, ALU.max), reads=R_, writes=R_)
                tt("dve", eq, lg, m2.to_broadcast([128, NT, NEXP]), ALU.is_ge, reads=R_, writes=R_)
                tt("dve", l2, lg, m1.to_broadcast([128, NT, NEXP]), ALU.subtract, reads=R_, writes=R_)
                act(ex, l2, AF.Exp, reads=R_, writes=R_)
                tt("dve", ex, ex, eq, ALU.mult, reads=R_, writes=R_)
                S.op("dve", lambda e: e.tensor_reduce(dn, ex, AX.X, ALU.add), reads=R_, writes=R_)
                S.op("dve", lambda e: e.reciprocal(dn, dn), reads=R_, writes=R_)
                tt("dve", CW.t[:, :, :], ex, dn.to_broadcast([128, NT, NEXP]), ALU.mult, reads=R_, writes=[CW])
                nexp, F, gu_of, dn_of = NEXP, DFFE, (lambda e_: moe_gu_d[jj, e_]), (lambda e_: moe_dn_d[jj, e_])
            else:
                nexp, F, gu_of, dn_of = 1, DFF, (lambda e_: ffn_gu_d[jj]), (lambda e_: ffn_dn_d[jj])
            nch = F // 128
            fgs = []
            c = 0
            while c < nch:
                n = min(4, nch - c)
                fgs.append((c, n))
                c += n
            fi = 0
            for e_ in range(nexp):
                gu = kview(gu_of(e_))
                dnw = dn_of(e_)
                for (c0, n) in fgs:
                    Wg = ring_next(); Wu = ring_next(); Wd = ring_next()
                    wg = rv(Wg, [8, n * 128]); wu = rv(Wu, [8, n * 128]); wd = rv(Wd, [n, D])
                    wload(Wg, wg, gu[:, :, c0 * 128:(c0 + n) * 128])
                    wload(Wu, wu, gu[:, :, F + c0 * 128:F + (c0 + n) * 128])
                    wload(Wd, wd, dnw[c0 * 128:(c0 + n) * 128, :].rearrange("(c p) d -> p c d", p=128))
                    HT = sv(fi % 2, BF16, 0, [4, G])
                    fi += 1
                    for s_ in range(2):
                        a0, a1 = s_ * 512, (s_ + 1) * 512
                        for fc in range(n):
                            pg = ps_next()
                            pu = ps_next()
                            S.mm(pg, pg.t[:, :], [(wg[:, k, fc * 128:(fc + 1) * 128], xt(k, a0, a1)) for k in range(8)], reads=[Wg, XTb[s_]])
                            S.mm(pu, pu.t[:, :], [(wu[:, k, fc * 128:(fc + 1) * 128], xt(k, a0, a1)) for k in range(8)], reads=[Wu, XTb[s_]])
                            sg = sv(2, F32, ((fc + s_ * n) % 3) * 2048, [512])
                            act(sg.ap, pg.t[:, :], AF.Silu, reads=[pg], writes=sg.b)
                            tt("dve", HT.ap[:, fc, a0:a1], pu.t[:, :], sg.ap, ALU.mult, reads=[pu] + sg.b, pw=[HT.b[fc]])
                    for s_ in range(2):
                        for t4 in range(4):
                            t = s_ * 4 + t4
                            for hf in range(2):
                                p = ps_next()
                                S.mm(p, p.t[:, :], [(HT.ap[:, fc, t * 128:(t + 1) * 128], wd[:, fc, hf * 512:(hf + 1) * 512]) for fc in range(n)],
                                     reads=HT.b[0:n] + [Wd])
                                xa = X[t].t[:, hf * 512:(hf + 1) * 512]
                                if moe:
                                    stt(xa, p.t[:, :], CW.t[:, t, e_:e_ + 1], xa, ALU.mult, ALU.add, reads=[p, CW, X[t]], pw=[X[t]])
                                else:
                                    tt("dve", xa, xa, p.t[:, :], ALU.add, reads=[p, X[t]], pw=[X[t]])

        for g in range(ng):
            for t in range(NT):
                S.dma("sp", X[t].t[:, :], x_d[g * G + t * 128:g * G + (t + 1) * 128, :], writes=[X[t]])
                make_xt(t)
            PH = phases if phases is not None else {"attn", "ssm", "pool", "merge", "wo", "xattn", "xo", "ffn"}
            for l in range(depth):
                AOT = [sv(2, BF16, 0, [4, G]), sv(3, BF16, 0, [4, G])]
                GLUT = sv(10, BF16, 0, [4, G])
                PMT = sv(6, BF16, 0, [4, G])
                MT = [sv(7, BF16, 0, [4, G]), sv(8, BF16, 0, [4, G])]
                OXT = [sv(4, BF16, 0, [4, G]), sv(5, BF16, 0, [4, G])]
                if "attn" in PH:
                    AOT = phase_attn(g, l)
                if "ssm" in PH:
                    GLUT = phase_ssm(g, l)
                if "pool" in PH:
                    PMT = phase_pool(g, l)
                if "merge" in PH:
                    MT = phase_merge(g, l, AOT, GLUT, PMT)
                if "wo" in PH:
                    out_proj(lambda k: MT[k // 4].ap[:, k % 4, :], MT[0].b + MT[1].b, w_o_d[l], l, 0, g, False,
                             filler=(lambda g=g, l=l: phase_xattn_kv(g, l)) if "xattn" in PH else None)
                if "xattn" in PH:
                    OXT = phase_xattn(g, l)
                if "xo" in PH:
                    out_proj(lambda k: OXT[k // 4].ap[:, k % 4, :], OXT[0].b + OXT[1].b, xa_wo_d[l], l, 1, g, False)
                if "ffn" in PH:
                    phase_ffn(g, l)
                layernorm(2, l, g, l == depth - 1)
        fin = [B_out] + ([B_dbg] if dbg else [])
        S.finish(fin)
        S.emit()
        build.stats = (S.ninst, S.nsem, {e: len(v) for e, v in S.ops.items()})
    return nc


def make_consts(seq):
    bf = ml_dtypes.bfloat16
    c_bf = np.zeros((128, 1280), np.float32)
    c_bf[:, 0:128] = np.eye(128)
    c_bf[:, 128:256] = 1.0
    k = np.arange(128)[:, None]
    q = np.arange(128)[None, :]
    mc = np.where(k <= q, 0.0, -30000.0)
    mp = np.where(k > q, 0.0, -30000.0)
    c_bf[:, 256:768] = np.tile(mc, (1, 4))
    c_bf[:, 768:1280] = np.tile(mp, (1, 4))
    c_f = np.zeros((128, 72), np.float32)
    for j, w in enumerate((2, 4, 8, 16)):
        c_f[:, j * 16:(j + 1) * 16] = 1.0 / np.minimum(np.arange(1, 17), w)
    p = np.arange(128)
    for qq in range(4):
        c_f[:, 64 + qq] = (p // 32 == qq)
    c_f[:, 68] = ((p // 16) % 2 == 0)
    c_f[:, 69] = ((p // 16) % 2 == 1)
    half = 8
    inv_freq = np.power(np.float32(500000.0), -np.arange(half, dtype=np.float32) / half).astype(np.float32)
    ang = np.arange(seq, dtype=np.float32)[None, :] * inv_freq[:, None]
    c_rope = np.zeros((32, 2, seq), np.float32)
    c_rope[:, 0, :] = 1.0
    c_rope[0:8, 0, :] = np.cos(ang)
    c_rope[8:16, 0, :] = np.cos(ang)
    c_rope[0:8, 1, :] = -np.sin(ang)
    c_rope[8:16, 1, :] = np.sin(ang)
    return c_bf.astype(bf), c_f, c_rope


_NC_CACHE = {}


def kernel(**inputs):
    x = np.asarray(inputs["x"], np.float32)
    B, SEQ, _ = x.shape
    ng = SEQ // G
    key = (ng, DEPTH)
    if key not in _NC_CACHE:
        _NC_CACHE[key] = build(ng, DEPTH)
    nc = _NC_CACHE[key]
    c_bf, c_f, c_rope = make_consts(SEQ)
    shared = {k: np.ascontiguousarray(np.asarray(v, np.float32)) for k, v in inputs.items() if k not in ("x", "mem")}
    shared.update(c_bf=c_bf, c_f=c_f, c_rope=c_rope)
    in_maps = []
    for b in range(B):
        m = dict(shared)
        m["x"] = np.ascontiguousarray(x[b])
        m["mem"] = np.ascontiguousarray(np.asarray(inputs["mem"], np.float32)[b])
        in_maps.append(m)
    res = run_bass_kernel_spmd(nc, in_maps, core_ids=list(range(B)))
    return np.stack([np.asarray(r["out"], np.float32) for r in res.results], axis=0)
```

```python
import math
from contextlib import ExitStack

import ml_dtypes
import numpy as np

import concourse.bass as bass
import concourse.mybir as mybir
from concourse.bass_utils import run_bass_kernel_spmd

F32 = mybir.dt.float32
BF16 = mybir.dt.bfloat16
I32 = mybir.dt.int32
AF = mybir.ActivationFunctionType
ALU = mybir.AluOpType
AX = mybir.AxisListType

D = 1024
G = 1024
NT = G // 128
NMEM = 256
DFF = 2816
DFFE = 3584
NEXP = 8
DEPTH = 4
ALPHA = (2.0 * DEPTH) ** 0.25
LN_EPS = 1e-5
IN_W = 4864
C_Q, C_K, C_V, C_SSM, C_POOL, C_GATE = 0, 512, 640, 768, 1280, 1792
SLOT_B = 8192
NSLOT = 11
NRING = 6
TWO_PI = 2.0 * math.pi


class Buf:
    def __init__(self, name, t=None):
        self.name = name
        self.t = t
        self.w = {}
        self.r = {}
        self.sem = None
        self.dcnt = 0


class Sched:
    ENG = ("pe", "act", "dve", "pool", "sp")

    def __init__(self, nc, stack, same_eng_sync=("act", "dve", "pool")):
        self.nc = nc
        self.stack = stack
        self.same = set(same_eng_sync)
        self.ops = {e: [] for e in self.ENG}
        self.sem = {e: stack.enter_context(nc.semaphore("s_" + e)) for e in self.ENG}
        self.cnt = {e: 0 for e in self.ENG}
        self.waited = {e: {} for e in self.ENG}
        self.nsem = 5
        self.ninst = 0

    @staticmethod
    def _add(deps, k, h, v):
        if k not in deps or deps[k][1] < v:
            deps[k] = (h, v)

    def _deps(self, reads, writes, pwrites):
        deps = {}
        for b in reads:
            for k, (h, v, _p) in b.w.items():
                self._add(deps, k, h, v)
        for b in writes:
            for k, (h, v, _p) in b.w.items():
                self._add(deps, k, h, v)
            for k, (h, v) in b.r.items():
                self._add(deps, k, h, v)
        for b in pwrites:
            for k, (h, v, p) in b.w.items():
                if not p:
                    self._add(deps, k, h, v)
            for k, (h, v) in b.r.items():
                self._add(deps, k, h, v)
        return deps

    def _waits(self, eng, deps):
        for k, (h, v) in deps.items():
            if k == eng and eng not in self.same:
                continue
            if self.waited[eng].get(k, 0) >= v:
                continue
            self.waited[eng][k] = v
            self.ops[eng].append(lambda e, h=h, v=v: e.wait_ge(h, v))
            self.ninst += 1

    def _reg(self, t, reads, writes, pwrites):
        k, h, v = t
        for b in reads:
            if k not in b.r or b.r[k][1] < v:
                b.r[k] = (h, v)
        for b in writes:
            b.w = {k: (h, v, False)}
            b.r = {}
        for b in pwrites:
            if b.r:
                b.w = {k: (h, v, True)}
                b.r = {}
            else:
                if k not in b.w or b.w[k][1] < v:
                    b.w[k] = (h, v, True)

    def op(self, eng, fn, reads=(), writes=(), pwrites=(), inc=True):
        self._waits(eng, self._deps(reads, writes, pwrites))
        sem = self.sem[eng]
        if inc:
            self.cnt[eng] += 1
            n = self.cnt[eng]
            self.ops[eng].append(lambda e, fn=fn, sem=sem: fn(e).then_inc(sem, 1))
        else:
            n = self.cnt[eng] + 1
            self.ops[eng].append(lambda e, fn=fn: fn(e))
        self.ninst += 1
        self._reg((eng, sem, n), reads, writes, pwrites)

    def dma(self, q, out_ap, in_ap, reads=(), writes=(), pwrites=(), **kw):
        dst = (list(writes) + list(pwrites))[0]
        if dst.sem is None:
            dst.sem = self.stack.enter_context(self.nc.semaphore("d_" + dst.name))
            self.nsem += 1
        self._waits(q, self._deps(reads, writes, pwrites))
        dst.dcnt += 16
        sem = dst.sem
        self.ops[q].append(
            lambda e, o=out_ap, i=in_ap, sem=sem, kw=kw: e.dma_start(out=o, in_=i, **kw).then_inc(sem, 16)
        )
        self.ninst += 1
        self._reg(("d_" + dst.name, sem, dst.dcnt), reads, writes, pwrites)

    def mm(self, ps, out_ap, terms, reads):
        n = len(terms)
        for i, (l, r) in enumerate(terms):
            first, last = i == 0, i == n - 1
            self.op(
                "pe",
                lambda e, l=l, r=r, first=first, last=last, o=out_ap: e.matmul(o, l, r, start=first, stop=last),
                reads=reads if first else (),
                writes=[ps] if first else (),
                inc=last,
            )

    def finish(self, bufs):
        deps = {}
        for b in bufs:
            for k, (h, v, _p) in b.w.items():
                self._add(deps, k, h, v)
        self._waits("sp", deps)

    def emit(self):
        nc = self.nc
        ops = self.ops
        with nc.Block() as block:
            @block.tensor
            def _(e):
                for f in ops["pe"]:
                    f(e)

            @block.scalar
            def _(e):
                for f in ops["act"]:
                    f(e)

            @block.vector
            def _(e):
                for f in ops["dve"]:
                    f(e)

            @block.gpsimd
            def _(e):
                for f in ops["pool"]:
                    f(e)

            @block.sync
            def _(e):
                for f in ops["sp"]:
                    f(e)


class V:
    def __init__(self, ap, b):
        self.ap = ap
        self.b = b


def build(ng, depth, dbg=False, phases=None, same=("act", "dve", "pool")):
    nc = bass.Bass("TRN2", target_bir_lowering=False)
    SEQ = ng * G
    nden = (depth + 1) // 2
    nmoe = depth // 2

    def din(name, shape, dt=F32):
        return nc.dram_tensor(name, list(shape), dt, kind="ExternalInput").ap()

    x_d = din("x", [SEQ, D])
    mem_d = din("mem", [NMEM, D])
    w_in_d = din("w_in", [depth, D, IN_W])
    b_gate_d = din("b_gate", [depth, 3 * D])
    sinks_d = din("attn_sinks", [depth, 8])
    a_re_d = din("ssm_a_re", [depth, 32, 64])
    a_im_d = din("ssm_a_im", [depth, 32, 64])
    ldt_d = din("ssm_log_dt", [depth, 32])
    b_re_d = din("ssm_b_re", [depth, 32, 64, 16])
    b_im_d = din("ssm_b_im", [depth, 32, 64, 16])
    c_re_d = din("ssm_c_re", [depth, 32, 16, 64])
    c_im_d = din("ssm_c_im", [depth, 32, 16, 64])
    ssm_d_d = din("ssm_d", [depth, 512])
    w_glu_d = din("ssm_w_glu", [depth, 512, 1024])
    pool_w_d = din("pool_w", [depth, 4, 128, 128])
    pool_sc_d = din("pool_scale", [depth, 512])
    w_bra_d = din("w_br_attn", [depth, 512, D])
    w_brs_d = din("w_br_ssm", [depth, 512, D])
    w_brp_d = din("w_br_pool", [depth, 512, D])
    w_o_d = din("w_o", [depth, D, D])
    ln_g_d = [din(f"ln{i}_g", [depth, D]) for i in (1, 2, 3)]
    ln_b_d = [din(f"ln{i}_b", [depth, D]) for i in (1, 2, 3)]
    xa_wq_d = din("xa_wq", [depth, D, D])
    xa_wkv_d = din("xa_wkv", [depth, D, 2 * D])
    xa_wo_d = din("xa_wo", [depth, D, D])
    ffn_gu_d = din("ffn_w_gu", [nden, D, 2 * DFF])
    ffn_dn_d = din("ffn_w_down", [nden, DFF, D])
    if nmoe:
        moe_wr_d = din("moe_w_router", [nmoe, D, NEXP])
        moe_br_d = din("moe_b_router", [nmoe, NEXP])
        moe_gu_d = din("moe_w_gu", [nmoe, NEXP, D, 2 * DFFE])
        moe_dn_d = din("moe_w_down", [nmoe, NEXP, DFFE, D])
    c_bf_d = din("c_bf", [128, 1280], BF16)
    c_f_d = din("c_f", [128, 72])
    c_rope_d = din("c_rope", [32, 2, SEQ])
    out_d = nc.dram_tensor("out", [SEQ, D], F32, kind="ExternalOutput").ap()
    ssmt_d = nc.dram_tensor("ssmt", [depth, 16, 128, 2, G], F32).ap()
    ssmw_d = nc.dram_tensor("ssmw", [depth, 16, 128, 4, 128], BF16).ap()
    ssmd_d = nc.dram_tensor("ssmd", [depth, 128, 4, 128], BF16).ap()
    dbg_d = {}
    if dbg:
        for nm in ("d_x1", "d_x2", "d_x3"):
            dbg_d[nm] = nc.dram_tensor(nm, [G, D], F32, kind="ExternalOutput").ap()
        for nm in ("d_aot0", "d_aot1", "d_glut", "d_pmt", "d_mt0", "d_mt1"):
            dbg_d[nm] = nc.dram_tensor(nm, [128, SLOT_B // 2], BF16, kind="ExternalOutput").ap()

    with ExitStack() as st:
        S = Sched(nc, st, same_eng_sync=same)

        def sb(name, shape, dt):
            return Buf(name, st.enter_context(nc.sbuf_tensor(name, list(shape), dt)))

        X = [sb(f"X{i}", [128, D], F32) for i in range(NT)]
        XTt = st.enter_context(nc.sbuf_tensor("XT", [128, 8, G], BF16))
        XTb = [Buf("XT0"), Buf("XT1")]
        CB = sb("CB", [128, 1280], BF16)
        CF = sb("CF", [128, 72], F32)
        MEMT = sb("MEMT", [128, 8, NMEM], BF16)
        SSMC = sb("SSMC", [128, depth, 16, 2], F32)
        KTC = sb("KTC", [64, depth, 2, 128], BF16)
        VC = sb("VC", [128, depth, 128], BF16)
        UPC = sb("UPC", [128, depth, 4, 16], F32)
        SSMR = sb("SSMR", [128, depth, 16], F32)
        ESK = sb("ESK", [64, depth, 8], F32)
        BG = sb("BG", [128, depth, 24], F32)
        PSC = sb("PSC", [128, depth, 4], F32)
        LNG = sb("LNG", [128, 2, D], F32)
        SM = sb("SM", [128, 64], F32)
        SM2 = sb("SM2", [128, 32], F32)
        CW = sb("CW", [128, NT, NEXP], F32)
        RT = sb("RT", [128, 8, 64], F32)
        BR = sb("BRT", [128, NEXP], F32)
        EPS = sb("EPS", [128, 1], F32)
        RING = [sb(f"R{i}", [128, SLOT_B // 2], BF16) for i in range(NRING)]
        SLt = [st.enter_context(nc.sbuf_tensor(f"SL{i}", [128, SLOT_B // 2], BF16)) for i in range(NSLOT)]
        SLq = [[Buf(f"SL{i}q{j}") for j in range(4)] for i in range(NSLOT)]
        PSD = [st.enter_context(nc.psum_tensor(f"psd{i}", [128, 1024], F32)) for i in range(4)]
        PS = [Buf(f"ps{i}", PSD[i // 2][:, (i % 2) * 512:(i % 2 + 1) * 512]) for i in range(8)]
        B_ssmw = Buf("ssmw")
        B_ssmt = Buf("ssmt")
        B_ssmd = Buf("ssmd")
        B_out = Buf("outb")
        B_dbg = Buf("dbgb")
        state = {"ps": 0, "ring": 0}

        def ps_next():
            p = PS[state["ps"] % 8]
            state["ps"] += 1
            return p

        def ps_sub(lo, n, key):
            i = state.get(key, 0)
            state[key] = i + 1
            return PS[lo + i % n]

        def ring_next():
            r = RING[state["ring"] % NRING]
            state["ring"] += 1
            return r

        def sv(i, dt, off_b, shape):
            n = 1
            for s_ in shape:
                n *= s_
            esz = 4 if dt in (F32, I32) else 2
            nb = n * esz
            assert off_b + nb <= SLOT_B, (i, off_b, nb)
            a = SLt[i][:, off_b // 2: (off_b + nb) // 2]
            if dt != BF16:
                a = a.bitcast(dt)
            if len(shape) == 2:
                a = a.rearrange("p (a b) -> p a b", a=shape[0])
            elif len(shape) == 3:
                a = a.rearrange("p (a b c) -> p a b c", a=shape[0], b=shape[1])
            q0, q1 = off_b // 2048, (off_b + nb - 1) // 2048
            return V(a, [SLq[i][j] for j in range(q0, q1 + 1)])

        def rv(rbuf, shape):
            n = 1
            for s_ in shape:
                n *= s_
            a = rbuf.t[:, 0:n]
            if len(shape) == 2:
                a = a.rearrange("p (a b) -> p a b", a=shape[0])
            elif len(shape) == 3:
                a = a.rearrange("p (a b c) -> p a b c", a=shape[0], b=shape[1])
            return a

        ident = CB.t[:, 0:128]
        ones = CB.t[:, 128:256]
        mcur = CB.t[:, 256:768].rearrange("p (h q) -> p h q", h=4)
        mprev = CB.t[:, 768:1280].rearrange("p (h q) -> p h q", h=4)
        INVC = CF.t[:, 0:64].rearrange("p (a b) -> p a b", a=4)
        ROWM = CF.t[:, 64:68]
        EVENM = CF.t[:, 68:69]
        ODDM = CF.t[:, 69:70]

        def act(out, in_, func, reads, writes=(), pw=(), bias=None, scale=None):
            kw = {}
            if bias is not None:
                kw["bias"] = bias
            if scale is not None:
                kw["scale"] = scale
            S.op("act", lambda e: e.activation(out, in_, func, **kw), reads=reads, writes=writes, pwrites=pw)

        def tt(eng, out, in0, in1, op, reads, writes=(), pw=()):
            S.op(eng, lambda e: e.tensor_tensor(out, in0, in1, op), reads=reads, writes=writes, pwrites=pw)

        def ts(eng, out, in0, s1, s2, op0, op1, reads, writes=(), pw=()):
            if s2 is None:
                S.op(eng, lambda e: e.tensor_scalar(out, in0, s1, None, op0), reads=reads, writes=writes, pwrites=pw)
            else:
                S.op(eng, lambda e: e.tensor_scalar(out, in0, s1, s2, op0, op1), reads=reads, writes=writes, pwrites=pw)

        def stt(out, in0, sc, in1, op0, op1, reads, writes=(), pw=()):
            S.op("dve", lambda e: e.scalar_tensor_tensor(out, in0, sc, in1, op0, op1), reads=reads, writes=writes, pwrites=pw)

        def cp(eng, out, in_, reads, writes=(), pw=()):
            if eng == "act":
                S.op("act", lambda e: e.activation(out, in_, AF.Copy), reads=reads, writes=writes, pwrites=pw)
            else:
                S.op(eng, lambda e: e.tensor_copy(out, in_), reads=reads, writes=writes, pwrites=pw)

        def memset(eng, ap, val, writes=(), pw=()):
            S.op(eng, lambda e: e.memset(ap, val), writes=writes, pwrites=pw)

        def wload(dst_buf, dst_ap, src_ap, partial=False):
            if partial:
                S.dma("pool", dst_ap, src_ap, pwrites=[dst_buf])
            else:
                S.dma("pool", dst_ap, src_ap, writes=[dst_buf])

        def kview(w2d):
            return w2d.rearrange("(k p) c -> p k c", p=128)

        def xt(k, c0, c1):
            return XTt[:, k, c0:c1]

        S.dma("sp", CB.t[:, :], c_bf_d[:, :], writes=[CB])
        S.dma("sp", CF.t[:, :], c_f_d[:, :], writes=[CF])
        memset("dve", EPS.t[:, :], LN_EPS, writes=[EPS])
        memset("dve", SSMC.t[:, :, :, :], 0.0, writes=[SSMC])
        memset("dve", UPC.t[:, :, :, :], 0.0, writes=[UPC])
        memset("dve", KTC.t[:, :, :, :], 0.0, writes=[KTC])
        memset("dve", VC.t[:, :, :], 0.0, writes=[VC])
        NCD = dict(allow_slow_non_contiguous=True)
        for l in range(depth):
            S.dma("sp", BG.t[:, l, :], b_gate_d[l].rearrange("(c p) -> p c", p=128), pwrites=[BG], **NCD)
            S.dma("sp", PSC.t[:, l, :], pool_sc_d[l].rearrange("(c p) -> p c", p=128), pwrites=[PSC], **NCD)
            S.dma("sp", ESK.t[:, l, :], sinks_d[l].partition_broadcast(64), pwrites=[ESK], **NCD)
        act(ESK.t[:, :, :], ESK.t[:, :, :], AF.Exp, reads=[ESK], writes=[ESK])

        for mt in range(2):
            mf = sv(0, F32, 0, [D])
            S.dma("sp", mf.ap, mem_d[mt * 128:(mt + 1) * 128, :], writes=mf.b)
            mb = sv(1, BF16, 0, [D])
            cp("act", mb.ap, mf.ap, reads=mf.b, writes=mb.b)
            p = ps_next()
            pb = p.t[:, :].bitcast(BF16)
            for kc in range(8):
                S.op("pe", lambda e, kc=kc, pb=pb, mb=mb: e.transpose(pb[:, kc * 128:(kc + 1) * 128], mb.ap[:, kc * 128:(kc + 1) * 128], ident),
                     reads=mb.b + [CB] if kc == 0 else (), writes=[p] if kc == 0 else (), inc=(kc == 7))
            cp("dve", MEMT.t[:, :, mt * 128:(mt + 1) * 128], pb.rearrange("p (k t) -> p k t", k=8), reads=[p], pw=[MEMT])

        def ssm_prologue(l):
            PAv = sv(0, F32, 0, [2048])
            PA = PAv.b
            pa = PAv.ap

            def pv(i):
                return pa[:, i * 16:(i + 1) * 16]
            are, aim, dtv, th, rr, cs, sn, tA, fre, fim, nre, inv, tB = [pv(i) for i in range(13)]
            tI = pa[:, 13 * 16:14 * 16].bitcast(I32)
            dcol = pa[:, 14 * 16:14 * 16 + 4]
            S.dma("sp", are, a_re_d[l].rearrange("(q g) n -> (g n) q", g=2), writes=PA, **NCD)
            S.dma("sp", aim, a_im_d[l].rearrange("(q g) n -> (g n) q", g=2), pwrites=PA, **NCD)
            for g2 in range(2):
                S.dma("sp", dtv[g2 * 64:(g2 + 1) * 64, :],
                      ldt_d[l].rearrange("(q g) -> g q", g=2)[g2].partition_broadcast(64), pwrites=PA, **NCD)
            S.dma("sp", dcol, ssm_d_d[l].rearrange("(j p) -> p j", p=128), pwrites=PA, **NCD)

            def d(fn_out, *a, **k):
                pass
            act(dtv, dtv, AF.Exp, reads=PA, writes=PA)
            tt("dve", rr, dtv, are, ALU.mult, reads=PA, writes=PA)
            act(rr, rr, AF.Exp, reads=PA, writes=PA)
            cp("dve", SSMR.t[:, l, :], rr, reads=PA, pw=[SSMR])
            tt("dve", th, dtv, aim, ALU.mult, reads=PA, writes=PA)
            for dst, shift in ((sn, 0.0), (cs, 0.25)):
                ts("dve", tA, th, 1.0 / TWO_PI, shift, ALU.mult, ALU.add, reads=PA, writes=PA)
                cp("dve", tI, tA, reads=PA, writes=PA)
                cp("dve", dst, tI, reads=PA, writes=PA)
                tt("dve", tA, tA, dst, ALU.subtract, reads=PA, writes=PA)
                act(dst, tA, AF.Sin, reads=PA, writes=PA, scale=TWO_PI)
            tt("dve", nre, rr, cs, ALU.mult, reads=PA, writes=PA)
            ts("dve", nre, nre, -1.0, None, ALU.add, None, reads=PA, writes=PA)
            tt("dve", tA, rr, sn, ALU.mult, reads=PA, writes=PA)
            tt("dve", inv, are, are, ALU.mult, reads=PA, writes=PA)
            tt("dve", tB, aim, aim, ALU.mult, reads=PA, writes=PA)
            tt("dve", inv, inv, tB, ALU.add, reads=PA, writes=PA)
            S.op("dve", lambda e: e.reciprocal(inv, inv), reads=PA, writes=PA)
            tt("dve", fre, nre, are, ALU.mult, reads=PA, writes=PA)
            tt("dve", tB, tA, aim, ALU.mult, reads=PA, writes=PA)
            tt("dve", fre, fre, tB, ALU.add, reads=PA, writes=PA)
            tt("dve", fre, fre, inv, ALU.mult, reads=PA, writes=PA)
            tt("dve", fim, tA, are, ALU.mult, reads=PA, writes=PA)
            tt("dve", tB, nre, aim, ALU.mult, reads=PA, writes=PA)
            tt("dve", fim, fim, tB, ALU.subtract, reads=PA, writes=PA)
            tt("dve", fim, fim, inv, ALU.mult, reads=PA, writes=PA)
            STG = [sv(6, BF16, 0, [8, 4, 128]), sv(7, BF16, 0, [8, 4, 128])]
            for s_ in STG:
                memset("dve", s_.ap, 0.0, writes=s_.b)
            bre = sv(1, F32, 0, [16, 16]); bim = sv(1, F32, 1024, [16, 16])
            bbr = sv(2, F32, 0, [16, 16]); bbi = sv(2, F32, 1024, [16, 16]); btm = sv(2, F32, 2048, [16, 16])
            S.dma("sp", bre.ap, b_re_d[l].rearrange("(q g) n p -> (g n) q p", g=2), writes=bre.b)
            S.dma("sp", bim.ap, b_im_d[l].rearrange("(q g) n p -> (g n) q p", g=2), pwrites=bim.b)
            freb = fre.unsqueeze(2).to_broadcast([128, 16, 16])
            fimb = fim.unsqueeze(2).to_broadcast([128, 16, 16])
            R_ = PA + bre.b
            BBb = [SLq[2][0], SLq[2][1]]
            tt("dve", bbr.ap, bre.ap, freb, ALU.mult, reads=R_, writes=BBb)
            tt("dve", btm.ap, bim.ap, fimb, ALU.mult, reads=R_, writes=BBb)
            tt("dve", bbr.ap, bbr.ap, btm.ap, ALU.subtract, reads=BBb, writes=BBb)
            tt("dve", bbi.ap, bim.ap, freb, ALU.mult, reads=R_, writes=BBb)
            tt("dve", btm.ap, bre.ap, fimb, ALU.mult, reads=R_, writes=BBb)
            tt("dve", bbi.ap, bbi.ap, btm.ap, ALU.add, reads=BBb, writes=BBb)
            SRC = sv(3, BF16, 0, [128])
            for comp, bsrc in ((0, bbr), (1, bbi)):
                for j in range(4):
                    memset("dve", SRC.ap, 0.0, writes=SRC.b)
                    srcv = SRC.ap.rearrange("p (qq g p2) -> p qq g p2", qq=4, g=2)
                    for g2 in range(2):
                        cp("dve", srcv[g2 * 64:(g2 + 1) * 64, :, g2, :], bsrc.ap[g2 * 64:(g2 + 1) * 64, 4 * j:4 * j + 4, :],
                           reads=BBb, pw=SRC.b)
                    p = ps_next()
                    pb = p.t[:, :].bitcast(BF16)
                    S.op("pe", lambda e, pb=pb: e.transpose(pb[:, 0:128], SRC.ap, ident), reads=SRC.b + [CB], writes=[p])
                    for qq in range(4):
                        q = 4 * j + qq
                        s_ = STG[q // 8]
                        ts("dve", s_.ap[:, q % 8, comp, :], pb[:, 0:128], ROWM[:, qq:qq + 1], None, ALU.mult, None,
                           reads=[p, CF], pw=s_.b)
            cn = sv(1, F32, 0, [4, 64])
            for comp, csrc_d, sgn in ((2, c_re_d, 1.0), (3, c_im_d, -1.0)):
                S.dma("sp", cn.ap, csrc_d[l].rearrange("(j g) p n -> (g p) j n", g=8), writes=cn.b)
                for j in range(4):
                    ts("dve", SRC.ap[:, 0:64], cn.ap[:, j, :], EVENM, sgn, ALU.mult, ALU.mult, reads=cn.b + [CF], writes=SRC.b)
                    ts("dve", SRC.ap[:, 64:128], cn.ap[:, j, :], ODDM, sgn, ALU.mult, ALU.mult, reads=cn.b + [CF], pw=SRC.b)
                    p = ps_next()
                    pb = p.t[:, :].bitcast(BF16)
                    S.op("pe", lambda e, pb=pb: e.transpose(pb[:, 0:128], SRC.ap, ident), reads=SRC.b + [CB], writes=[p])
                    for qq in range(4):
                        q = 4 * j + qq
                        s_ = STG[q // 8]
                        cp("dve", s_.ap[:, q % 8, comp, qq * 32:(qq + 1) * 32], pb[:, qq * 32:(qq + 1) * 32], reads=[p], pw=s_.b)
            for hf in range(2):
                S.dma("sp", ssmw_d[l, hf * 8:(hf + 1) * 8].rearrange("q p c m -> p q c m"), STG[hf].ap, reads=STG[hf].b, pwrites=[B_ssmw])
            dk = sv(3, BF16, 2048, [4, 128])
            for j in range(4):
                ts("dve", dk.ap[:, j, :], ident, dcol[:, j:j + 1], None, ALU.mult, None, reads=[CB] + PA, pw=dk.b)
            S.dma("sp", ssmd_d[l], dk.ap, reads=dk.b, pwrites=[B_ssmd])
            smallb = [RING[1]]
            sm_ = rv(RING[1], [4, 16, 32]).bitcast(F32) if False else None
            tabs = RING[1].t[:, 0:4096].bitcast(F32).rearrange("p (c q b) -> p c q b", c=4, q=16)
            tbr, tbi, tar, tai = tabs[:, 0], tabs[:, 1], tabs[:, 2], tabs[:, 3]
            tmps = RING[2].t[:, 0:1024].bitcast(F32).rearrange("p (c q b) -> p c q b", c=2, q=16)
            tmpb = [RING[2]]

            def cmul_step(dr, di, ar, ai, br, bi, n):
                ta_, tb_ = tmps[:, 0, :, 0:n], tmps[:, 1, :, 0:n]
                tt("dve", ta_, ar, br, ALU.mult, reads=smallb, writes=tmpb)
                tt("dve", tb_, ai, bi, ALU.mult, reads=smallb, writes=tmpb)
                tt("dve", dr, ta_, tb_, ALU.subtract, reads=tmpb, writes=smallb)
                tt("dve", ta_, ar, bi, ALU.mult, reads=smallb, writes=tmpb)
                tt("dve", tb_, ai, br, ALU.mult, reads=smallb, writes=tmpb)
                tt("dve", di, ta_, tb_, ALU.add, reads=tmpb, writes=smallb)

            cp("dve", tbr[:, :, 0:1], cs.unsqueeze(2), reads=PA, writes=smallb)
            cp("dve", tbi[:, :, 0:1], sn.unsqueeze(2), reads=PA, writes=smallb)
            m = 1
            while m < 32:
                cmul_step(tbr[:, :, m:2 * m], tbi[:, :, m:2 * m], tbr[:, :, 0:m], tbi[:, :, 0:m],
                          tbr[:, :, m - 1:m].to_broadcast([128, 16, m]), tbi[:, :, m - 1:m].to_broadcast([128, 16, m]), m)
                m *= 2
            memset("dve", tar[:, :, 0:1], 1.0, writes=smallb)
            memset("dve", tai[:, :, 0:1], 0.0, writes=smallb)
            cp("dve", tar[:, :, 1:2], tbr[:, :, 31:32], reads=smallb, writes=smallb)
            cp("dve", tai[:, :, 1:2], tbi[:, :, 31:32], reads=smallb, writes=smallb)
            m = 1
            while m < 31:
                n = min(m, 31 - m)
                cmul_step(tar[:, :, 1 + m:1 + m + n], tai[:, :, 1 + m:1 + m + n], tar[:, :, 1:1 + n], tai[:, :, 1:1 + n],
                          tar[:, :, m:m + 1].to_broadcast([128, 16, n]), tai[:, :, m:m + 1].to_broadcast([128, 16, n]), n)
                m *= 2
            for qb in range(8):
                par = qb % 2
                tr = sv(8, F32, 0, [2, G]) if par == 0 else sv(4, F32, 0, [2, G])
                ti = sv(9, F32, 0, [2, G]) if par == 0 else sv(5, F32, 0, [2, G])
                tm1 = sv(10, F32, 0, [2, G])
                q0 = 2 * qb
                tr4 = tr.ap.rearrange("p q (a b) -> p q a b", a=32)
                ti4 = ti.ap.rearrange("p q (a b) -> p q a b", a=32)
                tm4 = tm1.ap.rearrange("p q (a b) -> p q a b", a=32)
                Ar = tar[:, q0:q0 + 2, :].unsqueeze(3).to_broadcast([128, 2, 32, 32])
                Ai = tai[:, q0:q0 + 2, :].unsqueeze(3).to_broadcast([128, 2, 32, 32])
                Br = tbr[:, q0:q0 + 2, :].unsqueeze(2).to_broadcast([128, 2, 32, 32])
                Bi = tbi[:, q0:q0 + 2, :].unsqueeze(2).to_broadcast([128, 2, 32, 32])
                tt("dve", tr4, Ar, Br, ALU.mult, reads=smallb, writes=tr.b)
                tt("dve", tm4, Ai, Bi, ALU.mult, reads=smallb, writes=tm1.b)
                tt("dve", tr4, tr4, tm4, ALU.subtract, reads=tr.b + tm1.b, writes=tr.b)
                tt("dve", ti4, Ar, Bi, ALU.mult, reads=smallb, writes=ti.b)
                tt("dve", tm4, Ai, Br, ALU.mult, reads=smallb, writes=tm1.b)
                tt("dve", ti4, ti4, tm4, ALU.add, reads=ti.b + tm1.b, writes=ti.b)
                for qq in range(2):
                    S.dma("sp", ssmt_d[l, q0 + qq, :, 0, :], tr.ap[:, qq, :], reads=tr.b, pwrites=[B_ssmt])
                    S.dma("sp", ssmt_d[l, q0 + qq, :, 1, :], ti.ap[:, qq, :], reads=ti.b, pwrites=[B_ssmt])

        if phases is None or "pro" in phases:
            for l in range(depth):
                ssm_prologue(l)

        def dbg_dump(name, ap, bufs):
            if dbg and name in dbg_d:
                S.dma("sp", dbg_d[name], ap, reads=bufs, pwrites=[B_dbg])

        def make_xt(t):
            xb = sv(9, BF16, (t % 4) * 2048, [D])
            cp("act", xb.ap, X[t].t[:, :], reads=[X[t]], writes=xb.b)
            p = ps_next()
            pb = p.t[:, :].bitcast(BF16)
            for kc in range(8):
                S.op("pe", lambda e, kc=kc, pb=pb, xb=xb: e.transpose(pb[:, kc * 128:(kc + 1) * 128], xb.ap[:, kc * 128:(kc + 1) * 128], ident),
                     reads=xb.b + [CB] if kc == 0 else (), writes=[p] if kc == 0 else (), inc=(kc == 7))
            cp("act", XTt[:, :, t * 128:(t + 1) * 128], pb.rearrange("p (k t) -> p k t", k=8), reads=[p], pw=[XTb[t // 4]])

        def layernorm(li, l, g, last):
            S.dma("sp", LNG.t[:, 0, :], ln_g_d[li][l].partition_broadcast(128), writes=[LNG])
            S.dma("sp", LNG.t[:, 1, :], ln_b_d[li][l].partition_broadcast(128), pwrites=[LNG])
            smv = SM.t[:, 0:48].rearrange("p (a b c) -> p a b c", a=4, b=2)
            mv = SM2.t[:, 0:8].rearrange("p (a b) -> p a b", a=4)
            sd = SM2.t[:, 8:12]
            rs = SM2.t[:, 12:16]
            for hf in range(2):
                for t4 in range(4):
                    t = hf * 4 + t4
                    for c in range(2):
                        S.op("dve", lambda e, t=t, t4=t4, c=c: e.bn_stats(smv[:, t4, c, :], X[t].t[:, c * 512:(c + 1) * 512]),
                             reads=[X[t]], writes=[SM] if (t4 == 0 and c == 0) else (), pwrites=() if (t4 == 0 and c == 0) else [SM])
                    S.op("dve", lambda e, t4=t4: e.bn_aggr(mv[:, t4, :], smv[:, t4, :, :]), reads=[SM], writes=[SM2] if t4 == 0 else (), pwrites=() if t4 == 0 else [SM2])
                act(sd, mv[:, :, 1], AF.Sqrt, reads=[SM2, EPS], pw=[SM2], bias=EPS.t[:, 0:1])
                S.op("dve", lambda e: e.reciprocal(rs, sd), reads=[SM2], pwrites=[SM2])
                for t4 in range(4):
                    t = hf * 4 + t4
                    xa = X[t].t[:, :]
                    stt(xa, xa, mv[:, t4, 0:1], LNG.t[:, 0, :], ALU.subtract, ALU.mult, reads=[X[t], SM2, LNG], writes=[X[t]])
                    stt(xa, xa, rs[:, t4:t4 + 1], LNG.t[:, 1, :], ALU.mult, ALU.add, reads=[X[t], SM2, LNG], writes=[X[t]])
                    if last:
                        S.dma("sp", out_d[g * G + t * 128: g * G + (t + 1) * 128, :], xa, reads=[X[t]], pwrites=[B_out])
                    else:
                        make_xt(t)
                    if dbg and g == 0 and l == 0:
                        S.dma("sp", dbg_d[f"d_x{li + 1}"][t * 128:(t + 1) * 128, :], xa, reads=[X[t]], pwrites=[B_dbg])

        def out_proj(src_of_k, src_bufs, w_d2, l, li, g, last, filler=None):
            WU = []
            for hf in range(2):
                r = ring_next()
                wload(r, rv(r, [8, 512]), kview(w_d2)[:, :, hf * 512:(hf + 1) * 512])
                WU.append(r)
            for t in range(NT):
                for hf in range(2):
                    p = ps_next()
                    w = rv(WU[hf], [8, 512])
                    S.mm(p, p.t[:, :], [(src_of_k(k)[:, t * 128:(t + 1) * 128], w[:, k, :]) for k in range(8)],
                         reads=src_bufs + [WU[hf]])
                    xa = X[t].t[:, hf * 512:(hf + 1) * 512]
                    stt(xa, xa, ALPHA, p.t[:, :], ALU.mult, ALU.add, reads=[X[t], p], pw=[X[t]])
            if filler is not None:
                filler()
            layernorm(li, l, g, last)

        def phase_attn(g, l):
            Wq = ring_next()
            wq = rv(Wq, [8, 512])
            wload(Wq, wq, kview(w_in_d[l])[:, :, C_Q:C_Q + 512])
            Wkv = ring_next()
            wkv = rv(Wkv, [8, 256])
            wload(Wkv, wkv, kview(w_in_d[l])[:, :, C_K:C_K + 256])
            rope = sv(5, F32, 0, [2, G])
            S.dma("sp", rope.ap[0:32], c_rope_d[:, :, g * G:(g + 1) * G], writes=rope.b)
            wrot = sv(6, BF16, 0, [8, 10, 32])
            memset("dve", wrot.ap, 0.0, writes=wrot.b)
            wq4 = wq.rearrange("p k (h d) -> p k h d", h=8)
            wk4 = wkv[:, :, 0:128].rearrange("p k (h d) -> p k h d", h=2)
            cp("dve", wrot.ap[:, :, 0:8, 0:8], wq4[:, :, :, 8:16], reads=[Wq], pw=wrot.b)
            cp("dve", wrot.ap[:, :, 0:8, 8:16], wq4[:, :, :, 0:8], reads=[Wq], pw=wrot.b)
            cp("dve", wrot.ap[:, :, 8:10, 0:8], wk4[:, :, :, 8:16], reads=[Wkv], pw=wrot.b)
            cp("dve", wrot.ap[:, :, 8:10, 8:16], wk4[:, :, :, 0:8], reads=[Wkv], pw=wrot.b)
            QT = [sv(0, BF16, 0, [4, G]), sv(1, BF16, 0, [4, G])]
            AOT = [sv(2, BF16, 0, [4, G]), sv(3, BF16, 0, [4, G])]
            KT = sv(4, BF16, 0, [2, 1152])
            Vv = sv(4, BF16, 4608, [9, 128])
            cp("dve", KT.ap[0:64, :, 0:128], KTC.t[:, l, :, :], reads=[KTC], writes=KT.b)
            cp("dve", Vv.ap[:, 0, :], VC.t[:, l, :], reads=[VC], pw=Vv.b)
            for h in range(10):
                for s_ in range(2):
                    pq = ps_next()
                    pr = ps_next()
                    if h < 8:
                        lq = [wq[:, k, h * 64:(h + 1) * 64] for k in range(8)]
                        wb = Wq
                    else:
                        lq = [wkv[:, k, (h - 8) * 64:(h - 7) * 64] for k in range(8)]
                        wb = Wkv
                    S.mm(pq, pq.t[0:64, :], [(lq[k], xt(k, s_ * 512, (s_ + 1) * 512)) for k in range(8)], reads=[wb, XTb[s_]])
                    S.mm(pr, pr.t[0:32, :], [(wrot.ap[:, k, h, :], xt(k, s_ * 512, (s_ + 1) * 512)) for k in range(8)],
                         reads=wrot.b + [XTb[s_]])
                    par = (h * 2 + s_) % 2
                    T1 = sv(7, F32, par * 4096, [512])
                    T2 = sv(7, F32, par * 4096 + 2048, [512])
                    cs_ = rope.ap[0:32, 0, s_ * 512:(s_ + 1) * 512]
                    sn_ = rope.ap[0:32, 1, s_ * 512:(s_ + 1) * 512]
                    tt("dve", T1.ap[0:32], pq.t[0:32, :], cs_, ALU.mult, reads=[pq] + rope.b, writes=T1.b)
                    tt("dve", T2.ap[0:32], pr.t[0:32, :], sn_, ALU.mult, reads=[pr] + rope.b, writes=T2.b)
                    if h < 8:
                        dst = QT[h // 4].ap[:, h % 4, s_ * 512:(s_ + 1) * 512]
                        db = [QT[h // 4].b[h % 4]]
                    else:
                        dst = KT.ap[:, h - 8, 128 + s_ * 512:128 + (s_ + 1) * 512]
                        db = KT.b
                    tt("dve", dst[0:32], T1.ap[0:32], T2.ap[0:32], ALU.add, reads=T1.b + T2.b, pw=db)
                    cp("act", dst[32:64], pq.t[32:64, :], reads=[pq], pw=db)
            for t in range(NT):
                pv_ = ps_next()
                S.mm(pv_, pv_.t[:, 0:128], [(xt(k, t * 128, (t + 1) * 128), wkv[:, k, 128:256]) for k in range(8)],
                     reads=[XTb[t // 4], Wkv])
                cp("act", Vv.ap[:, 1 + t, :], pv_.t[:, 0:128], reads=[pv_], pw=Vv.b)
            cp("dve", KTC.t[:, l, :, :], KT.ap[0:64, :, G:G + 128], reads=KT.b, writes=[KTC])
            cp("dve", VC.t[:, l, :], Vv.ap[:, NT, :], reads=Vv.b, writes=[VC])
            it = 0
            for i in range(NT):
                for kv in range(2):
                    has_prev = not (g == 0 and i == 0)
                    qr = QT[kv].ap[0:64, :, i * 128:(i + 1) * 128]
                    PTc = sv(8, BF16, (it % 2) * 2048, [512])
                    PTp = sv(8, BF16, (it % 2) * 2048 + 1024, [512])
                    d1 = sv(8, F32, 4096 + (it % 2) * 2048, [512])
                    it += 1
                    pc = ps_next()
                    o3 = pc.t[:, :].rearrange("p (h q) -> p h q", h=4)
                    S.mm(pc, o3, [(KT.ap[0:64, kv, 128 + i * 128:128 + (i + 1) * 128], qr), (ident, mcur)],
                         reads=KT.b + QT[kv].b + [CB])
                    act(PTc.ap, pc.t[:, :], AF.Exp, reads=[pc], writes=PTc.b, scale=0.125)
                    if has_prev:
                        pp = ps_next()
                        o3p = pp.t[:, :].rearrange("p (h q) -> p h q", h=4)
                        S.mm(pp, o3p, [(KT.ap[0:64, kv, i * 128:(i + 1) * 128], qr), (ident, mprev)],
                             reads=KT.b + QT[kv].b + [CB])
                        act(PTp.ap, pp.t[:, :], AF.Exp, reads=[pp], pw=PTp.b, scale=0.125)
                    po = ps_next()
                    pd = ps_next()
                    tv = [(Vv.ap[:, 1 + i, kv * 64:(kv + 1) * 64], PTc.ap)]
                    td = [(ones[:, 0:64], PTc.ap)]
                    if has_prev:
                        tv.append((Vv.ap[:, i, kv * 64:(kv + 1) * 64], PTp.ap))
                        td.append((ones[:, 0:64], PTp.ap))
                    S.mm(po, po.t[0:64, :], tv, reads=Vv.b + PTc.b)
                    S.mm(pd, pd.t[0:64, :], td, reads=PTc.b + [CB])
                    esk = ESK.t[:, l, 4 * kv:4 * kv + 4].unsqueeze(2).to_broadcast([64, 4, 128])
                    d13 = d1.ap[0:64].rearrange("p (h q) -> p h q", h=4)
                    tt("dve", d13, pd.t[0:64, :].rearrange("p (h q) -> p h q", h=4), esk, ALU.add, reads=[pd, ESK], writes=d1.b)
                    S.op("dve", lambda e, d13=d13: e.reciprocal(d13, d13), reads=d1.b, writes=d1.b)
                    tt("dve", AOT[kv].ap[0:64, :, i * 128:(i + 1) * 128], po.t[0:64, :].rearrange("p (h q) -> p h q", h=4), d13,
                       ALU.mult, reads=[po] + d1.b, pw=AOT[kv].b)
            if g == 0 and l == 0:
                dbg_dump("d_aot0", SLt[2][:, :], AOT[0].b)
                dbg_dump("d_aot1", SLt[3][:, :], AOT[1].b)
            return AOT

        def phase_ssm(g, l):
            Wss = ring_next()
            wss = rv(Wss, [8, 512])
            wload(Wss, wss, kview(w_in_d[l])[:, :, C_SSM:C_SSM + 512])
            UT = sv(0, BF16, 0, [4, G])
            YG = UT
            GLUT = sv(10, BF16, 0, [4, G])
            dsk = sv(6, BF16, 4096, [4, 128])
            S.dma("sp", dsk.ap, ssmd_d[l], reads=[B_ssmd], writes=dsk.b)
            for j in range(4):
                for s_ in range(2):
                    p = ps_next()
                    S.mm(p, p.t[:, :], [(wss[:, k, j * 128:(j + 1) * 128], xt(k, s_ * 512, (s_ + 1) * 512)) for k in range(8)],
                         reads=[Wss, XTb[s_]])
                    cp("act", UT.ap[:, j, s_ * 512:(s_ + 1) * 512], p.t[:, :], reads=[p], pw=[UT.b[j]])
            t1 = sv(7, F32, 0, [G]); t2 = sv(7, F32, 4096, [G])
            t3 = sv(8, F32, 0, [G]); t4 = sv(8, F32, 4096, [G])
            gre = sv(9, F32, 0, [G]); gim = sv(9, F32, 4096, [G])
            pr = [sv(1, BF16, i * 2048, [G]) for i in range(4)]
            BRb = [PS[4], PS[5]]
            BIb = [PS[6], PS[7]]
            bre_ps = PSD[2][:, :]
            bim_ps = PSD[3][:, :]
            for j in range(4):
                yp = [PS[(j % 2) * 2], PS[(j % 2) * 2 + 1]]
                for s_ in range(2):
                    S.op("pe", lambda e, j=j, s_=s_, yp=yp: e.matmul(yp[s_].t[:, :], dsk.ap[:, j, :], UT.ap[:, j, s_ * 512:(s_ + 1) * 512], start=True, stop=False),
                         reads=dsk.b + [UT.b[j]], writes=[yp[s_]], inc=False)
                for qq in range(4):
                    q = 4 * j + qq
                    tb = sv(4 + q % 2, F32, 0, [2, G])
                    tbw = sv(6, BF16, (q % 2) * 2048, [4, 128])
                    S.dma("sp", tb.ap, ssmt_d[l, q], reads=[B_ssmt], writes=tb.b)
                    S.dma("sp", tbw.ap, ssmw_d[l, q], reads=[B_ssmw], writes=tbw.b)
                    rb = SSMR.t[:, l, q:q + 1].to_broadcast([128, G])
                    for s_ in range(2):
                        c0, c1 = s_ * 512, (s_ + 1) * 512
                        S.mm(BRb[s_], BRb[s_].t[:, :], [(tbw.ap[:, 0, :], UT.ap[:, j, c0:c1])], reads=tbw.b + [UT.b[j]])
                        S.mm(BIb[s_], BIb[s_].t[:, :], [(tbw.ap[:, 1, :], UT.ap[:, j, c0:c1])], reads=tbw.b + [UT.b[j]])
                    Tc = tb.ap[:, 0, :]
                    Ts = tb.ap[:, 1, :]
                    tt("dve", t1.ap, bre_ps, Tc, ALU.mult, reads=BRb + tb.b, writes=t1.b)
                    tt("dve", t2.ap, bim_ps, Ts, ALU.mult, reads=BIb + tb.b, writes=t2.b)
                    tt("dve", t3.ap, bim_ps, Tc, ALU.mult, reads=BIb + tb.b, writes=t3.b)
                    tt("dve", t4.ap, bre_ps, Ts, ALU.mult, reads=BRb + tb.b, writes=t4.b)
                    tt("dve", t1.ap, t1.ap, t2.ap, ALU.add, reads=t1.b + t2.b, writes=t1.b)
                    tt("dve", t3.ap, t3.ap, t4.ap, ALU.subtract, reads=t3.b + t4.b, writes=t3.b)
                    S.op("dve", lambda e, rb=rb, q=q: e.tensor_tensor_scan(gre.ap, rb, t1.ap, SSMC.t[:, l, q, 0:1], ALU.mult, ALU.add),
                         reads=t1.b + [SSMR, SSMC], writes=gre.b)
                    S.op("dve", lambda e, rb=rb, q=q: e.tensor_tensor_scan(gim.ap, rb, t3.ap, SSMC.t[:, l, q, 1:2], ALU.mult, ALU.add),
                         reads=t3.b + [SSMR, SSMC], writes=gim.b)
                    GB = gre.b + gim.b
                    tt("dve", pr[0].ap, gre.ap, Tc, ALU.mult, reads=GB + tb.b, writes=pr[0].b)
                    stt(pr[1].ap, gim.ap, -1.0, Ts, ALU.mult, ALU.mult, reads=GB + tb.b, writes=pr[1].b)
                    tt("dve", pr[2].ap, gre.ap, Ts, ALU.mult, reads=GB + tb.b, writes=pr[2].b)
                    tt("dve", pr[3].ap, gim.ap, Tc, ALU.mult, reads=GB + tb.b, writes=pr[3].b)
                    lastq = qq == 3
                    for s_ in range(2):
                        c0, c1 = s_ * 512, (s_ + 1) * 512
                        for i in range(4):
                            fin = lastq and i == 3
                            S.op("pe", lambda e, s_=s_, yp=yp, tbw=tbw, i=i, c0=c0, c1=c1, fin=fin:
                                 e.matmul(yp[s_].t[:, :], tbw.ap[:, 2 + i // 2, :], pr[i].ap[:, c0:c1], start=False, stop=fin),
                                 reads=tbw.b + pr[i].b, pwrites=[yp[s_]], inc=(i == 3))
                    cz = SM2.t[:, 16:17]
                    Tcl = tb.ap[:, 0, G - 1:G]
                    Tsl = tb.ap[:, 1, G - 1:G]
                    GBt = GB + tb.b
                    ts("dve", cz, gim.ap[:, G - 1:G], Tsl, None, ALU.mult, None, reads=GBt, pw=[SM2])
                    stt(SSMC.t[:, l, q, 0:1], gre.ap[:, G - 1:G], Tcl, cz, ALU.mult, ALU.subtract, reads=GBt + [SM2], pw=[SSMC])
                    ts("dve", cz, gim.ap[:, G - 1:G], Tcl, None, ALU.mult, None, reads=GBt, pw=[SM2])
                    stt(SSMC.t[:, l, q, 1:2], gre.ap[:, G - 1:G], Tsl, cz, ALU.mult, ALU.add, reads=GBt + [SM2], pw=[SSMC])
                for s_ in range(2):
                    act(YG.ap[:, j, s_ * 512:(s_ + 1) * 512], yp[s_].t[:, :], AF.Gelu_apprx_tanh, reads=[yp[s_]], pw=[YG.b[j]])
            Wgl = ring_next()
            wgl = rv(Wgl, [4, 1024])
            wload(Wgl, wgl, kview(w_glu_d[l]))
            for c in range(4):
                for s_ in range(2):
                    phv = ps_next()
                    phg = ps_next()
                    S.mm(phv, phv.t[:, :], [(wgl[:, k, c * 128:(c + 1) * 128], YG.ap[:, k, s_ * 512:(s_ + 1) * 512]) for k in range(4)],
                         reads=[Wgl] + YG.b)
                    S.mm(phg, phg.t[:, :], [(wgl[:, k, 512 + c * 128:512 + (c + 1) * 128], YG.ap[:, k, s_ * 512:(s_ + 1) * 512]) for k in range(4)],
                         reads=[Wgl] + YG.b)
                    sg = sv(7, F32, ((c * 2 + s_) % 2) * 2048, [512])
                    act(sg.ap, phg.t[:, :], AF.Sigmoid, reads=[phg], writes=sg.b)
                    tt("dve", GLUT.ap[:, c, s_ * 512:(s_ + 1) * 512], phv.t[:, :], sg.ap, ALU.mult, reads=[phv] + sg.b, pw=[GLUT.b[c]])
            if g == 0 and l == 0:
                dbg_dump("d_glut", SLt[10][:, :], GLUT.b)
            return GLUT

        def phase_pool(g, l):
            Wpo = ring_next()
            wpo = rv(Wpo, [8, 512])
            wload(Wpo, wpo, kview(w_in_d[l])[:, :, C_POOL:C_POOL + 512])
            Wpw = ring_next()
            wpw = rv(Wpw, [4, 128])
            wload(Wpw, wpw, pool_w_d[l].rearrange("g c d -> c g d"))
            PL = sv(5, BF16, 0, [4, G])
            PMT = sv(6, BF16, 0, [4, G])
            L = G + 16
            for j in range(4):
                UP = sv(0, F32, 0, [L])
                SA = sv(1, F32, 0, [L])
                SB_ = sv(4, F32, 0, [L])
                cp("dve", UP.ap[:, 0:16], UPC.t[:, l, j, :], reads=[UPC], writes=UP.b)
                for s_ in range(2):
                    p = ps_next()
                    S.mm(p, p.t[:, :], [(wpo[:, k, j * 128:(j + 1) * 128], xt(k, s_ * 512, (s_ + 1) * 512)) for k in range(8)],
                         reads=[Wpo, XTb[s_]])
                    cp("act", UP.ap[:, 16 + s_ * 512:16 + (s_ + 1) * 512], p.t[:, :], reads=[p], pw=UP.b)
                cp("dve", UPC.t[:, l, j, :], UP.ap[:, G:G + 16], reads=UP.b, writes=[UPC])
                cur, nxt, other = UP, SA, SB_
                for s in range(1, j + 2):
                    sh = 1 << (s - 1)
                    lo = (1 << s) - 1
                    tt("dve", nxt.ap[:, lo:L], cur.ap[:, lo:L], cur.ap[:, lo - sh:L - sh], ALU.add, reads=cur.b, writes=nxt.b)
                    cur, nxt = nxt, (other if nxt is SA else SA)
                w = 1 << (j + 1)
                stt(PL.ap[:, j, :], cur.ap[:, 16:L], 1.0 / w, UP.ap[:, 16:L], ALU.mult, ALU.subtract, reads=cur.b + UP.b, writes=[PL.b[j]])
                if g == 0:
                    tmp = SM.t[:, 48:64]
                    tt("dve", tmp, cur.ap[:, 16:32], INVC[:, j, :], ALU.mult, reads=cur.b + [CF], pw=[SM])
                    tt("dve", PL.ap[:, j, 0:16], tmp, UP.ap[:, 16:32], ALU.subtract, reads=[SM] + UP.b, pw=[PL.b[j]])
                for s_ in range(2):
                    pm = ps_next()
                    S.mm(pm, pm.t[:, :], [(wpw[:, j, :], PL.ap[:, j, s_ * 512:(s_ + 1) * 512])], reads=[Wpw, PL.b[j]])
                    act(PMT.ap[:, j, s_ * 512:(s_ + 1) * 512], pm.t[:, :], AF.Identity, reads=[pm, PSC], pw=[PMT.b[j]],
                        scale=PSC.t[:, l, j:j + 1])
            if g == 0 and l == 0:
                dbg_dump("d_pmt", SLt[6][:, :], PMT.b)
            return PMT

        def phase_merge(g, l, AOT, GLUT, PMT):
            WA = [sv(4, BF16, 0, [4, D]), sv(5, BF16, 0, [4, D])]
            for hf in range(2):
                S.dma("pool", WA[hf].ap[0:64], w_bra_d[l].rearrange("(h p) d -> p h d", p=64)[:, hf * 4:(hf + 1) * 4, :], writes=WA[hf].b)
            WSs = sv(9, BF16, 0, [4, D])
            S.dma("pool", WSs.ap, kview(w_brs_d[l]), writes=WSs.b)
            r = [ring_next() for _ in range(4)]
            wbp = rv(r[0], [4, D])
            wload(r[0], wbp, kview(w_brp_d[l]))
            MT = [sv(7, BF16, 0, [4, G]), sv(8, BF16, 0, [4, G])]
            for hf in range(2):
                gw = []
                for i in range(3):
                    gv = rv(r[1 + i], [8, 512])
                    c0 = C_GATE + i * D + hf * 512
                    wload(r[1 + i], gv, kview(w_in_d[l])[:, :, c0:c0 + 512])
                    gw.append(gv)
                for d4 in range(4):
                    dc = hf * 4 + d4
                    for s_ in range(2):
                        c0, c1 = s_ * 512, (s_ + 1) * 512
                        pg = [ps_next() for _ in range(3)]
                        po = [ps_next() for _ in range(3)]
                        for i in range(3):
                            S.mm(pg[i], pg[i].t[:, :], [(gw[i][:, k, d4 * 128:(d4 + 1) * 128], xt(k, c0, c1)) for k in range(8)],
                                 reads=[r[1 + i], XTb[s_]])
                        S.mm(po[0], po[0].t[:, :],
                             [(WA[h // 4].ap[0:64, h % 4, dc * 128:(dc + 1) * 128], AOT[h // 4].ap[0:64, h % 4, c0:c1]) for h in range(8)],
                             reads=WA[0].b + WA[1].b + AOT[0].b + AOT[1].b)
                        S.mm(po[1], po[1].t[:, :], [(WSs.ap[:, k, dc * 128:(dc + 1) * 128], GLUT.ap[:, k, c0:c1]) for k in range(4)],
                             reads=WSs.b + GLUT.b)
                        S.mm(po[2], po[2].t[:, :], [(wbp[:, k, dc * 128:(dc + 1) * 128], PMT.ap[:, k, c0:c1]) for k in range(4)],
                             reads=[r[0]] + PMT.b)
                        sg = [sv(0, F32, i * 2048, [512]) for i in range(3)]
                        for i in range(3):
                            act(sg[i].ap, pg[i].t[:, :], AF.Sigmoid, reads=[pg[i], BG], writes=sg[i].b,
                                bias=BG.t[:, l, i * 8 + dc:i * 8 + dc + 1])
                        m1 = sv(1, F32, 0, [512])
                        m2 = sv(1, F32, 2048, [512])
                        tt("dve", m1.ap, po[0].t[:, :], sg[0].ap, ALU.mult, reads=[po[0]] + sg[0].b, writes=m1.b)
                        tt("dve", m2.ap, po[1].t[:, :], sg[1].ap, ALU.mult, reads=[po[1]] + sg[1].b, writes=m2.b)
                        tt("dve", m1.ap, m1.ap, m2.ap, ALU.add, reads=m1.b + m2.b, writes=m1.b)
                        tt("dve", m2.ap, po[2].t[:, :], sg[2].ap, ALU.mult, reads=[po[2]] + sg[2].b, writes=m2.b)
                        tt("dve", MT[hf].ap[:, d4, c0:c1], m1.ap, m2.ap, ALU.add, reads=m1.b + m2.b, pw=[MT[hf].b[d4]])
            if g == 0 and l == 0:
                dbg_dump("d_mt0", SLt[7][:, :], MT[0].b)
                dbg_dump("d_mt1", SLt[8][:, :], MT[1].b)
            return MT

        def phase_xattn_kv(g, l):
            KKT = sv(0, BF16, 0, [8, NMEM])
            VV = sv(0, BF16, 4096, [2, D])
            WK = []
            for u in range(4):
                rr_ = ring_next()
                wload(rr_, rv(rr_, [8, 512]), kview(xa_wkv_d[l])[:, :, u * 512:(u + 1) * 512])
                WK.append(rr_)
            for c in range(8):
                p = ps_next()
                w = rv(WK[c // 4], [8, 512])
                S.mm(p, p.t[:, 0:NMEM], [(w[:, k, (c % 4) * 128:(c % 4 + 1) * 128], MEMT.t[:, k, :]) for k in range(8)],
                     reads=[WK[c // 4], MEMT])
                cp("act", KKT.ap[:, c, :], p.t[:, 0:NMEM], reads=[p], pw=KKT.b)
            for mt in range(2):
                for hf in range(2):
                    p = ps_next()
                    w = rv(WK[2 + hf], [8, 512])
                    S.mm(p, p.t[:, :], [(MEMT.t[:, k, mt * 128:(mt + 1) * 128], w[:, k, :]) for k in range(8)], reads=[WK[2 + hf], MEMT])
                    cp("act", VV.ap[:, mt, hf * 512:(hf + 1) * 512], p.t[:, :], reads=[p], pw=VV.b)

        def phase_xattn(g, l):
            KKT = sv(0, BF16, 0, [8, NMEM])
            VV = sv(0, BF16, 4096, [2, D])
            QXT = [sv(1, BF16, 0, [4, G]), sv(2, BF16, 0, [4, G])]
            OXT = [sv(4, BF16, 0, [4, G]), sv(5, BF16, 0, [4, G])]
            WQ = []
            for u in range(2):
                rr_ = ring_next()
                wload(rr_, rv(rr_, [8, 512]), kview(xa_wq_d[l])[:, :, u * 512:(u + 1) * 512])
                WQ.append(rr_)
            for c in range(8):
                for s_ in range(2):
                    p = ps_next()
                    w = rv(WQ[c // 4], [8, 512])
                    S.mm(p, p.t[:, :], [(w[:, k, (c % 4) * 128:(c % 4 + 1) * 128], xt(k, s_ * 512, (s_ + 1) * 512)) for k in range(8)],
                         reads=[WQ[c // 4], XTb[s_]])
                    cp("act", QXT[c // 4].ap[:, c % 4, s_ * 512:(s_ + 1) * 512], p.t[:, :], reads=[p], pw=[QXT[c // 4].b[c % 4]])
            it = 0
            for h in range(4):
                for s_ in range(2):
                    c0, c1 = s_ * 512, (s_ + 1) * 512
                    PT = [sv(3, BF16, (it % 2) * 2048 + mt * 1024, [512]) for mt in range(2)]
                    rden = sv(3, F32, 4096 + (it % 2) * 2048, [512])
                    it += 1
                    for mt in range(2):
                        p = ps_next()
                        S.mm(p, p.t[:, :],
                             [(KKT.ap[:, 2 * h + dcc, mt * 128:(mt + 1) * 128], QXT[(2 * h + dcc) // 4].ap[:, (2 * h + dcc) % 4, c0:c1]) for dcc in range(2)],
                             reads=KKT.b + QXT[(2 * h) // 4].b)
                        if mt == 0:
                            act(PT[mt].ap, p.t[:, :], AF.Exp, reads=[p], writes=PT[mt].b, scale=1.0 / 16.0)
                        else:
                            act(PT[mt].ap, p.t[:, :], AF.Exp, reads=[p], pw=PT[mt].b, scale=1.0 / 16.0)
                    pd = ps_next()
                    S.mm(pd, pd.t[:, :], [(ones, PT[mt].ap) for mt in range(2)], reads=PT[0].b + [CB])
                    S.op("dve", lambda e, rden=rden, pd=pd: e.reciprocal(rden.ap, pd.t[:, :]), reads=[pd], writes=rden.b)
                    for dcc in range(2):
                        po = ps_next()
                        c = 2 * h + dcc
                        S.mm(po, po.t[:, :], [(VV.ap[:, mt, c * 128:(c + 1) * 128], PT[mt].ap) for mt in range(2)], reads=VV.b + PT[0].b)
                        tt("dve", OXT[c // 4].ap[:, c % 4, c0:c1], po.t[:, :], rden.ap, ALU.mult, reads=[po] + rden.b, pw=[OXT[c // 4].b[c % 4]])
            return OXT

        def phase_ffn(g, l):
            moe = (l % 2 == 1)
            jj = l // 2
            for t in range(NT):
                act(X[t].t[:, :], X[t].t[:, :], AF.Identity, reads=[X[t]], writes=[X[t]], scale=ALPHA)
            if moe:
                Wr = ring_next()
                wr = rv(Wr, [8, NEXP])
                S.dma("pool", wr, moe_wr_d[jj].rearrange("(k p) e -> p k e", p=128), writes=[Wr], **NCD)
                S.dma("sp", BR.t[:, :], moe_br_d[jj].partition_broadcast(128), writes=[BR], **NCD)
                pl = ps_next()
                for t in range(NT):
                    S.mm(pl, pl.t[:, t * 8:(t + 1) * 8], [(xt(k, t * 128, (t + 1) * 128), wr[:, k, :]) for k in range(8)],
                         reads=[Wr, XTb[t // 4]])
                lg = RT.t[:, :, 0:8]
                eq = RT.t[:, :, 8:16]
                l2 = RT.t[:, :, 16:24]
                ex = RT.t[:, :, 24:32]
                m1 = RT.t[:, :, 32:33]
                m2 = RT.t[:, :, 33:34]
                dn = RT.t[:, :, 34:35]
                R_ = [RT]
                tt("dve", lg, pl.t[:, 0:64].rearrange("p (t e) -> p t e", t=NT), BR.t[:, :].unsqueeze(1).to_broadcast([128, NT, NEXP]),
                   ALU.add, reads=[pl, BR], writes=R_)
                S.op("dve", lambda e: e.tensor_reduce(m1, lg, AX.X, ALU.max), reads=R_, writes=R_)
                tt("dve", eq, lg, m1.to_broadcast([128, NT, NEXP]), ALU.is_equal, reads=R_, writes=R_)
                S.op("dve", lambda e: e.scalar_tensor_tensor(l2, eq, -1e30, lg, ALU.mult, ALU.add), reads=R_, writes=R_)
                S.op("dve", lambda e: e.tensor_reduce(m2, l2, AX.X, ALU.max), reads=R_, writes=R_)
                tt("dve", eq, lg, m2.to_broadcast([128, NT, NEXP]), ALU.is_ge, reads=R_, writes=R_)
                tt("dve", l2, lg, m1.to_broadcast([128, NT, NEXP]), ALU.subtract, reads=R_, writes=R_)
                act(ex, l2, AF.Exp, reads=R_, writes=R_)
                tt("dve", ex, ex, eq, ALU.mult, reads=R_, writes=R_)
                S.op("dve", lambda e: e.tensor_reduce(dn, ex, AX.X, ALU.add), reads=R_, writes=R_)
                S.op("dve", lambda e: e.reciprocal(dn, dn), reads=R_, writes=R_)
                tt("dve", CW.t[:, :, :], ex, dn.to_broadcast([128, NT, NEXP]), ALU.mult, reads=R_, writes=[CW])
                nexp, F, gu_of, dn_of = NEXP, DFFE, (lambda e_: moe_gu_d[jj, e_]), (lambda e_: moe_dn_d[jj, e_])
            else:
                nexp, F, gu_of, dn_of = 1, DFF, (lambda e_: ffn_gu_d[jj]), (lambda e_: ffn_dn_d[jj])
            nch = F // 128
            fgs = []
            c = 0
            while c < nch:
                n = min(4, nch - c)
                fgs.append((c, n))
                c += n
            fi = 0
            for e_ in range(nexp):
                gu = kview(gu_of(e_))
                dnw = dn_of(e_)
                for (c0, n) in fgs:
                    Wg = ring_next(); Wu = ring_next(); Wd = ring_next()
                    wg = rv(Wg, [8, n * 128]); wu = rv(Wu, [8, n * 128]); wd = rv(Wd, [n, D])
                    wload(Wg, wg, gu[:, :, c0 * 128:(c0 + n) * 128])
                    wload(Wu, wu, gu[:, :, F + c0 * 128:F + (c0 + n) * 128])
                    wload(Wd, wd, dnw[c0 * 128:(c0 + n) * 128, :].rearrange("(c p) d -> p c d", p=128))
                    HT = sv(fi % 2, BF16, 0, [4, G])
                    fi += 1
                    for s_ in range(2):
                        a0, a1 = s_ * 512, (s_ + 1) * 512
                        for fc in range(n):
                            pg = ps_next()
                            pu = ps_next()
                            S.mm(pg, pg.t[:, :], [(wg[:, k, fc * 128:(fc + 1) * 128], xt(k, a0, a1)) for k in range(8)], reads=[Wg, XTb[s_]])
                            S.mm(pu, pu.t[:, :], [(wu[:, k, fc * 128:(fc + 1) * 128], xt(k, a0, a1)) for k in range(8)], reads=[Wu, XTb[s_]])
                            sg = sv(2, F32, ((fc + s_ * n) % 3) * 2048, [512])
                            act(sg.ap, pg.t[:, :], AF.Silu, reads=[pg], writes=sg.b)
                            tt("dve", HT.ap[:, fc, a0:a1], pu.t[:, :], sg.ap, ALU.mult, reads=[pu] + sg.b, pw=[HT.b[fc]])
                    for s_ in range(2):
                        for t4 in range(4):
                            t = s_ * 4 + t4
                            for hf in range(2):
                                p = ps_next()
                                S.mm(p, p.t[:, :], [(HT.ap[:, fc, t * 128:(t + 1) * 128], wd[:, fc, hf * 512:(hf + 1) * 512]) for fc in range(n)],
                                     reads=HT.b[0:n] + [Wd])
                                xa = X[t].t[:, hf * 512:(hf + 1) * 512]
                                if moe:
                                    stt(xa, p.t[:, :], CW.t[:, t, e_:e_ + 1], xa, ALU.mult, ALU.add, reads=[p, CW, X[t]], pw=[X[t]])
                                else:
                                    tt("dve", xa, xa, p.t[:, :], ALU.add, reads=[p, X[t]], pw=[X[t]])

        for g in range(ng):
            for t in range(NT):
                S.dma("sp", X[t].t[:, :], x_d[g * G + t * 128:g * G + (t + 1) * 128, :], writes=[X[t]])
                make_xt(t)
            PH = phases if phases is not None else {"attn", "ssm", "pool", "merge", "wo", "xattn", "xo", "ffn"}
            for l in range(depth):
                AOT = [sv(2, BF16, 0, [4, G]), sv(3, BF16, 0, [4, G])]
                GLUT = sv(10, BF16, 0, [4, G])
                PMT = sv(6, BF16, 0, [4, G])
                MT = [sv(7, BF16, 0, [4, G]), sv(8, BF16, 0, [4, G])]
                OXT = [sv(4, BF16, 0, [4, G]), sv(5, BF16, 0, [4, G])]
                if "attn" in PH:
                    AOT = phase_attn(g, l)
                if "ssm" in PH:
                    GLUT = phase_ssm(g, l)
                if "pool" in PH:
                    PMT = phase_pool(g, l)
                if "merge" in PH:
                    MT = phase_merge(g, l, AOT, GLUT, PMT)
                if "wo" in PH:
                    out_proj(lambda k: MT[k // 4].ap[:, k % 4, :], MT[0].b + MT[1].b, w_o_d[l], l, 0, g, False,
                             filler=(lambda g=g, l=l: phase_xattn_kv(g, l)) if "xattn" in PH else None)
                if "xattn" in PH:
                    OXT = phase_xattn(g, l)
                if "xo" in PH:
                    out_proj(lambda k: OXT[k // 4].ap[:, k % 4, :], OXT[0].b + OXT[1].b, xa_wo_d[l], l, 1, g, False)
                if "ffn" in PH:
                    phase_ffn(g, l)
                layernorm(2, l, g, l == depth - 1)
        fin = [B_out] + ([B_dbg] if dbg else [])
        S.finish(fin)
        S.emit()
        build.stats = (S.ninst, S.nsem, {e: len(v) for e, v in S.ops.items()})
    return nc


def make_consts(seq):
    bf = ml_dtypes.bfloat16
    c_bf = np.zeros((128, 1280), np.float32)
    c_bf[:, 0:128] = np.eye(128)
    c_bf[:, 128:256] = 1.0
    k = np.arange(128)[:, None]
    q = np.arange(128)[None, :]
    mc = np.where(k <= q, 0.0, -30000.0)
    mp = np.where(k > q, 0.0, -30000.0)
    c_bf[:, 256:768] = np.tile(mc, (1, 4))
    c_bf[:, 768:1280] = np.tile(mp, (1, 4))
    c_f = np.zeros((128, 72), np.float32)
    for j, w in enumerate((2, 4, 8, 16)):
        c_f[:, j * 16:(j + 1) * 16] = 1.0 / np.minimum(np.arange(1, 17), w)
    p = np.arange(128)
    for qq in range(4):
        c_f[:, 64 + qq] = (p // 32 == qq)
    c_f[:, 68] = ((p // 16) % 2 == 0)
    c_f[:, 69] = ((p // 16) % 2 == 1)
    half = 8
    inv_freq = np.power(np.float32(500000.0), -np.arange(half, dtype=np.float32) / half).astype(np.float32)
    ang = np.arange(seq, dtype=np.float32)[None, :] * inv_freq[:, None]
    c_rope = np.zeros((32, 2, seq), np.float32)
    c_rope[:, 0, :] = 1.0
    c_rope[0:8, 0, :] = np.cos(ang)
    c_rope[8:16, 0, :] = np.cos(ang)
    c_rope[0:8, 1, :] = -np.sin(ang)
    c_rope[8:16, 1, :] = np.sin(ang)
    return c_bf.astype(bf), c_f, c_rope


_NC_CACHE = {}


def kernel(**inputs):
    x = np.asarray(inputs["x"], np.float32)
    B, SEQ, _ = x.shape
    ng = SEQ // G
    key = (ng, DEPTH)
    if key not in _NC_CACHE:
        _NC_CACHE[key] = build(ng, DEPTH)
    nc = _NC_CACHE[key]
    c_bf, c_f, c_rope = make_consts(SEQ)
    shared = {k: np.ascontiguousarray(np.asarray(v, np.float32)) for k, v in inputs.items() if k not in ("x", "mem")}
    shared.update(c_bf=c_bf, c_f=c_f, c_rope=c_rope)
    in_maps = []
    for b in range(B):
        m = dict(shared)
        m["x"] = np.ascontiguousarray(x[b])
        m["mem"] = np.ascontiguousarray(np.asarray(inputs["mem"], np.float32)[b])
        in_maps.append(m)
    res = run_bass_kernel_spmd(nc, in_maps, core_ids=list(range(B)))
    return np.stack([np.asarray(r["out"], np.float32) for r in res.results], axis=0)
```

```python
import math
from contextlib import ExitStack

import ml_dtypes
import numpy as np

import concourse.bass as bass
import concourse.mybir as mybir
from concourse.bass_utils import run_bass_kernel_spmd

F32 = mybir.dt.float32
BF16 = mybir.dt.bfloat16
I32 = mybir.dt.int32
AF = mybir.ActivationFunctionType
ALU = mybir.AluOpType
AX = mybir.AxisListType

D = 1024
G = 1024
NT = G // 128
NMEM = 256
DFF = 2816
DFFE = 3584
NEXP = 8
DEPTH = 4
ALPHA = (2.0 * DEPTH) ** 0.25
LN_EPS = 1e-5
IN_W = 4864
C_Q, C_K, C_V, C_SSM, C_POOL, C_GATE = 0, 512, 640, 768, 1280, 1792
SLOT_B = 8192
NSLOT = 11
NRING = 6
TWO_PI = 2.0 * math.pi


class Buf:
    def __init__(self, name, t=None):
        self.name = name
        self.t = t
        self.w = {}
        self.r = {}
        self.sem = None
        self.dcnt = 0


class Sched:
    ENG = ("pe", "act", "dve", "pool", "sp")

    def __init__(self, nc, stack, same_eng_sync=("act", "dve", "pool")):
        self.nc = nc
        self.stack = stack
        self.same = set(same_eng_sync)
        self.ops = {e: [] for e in self.ENG}
        self.sem = {e: stack.enter_context(nc.semaphore("s_" + e)) for e in self.ENG}
        self.cnt = {e: 0 for e in self.ENG}
        self.waited = {e: {} for e in self.ENG}
        self.nsem = 5
        self.ninst = 0

    @staticmethod
    def _add(deps, k, h, v):
        if k not in deps or deps[k][1] < v:
            deps[k] = (h, v)

    def _deps(self, reads, writes, pwrites):
        deps = {}
        for b in reads:
            for k, (h, v, _p) in b.w.items():
                self._add(deps, k, h, v)
        for b in writes:
            for k, (h, v, _p) in b.w.items():
                self._add(deps, k, h, v)
            for k, (h, v) in b.r.items():
                self._add(deps, k, h, v)
        for b in pwrites:
            for k, (h, v, p) in b.w.items():
                if not p:
                    self._add(deps, k, h, v)
            for k, (h, v) in b.r.items():
                self._add(deps, k, h, v)
        return deps

    def _waits(self, eng, deps):
        for k, (h, v) in deps.items():
            if k == eng and eng not in self.same:
                continue
            if self.waited[eng].get(k, 0) >= v:
                continue
            self.waited[eng][k] = v
            self.ops[eng].append(lambda e, h=h, v=v: e.wait_ge(h, v))
            self.ninst += 1

    def _reg(self, t, reads, writes, pwrites):
        k, h, v = t
        for b in reads:
            if k not in b.r or b.r[k][1] < v:
                b.r[k] = (h, v)
        for b in writes:
            b.w = {k: (h, v, False)}
            b.r = {}
        for b in pwrites:
            if b.r:
                b.w = {k: (h, v, True)}
                b.r = {}
            else:
                if k not in b.w or b.w[k][1] < v:
                    b.w[k] = (h, v, True)

    def op(self, eng, fn, reads=(), writes=(), pwrites=(), inc=True):
        self._waits(eng, self._deps(reads, writes, pwrites))
        sem = self.sem[eng]
        if inc:
            self.cnt[eng] += 1
            n = self.cnt[eng]
            self.ops[eng].append(lambda e, fn=fn, sem=sem: fn(e).then_inc(sem, 1))
        else:
            n = self.cnt[eng] + 1
            self.ops[eng].append(lambda e, fn=fn: fn(e))
        self.ninst += 1
        self._reg((eng, sem, n), reads, writes, pwrites)

    def dma(self, q, out_ap, in_ap, reads=(), writes=(), pwrites=(), **kw):
        dst = (list(writes) + list(pwrites))[0]
        if dst.sem is None:
            dst.sem = self.stack.enter_context(self.nc.semaphore("d_" + dst.name))
            self.nsem += 1
        self._waits(q, self._deps(reads, writes, pwrites))
        dst.dcnt += 16
        sem = dst.sem
        self.ops[q].append(
            lambda e, o=out_ap, i=in_ap, sem=sem, kw=kw: e.dma_start(out=o, in_=i, **kw).then_inc(sem, 16)
        )
        self.ninst += 1
        self._reg(("d_" + dst.name, sem, dst.dcnt), reads, writes, pwrites)

    def mm(self, ps, out_ap, terms, reads):
        n = len(terms)
        for i, (l, r) in enumerate(terms):
            first, last = i == 0, i == n - 1
            self.op(
                "pe",
                lambda e, l=l, r=r, first=first, last=last, o=out_ap: e.matmul(o, l, r, start=first, stop=last),
                reads=reads if first else (),
                writes=[ps] if first else (),
                inc=last,
            )

    def finish(self, bufs):
        deps = {}
        for b in bufs:
            for k, (h, v, _p) in b.w.items():
                self._add(deps, k, h, v)
        self._waits("sp", deps)

    def emit(self):
        nc = self.nc
        ops = self.ops
        with nc.Block() as block:
            @block.tensor
            def _(e):
                for f in ops["pe"]:
                    f(e)

            @block.scalar
            def _(e):
                for f in ops["act"]:
                    f(e)

            @block.vector
            def _(e):
                for f in ops["dve"]:
                    f(e)

            @block.gpsimd
            def _(e):
                for f in ops["pool"]:
                    f(e)

            @block.sync
            def _(e):
                for f in ops["sp"]:
                    f(e)


class V:
    def __init__(self, ap, b):
        self.ap = ap
        self.b = b


def build(ng, depth, dbg=False, phases=None, same=("act", "dve", "pool")):
    nc = bass.Bass("TRN2", target_bir_lowering=False)
    SEQ = ng * G
    nden = (depth + 1) // 2
    nmoe = depth // 2

    def din(name, shape, dt=F32):
        return nc.dram_tensor(name, list(shape), dt, kind="ExternalInput").ap()

    x_d = din("x", [SEQ, D])
    mem_d = din("mem", [NMEM, D])
    w_in_d = din("w_in", [depth, D, IN_W])
    b_gate_d = din("b_gate", [depth, 3 * D])
    sinks_d = din("attn_sinks", [depth, 8])
    a_re_d = din("ssm_a_re", [depth, 32, 64])
    a_im_d = din("ssm_a_im", [depth, 32, 64])
    ldt_d = din("ssm_log_dt", [depth, 32])
    b_re_d = din("ssm_b_re", [depth, 32, 64, 16])
    b_im_d = din("ssm_b_im", [depth, 32, 64, 16])
    c_re_d = din("ssm_c_re", [depth, 32, 16, 64])
    c_im_d = din("ssm_c_im", [depth, 32, 16, 64])
    ssm_d_d = din("ssm_d", [depth, 512])
    w_glu_d = din("ssm_w_glu", [depth, 512, 1024])
    pool_w_d = din("pool_w", [depth, 4, 128, 128])
    pool_sc_d = din("pool_scale", [depth, 512])
    w_bra_d = din("w_br_attn", [depth, 512, D])
    w_brs_d = din("w_br_ssm", [depth, 512, D])
    w_brp_d = din("w_br_pool", [depth, 512, D])
    w_o_d = din("w_o", [depth, D, D])
    ln_g_d = [din(f"ln{i}_g", [depth, D]) for i in (1, 2, 3)]
    ln_b_d = [din(f"ln{i}_b", [depth, D]) for i in (1, 2, 3)]
    xa_wq_d = din("xa_wq", [depth, D, D])
    xa_wkv_d = din("xa_wkv", [depth, D, 2 * D])
    xa_wo_d = din("xa_wo", [depth, D, D])
    ffn_gu_d = din("ffn_w_gu", [nden, D, 2 * DFF])
    ffn_dn_d = din("ffn_w_down", [nden, DFF, D])
    if nmoe:
        moe_wr_d = din("moe_w_router", [nmoe, D, NEXP])
        moe_br_d = din("moe_b_router", [nmoe, NEXP])
        moe_gu_d = din("moe_w_gu", [nmoe, NEXP, D, 2 * DFFE])
        moe_dn_d = din("moe_w_down", [nmoe, NEXP, DFFE, D])
    c_bf_d = din("c_bf", [128, 1280], BF16)
    c_f_d = din("c_f", [128, 72])
    c_rope_d = din("c_rope", [32, 2, SEQ])
    out_d = nc.dram_tensor("out", [SEQ, D], F32, kind="ExternalOutput").ap()
    ssmt_d = nc.dram_tensor("ssmt", [depth, 16, 128, 2, G], F32).ap()
    ssmw_d = nc.dram_tensor("ssmw", [depth, 16, 128, 4, 128], BF16).ap()
    ssmd_d = nc.dram_tensor("ssmd", [depth, 128, 4, 128], BF16).ap()
    dbg_d = {}
    if dbg:
        for nm in ("d_x1", "d_x2", "d_x3"):
            dbg_d[nm] = nc.dram_tensor(nm, [G, D], F32, kind="ExternalOutput").ap()
        for nm in ("d_aot0", "d_aot1", "d_glut", "d_pmt", "d_mt0", "d_mt1"):
            dbg_d[nm] = nc.dram_tensor(nm, [128, SLOT_B // 2], BF16, kind="ExternalOutput").ap()

    with ExitStack() as st:
        S = Sched(nc, st, same_eng_sync=same)

        def sb(name, shape, dt):
            return Buf(name, st.enter_context(nc.sbuf_tensor(name, list(shape), dt)))

        X = [sb(f"X{i}", [128, D], F32) for i in range(NT)]
        XTt = st.enter_context(nc.sbuf_tensor("XT", [128, 8, G], BF16))
        XTb = [Buf("XT0"), Buf("XT1")]
        CB = sb("CB", [128, 1280], BF16)
        CF = sb("CF", [128, 72], F32)
        MEMT = sb("MEMT", [128, 8, NMEM], BF16)
        SSMC = sb("SSMC", [128, depth, 16, 2], F32)
        KTC = sb("KTC", [64, depth, 2, 128], BF16)
        VC = sb("VC", [128, depth, 128], BF16)
        UPC = sb("UPC", [128, depth, 4, 16], F32)
        SSMR = sb("SSMR", [128, depth, 16], F32)
        GL = sb("GL", [128, depth, 16, 2], F32)
        TL = sb("TL", [128, depth, 16, 2], F32)
        ESK = sb("ESK", [64, depth, 8], F32)
        BG = sb("BG", [128, depth, 24], F32)
        PSC = sb("PSC", [128, depth, 4], F32)
        LNG = sb("LNG", [128, 2, D], F32)
        SM = sb("SM", [128, 64], F32)
        SM2 = sb("SM2", [128, 32], F32)
        CW = sb("CW", [128, NT, NEXP], F32)
        RT = sb("RT", [128, 8, 64], F32)
        BR = sb("BRT", [128, NEXP], F32)
        EPS = sb("EPS", [128, 1], F32)
        RING = [sb(f"R{i}", [128, SLOT_B // 2], BF16) for i in range(NRING)]
        SLt = [st.enter_context(nc.sbuf_tensor(f"SL{i}", [128, SLOT_B // 2], BF16)) for i in range(NSLOT)]
        SLq = [[Buf(f"SL{i}q{j}") for j in range(4)] for i in range(NSLOT)]
        PSD = [st.enter_context(nc.psum_tensor(f"psd{i}", [128, 1024], F32)) for i in range(4)]
        PS = [Buf(f"ps{i}", PSD[i // 2][:, (i % 2) * 512:(i % 2 + 1) * 512]) for i in range(8)]
        B_ssmw = Buf("ssmw")
        B_ssmt = Buf("ssmt")
        B_ssmd = Buf("ssmd")
        B_out = Buf("outb")
        B_dbg = Buf("dbgb")
        state = {"ps": 0, "ring": 0}

        def ps_next():
            p = PS[state["ps"] % 8]
            state["ps"] += 1
            return p

        def ps_sub(lo, n, key):
            i = state.get(key, 0)
            state[key] = i + 1
            return PS[lo + i % n]

        def ring_next():
            r = RING[state["ring"] % NRING]
            state["ring"] += 1
            return r

        def sv(i, dt, off_b, shape):
            n = 1
            for s_ in shape:
                n *= s_
            esz = 4 if dt in (F32, I32) else 2
            nb = n * esz
            assert off_b + nb <= SLOT_B, (i, off_b, nb)
            a = SLt[i][:, off_b // 2: (off_b + nb) // 2]
            if dt != BF16:
                a = a.bitcast(dt)
            if len(shape) == 2:
                a = a.rearrange("p (a b) -> p a b", a=shape[0])
            elif len(shape) == 3:
                a = a.rearrange("p (a b c) -> p a b c", a=shape[0], b=shape[1])
            q0, q1 = off_b // 2048, (off_b + nb - 1) // 2048
            return V(a, [SLq[i][j] for j in range(q0, q1 + 1)])

        def rv(rbuf, shape):
            n = 1
            for s_ in shape:
                n *= s_
            a = rbuf.t[:, 0:n]
            if len(shape) == 2:
                a = a.rearrange("p (a b) -> p a b", a=shape[0])
            elif len(shape) == 3:
                a = a.rearrange("p (a b c) -> p a b c", a=shape[0], b=shape[1])
            return a

        ident = CB.t[:, 0:128]
        ones = CB.t[:, 128:256]
        mcur = CB.t[:, 256:768].rearrange("p (h q) -> p h q", h=4)
        mprev = CB.t[:, 768:1280].rearrange("p (h q) -> p h q", h=4)
        INVC = CF.t[:, 0:64].rearrange("p (a b) -> p a b", a=4)
        ROWM = CF.t[:, 64:68]
        EVENM = CF.t[:, 68:69]
        ODDM = CF.t[:, 69:70]

        def act(out, in_, func, reads, writes=(), pw=(), bias=None, scale=None):
            kw = {}
            if bias is not None:
                kw["bias"] = bias
            if scale is not None:
                kw["scale"] = scale
            S.op("act", lambda e: e.activation(out, in_, func, **kw), reads=reads, writes=writes, pwrites=pw)

        def tt(eng, out, in0, in1, op, reads, writes=(), pw=()):
            S.op(eng, lambda e: e.tensor_tensor(out, in0, in1, op), reads=reads, writes=writes, pwrites=pw)

        def ts(eng, out, in0, s1, s2, op0, op1, reads, writes=(), pw=()):
            if s2 is None:
                S.op(eng, lambda e: e.tensor_scalar(out, in0, s1, None, op0), reads=reads, writes=writes, pwrites=pw)
            else:
                S.op(eng, lambda e: e.tensor_scalar(out, in0, s1, s2, op0, op1), reads=reads, writes=writes, pwrites=pw)

        def stt(out, in0, sc, in1, op0, op1, reads, writes=(), pw=()):
            S.op("dve", lambda e: e.scalar_tensor_tensor(out, in0, sc, in1, op0, op1), reads=reads, writes=writes, pwrites=pw)

        def cp(eng, out, in_, reads, writes=(), pw=()):
            if eng == "act":
                S.op("act", lambda e: e.activation(out, in_, AF.Copy), reads=reads, writes=writes, pwrites=pw)
            else:
                S.op(eng, lambda e: e.tensor_copy(out, in_), reads=reads, writes=writes, pwrites=pw)

        def memset(eng, ap, val, writes=(), pw=()):
            S.op(eng, lambda e: e.memset(ap, val), writes=writes, pwrites=pw)

        def wload(dst_buf, dst_ap, src_ap, partial=False):
            if partial:
                S.dma("pool", dst_ap, src_ap, pwrites=[dst_buf])
            else:
                S.dma("pool", dst_ap, src_ap, writes=[dst_buf])

        def kview(w2d):
            return w2d.rearrange("(k p) c -> p k c", p=128)

        def xt(k, c0, c1):
            return XTt[:, k, c0:c1]

        S.dma("sp", CB.t[:, :], c_bf_d[:, :], writes=[CB])
        S.dma("sp", CF.t[:, :], c_f_d[:, :], writes=[CF])
        memset("dve", EPS.t[:, :], LN_EPS, writes=[EPS])
        memset("dve", SSMC.t[:, :, :, :], 0.0, writes=[SSMC])
        memset("dve", UPC.t[:, :, :, :], 0.0, writes=[UPC])
        memset("dve", KTC.t[:, :, :, :], 0.0, writes=[KTC])
        memset("dve", VC.t[:, :, :], 0.0, writes=[VC])
        NCD = dict(allow_slow_non_contiguous=True)
        for l in range(depth):
            S.dma("sp", BG.t[:, l, :], b_gate_d[l].rearrange("(c p) -> p c", p=128), pwrites=[BG], **NCD)
            S.dma("sp", PSC.t[:, l, :], pool_sc_d[l].rearrange("(c p) -> p c", p=128), pwrites=[PSC], **NCD)
            S.dma("sp", ESK.t[:, l, :], sinks_d[l].partition_broadcast(64), pwrites=[ESK], **NCD)
        act(ESK.t[:, :, :], ESK.t[:, :, :], AF.Exp, reads=[ESK], writes=[ESK])

        for mt in range(2):
            mf = sv(0, F32, 0, [D])
            S.dma("sp", mf.ap, mem_d[mt * 128:(mt + 1) * 128, :], writes=mf.b)
            mb = sv(1, BF16, 0, [D])
            cp("act", mb.ap, mf.ap, reads=mf.b, writes=mb.b)
            p = ps_next()
            pb = p.t[:, :].bitcast(BF16)
            for kc in range(8):
                S.op("pe", lambda e, kc=kc, pb=pb, mb=mb: e.transpose(pb[:, kc * 128:(kc + 1) * 128], mb.ap[:, kc * 128:(kc + 1) * 128], ident),
                     reads=mb.b + [CB] if kc == 0 else (), writes=[p] if kc == 0 else (), inc=(kc == 7))
            cp("dve", MEMT.t[:, :, mt * 128:(mt + 1) * 128], pb.rearrange("p (k t) -> p k t", k=8), reads=[p], pw=[MEMT])

        def ssm_prologue(l):
            PAv = sv(0, F32, 0, [2048])
            PA = PAv.b
            pa = PAv.ap

            def pv(i):
                return pa[:, i * 16:(i + 1) * 16]
            are, aim, dtv, th, rr, cs, sn, tA, fre, fim, nre, inv, tB = [pv(i) for i in range(13)]
            tI = pa[:, 13 * 16:14 * 16].bitcast(I32)
            dcol = pa[:, 14 * 16:14 * 16 + 4]
            S.dma("sp", are, a_re_d[l].rearrange("(q g) n -> (g n) q", g=2), writes=PA, **NCD)
            S.dma("sp", aim, a_im_d[l].rearrange("(q g) n -> (g n) q", g=2), pwrites=PA, **NCD)
            for g2 in range(2):
                S.dma("sp", dtv[g2 * 64:(g2 + 1) * 64, :],
                      ldt_d[l].rearrange("(q g) -> g q", g=2)[g2].partition_broadcast(64), pwrites=PA, **NCD)
            S.dma("sp", dcol, ssm_d_d[l].rearrange("(j p) -> p j", p=128), pwrites=PA, **NCD)

            def d(fn_out, *a, **k):
                pass
            act(dtv, dtv, AF.Exp, reads=PA, writes=PA)
            tt("dve", rr, dtv, are, ALU.mult, reads=PA, writes=PA)
            act(rr, rr, AF.Exp, reads=PA, writes=PA)
            cp("dve", SSMR.t[:, l, :], rr, reads=PA, pw=[SSMR])
            tt("dve", th, dtv, aim, ALU.mult, reads=PA, writes=PA)
            for dst, shift in ((sn, 0.0), (cs, 0.25)):
                ts("dve", tA, th, 1.0 / TWO_PI, shift, ALU.mult, ALU.add, reads=PA, writes=PA)
                cp("dve", tI, tA, reads=PA, writes=PA)
                cp("dve", dst, tI, reads=PA, writes=PA)
                tt("dve", tA, tA, dst, ALU.subtract, reads=PA, writes=PA)
                act(dst, tA, AF.Sin, reads=PA, writes=PA, scale=TWO_PI)
            tt("dve", nre, rr, cs, ALU.mult, reads=PA, writes=PA)
            ts("dve", nre, nre, -1.0, None, ALU.add, None, reads=PA, writes=PA)
            tt("dve", tA, rr, sn, ALU.mult, reads=PA, writes=PA)
            tt("dve", inv, are, are, ALU.mult, reads=PA, writes=PA)
            tt("dve", tB, aim, aim, ALU.mult, reads=PA, writes=PA)
            tt("dve", inv, inv, tB, ALU.add, reads=PA, writes=PA)
            S.op("dve", lambda e: e.reciprocal(inv, inv), reads=PA, writes=PA)
            tt("dve", fre, nre, are, ALU.mult, reads=PA, writes=PA)
            tt("dve", tB, tA, aim, ALU.mult, reads=PA, writes=PA)
            tt("dve", fre, fre, tB, ALU.add, reads=PA, writes=PA)
            tt("dve", fre, fre, inv, ALU.mult, reads=PA, writes=PA)
            tt("dve", fim, tA, are, ALU.mult, reads=PA, writes=PA)
            tt("dve", tB, nre, aim, ALU.mult, reads=PA, writes=PA)
            tt("dve", fim, fim, tB, ALU.subtract, reads=PA, writes=PA)
            tt("dve", fim, fim, inv, ALU.mult, reads=PA, writes=PA)
            STG = [sv(6, BF16, 0, [8, 4, 128]), sv(7, BF16, 0, [8, 4, 128])]
            for s_ in STG:
                memset("dve", s_.ap, 0.0, writes=s_.b)
            bre = sv(1, F32, 0, [16, 16]); bim = sv(1, F32, 1024, [16, 16])
            bbr = sv(2, F32, 0, [16, 16]); bbi = sv(2, F32, 1024, [16, 16]); btm = sv(2, F32, 2048, [16, 16])
            S.dma("sp", bre.ap, b_re_d[l].rearrange("(q g) n p -> (g n) q p", g=2), writes=bre.b)
            S.dma("sp", bim.ap, b_im_d[l].rearrange("(q g) n p -> (g n) q p", g=2), pwrites=bim.b)
            freb = fre.unsqueeze(2).to_broadcast([128, 16, 16])
            fimb = fim.unsqueeze(2).to_broadcast([128, 16, 16])
            R_ = PA + bre.b
            BBb = [SLq[2][0], SLq[2][1]]
            tt("dve", bbr.ap, bre.ap, freb, ALU.mult, reads=R_, writes=BBb)
            tt("dve", btm.ap, bim.ap, fimb, ALU.mult, reads=R_, writes=BBb)
            tt("dve", bbr.ap, bbr.ap, btm.ap, ALU.subtract, reads=BBb, writes=BBb)
            tt("dve", bbi.ap, bim.ap, freb, ALU.mult, reads=R_, writes=BBb)
            tt("dve", btm.ap, bre.ap, fimb, ALU.mult, reads=R_, writes=BBb)
            tt("dve", bbi.ap, bbi.ap, btm.ap, ALU.add, reads=BBb, writes=BBb)
            SRC = sv(3, BF16, 0, [128])
            for comp, bsrc in ((0, bbr), (1, bbi)):
                for j in range(4):
                    memset("dve", SRC.ap, 0.0, writes=SRC.b)
                    srcv = SRC.ap.rearrange("p (qq g p2) -> p qq g p2", qq=4, g=2)
                    for g2 in range(2):
                        cp("dve", srcv[g2 * 64:(g2 + 1) * 64, :, g2, :], bsrc.ap[g2 * 64:(g2 + 1) * 64, 4 * j:4 * j + 4, :],
                           reads=BBb, pw=SRC.b)
                    p = ps_next()
                    pb = p.t[:, :].bitcast(BF16)
                    S.op("pe", lambda e, pb=pb: e.transpose(pb[:, 0:128], SRC.ap, ident), reads=SRC.b + [CB], writes=[p])
                    for qq in range(4):
                        q = 4 * j + qq
                        s_ = STG[q // 8]
                        ts("dve", s_.ap[:, q % 8, comp, :], pb[:, 0:128], ROWM[:, qq:qq + 1], None, ALU.mult, None,
                           reads=[p, CF], pw=s_.b)
            cn = sv(1, F32, 0, [4, 64])
            for comp, csrc_d, sgn in ((2, c_re_d, 1.0), (3, c_im_d, -1.0)):
                S.dma("sp", cn.ap, csrc_d[l].rearrange("(j g) p n -> (g p) j n", g=8), writes=cn.b)
                for j in range(4):
                    ts("dve", SRC.ap[:, 0:64], cn.ap[:, j, :], EVENM, sgn, ALU.mult, ALU.mult, reads=cn.b + [CF], writes=SRC.b)
                    ts("dve", SRC.ap[:, 64:128], cn.ap[:, j, :], ODDM, sgn, ALU.mult, ALU.mult, reads=cn.b + [CF], pw=SRC.b)
                    p = ps_next()
                    pb = p.t[:, :].bitcast(BF16)
                    S.op("pe", lambda e, pb=pb: e.transpose(pb[:, 0:128], SRC.ap, ident), reads=SRC.b + [CB], writes=[p])
                    for qq in range(4):
                        q = 4 * j + qq
                        s_ = STG[q // 8]
                        cp("dve", s_.ap[:, q % 8, comp, qq * 32:(qq + 1) * 32], pb[:, qq * 32:(qq + 1) * 32], reads=[p], pw=s_.b)
            for hf in range(2):
                S.dma("sp", ssmw_d[l, hf * 8:(hf + 1) * 8].rearrange("q p c m -> p q c m"), STG[hf].ap, reads=STG[hf].b, pwrites=[B_ssmw])
            dk = sv(3, BF16, 2048, [4, 128])
            for j in range(4):
                ts("dve", dk.ap[:, j, :], ident, dcol[:, j:j + 1], None, ALU.mult, None, reads=[CB] + PA, pw=dk.b)
            S.dma("sp", ssmd_d[l], dk.ap, reads=dk.b, pwrites=[B_ssmd])
            smallb = [RING[1]]
            sm_ = rv(RING[1], [4, 16, 32]).bitcast(F32) if False else None
            tabs = RING[1].t[:, 0:4096].bitcast(F32).rearrange("p (c q b) -> p c q b", c=4, q=16)
            tbr, tbi, tar, tai = tabs[:, 0], tabs[:, 1], tabs[:, 2], tabs[:, 3]
            tmps = RING[2].t[:, 0:1024].bitcast(F32).rearrange("p (c q b) -> p c q b", c=2, q=16)
            tmpb = [RING[2]]

            def cmul_step(dr, di, ar, ai, br, bi, n):
                ta_, tb_ = tmps[:, 0, :, 0:n], tmps[:, 1, :, 0:n]
                tt("dve", ta_, ar, br, ALU.mult, reads=smallb, writes=tmpb)
                tt("dve", tb_, ai, bi, ALU.mult, reads=smallb, writes=tmpb)
                tt("dve", dr, ta_, tb_, ALU.subtract, reads=tmpb, writes=smallb)
                tt("dve", ta_, ar, bi, ALU.mult, reads=smallb, writes=tmpb)
                tt("dve", tb_, ai, br, ALU.mult, reads=smallb, writes=tmpb)
                tt("dve", di, ta_, tb_, ALU.add, reads=tmpb, writes=smallb)

            cp("dve", tbr[:, :, 0:1], cs.unsqueeze(2), reads=PA, writes=smallb)
            cp("dve", tbi[:, :, 0:1], sn.unsqueeze(2), reads=PA, writes=smallb)
            m = 1
            while m < 32:
                cmul_step(tbr[:, :, m:2 * m], tbi[:, :, m:2 * m], tbr[:, :, 0:m], tbi[:, :, 0:m],
                          tbr[:, :, m - 1:m].to_broadcast([128, 16, m]), tbi[:, :, m - 1:m].to_broadcast([128, 16, m]), m)
                m *= 2
            memset("dve", tar[:, :, 0:1], 1.0, writes=smallb)
            memset("dve", tai[:, :, 0:1], 0.0, writes=smallb)
            cp("dve", tar[:, :, 1:2], tbr[:, :, 31:32], reads=smallb, writes=smallb)
            cp("dve", tai[:, :, 1:2], tbi[:, :, 31:32], reads=smallb, writes=smallb)
            m = 1
            while m < 31:
                n = min(m, 31 - m)
                cmul_step(tar[:, :, 1 + m:1 + m + n], tai[:, :, 1 + m:1 + m + n], tar[:, :, 1:1 + n], tai[:, :, 1:1 + n],
                          tar[:, :, m:m + 1].to_broadcast([128, 16, n]), tai[:, :, m:m + 1].to_broadcast([128, 16, n]), n)
                m *= 2
            for qb in range(8):
                par = qb % 2
                tr = sv(8, F32, 0, [2, G]) if par == 0 else sv(4, F32, 0, [2, G])
                ti = sv(9, F32, 0, [2, G]) if par == 0 else sv(5, F32, 0, [2, G])
                tm1 = sv(10, F32, 0, [2, G])
                q0 = 2 * qb
                tr4 = tr.ap.rearrange("p q (a b) -> p q a b", a=32)
                ti4 = ti.ap.rearrange("p q (a b) -> p q a b", a=32)
                tm4 = tm1.ap.rearrange("p q (a b) -> p q a b", a=32)
                Ar = tar[:, q0:q0 + 2, :].unsqueeze(3).to_broadcast([128, 2, 32, 32])
                Ai = tai[:, q0:q0 + 2, :].unsqueeze(3).to_broadcast([128, 2, 32, 32])
                Br = tbr[:, q0:q0 + 2, :].unsqueeze(2).to_broadcast([128, 2, 32, 32])
                Bi = tbi[:, q0:q0 + 2, :].unsqueeze(2).to_broadcast([128, 2, 32, 32])
                tt("dve", tr4, Ar, Br, ALU.mult, reads=smallb, writes=tr.b)
                tt("dve", tm4, Ai, Bi, ALU.mult, reads=smallb, writes=tm1.b)
                tt("dve", tr4, tr4, tm4, ALU.subtract, reads=tr.b + tm1.b, writes=tr.b)
                tt("dve", ti4, Ar, Bi, ALU.mult, reads=smallb, writes=ti.b)
                tt("dve", tm4, Ai, Br, ALU.mult, reads=smallb, writes=tm1.b)
                tt("dve", ti4, ti4, tm4, ALU.add, reads=ti.b + tm1.b, writes=ti.b)
                cp("dve", TL.t[:, l, q0:q0 + 2, 0:1], tr.ap[:, :, G - 1:G], reads=tr.b, pw=[TL])
                cp("dve", TL.t[:, l, q0:q0 + 2, 1:2], ti.ap[:, :, G - 1:G], reads=ti.b, pw=[TL])
                for qq in range(2):
                    S.dma("sp", ssmt_d[l, q0 + qq, :, 0, :], tr.ap[:, qq, :], reads=tr.b, pwrites=[B_ssmt])
                    S.dma("sp", ssmt_d[l, q0 + qq, :, 1, :], ti.ap[:, qq, :], reads=ti.b, pwrites=[B_ssmt])

        if phases is None or "pro" in phases:
            for l in range(depth):
                ssm_prologue(l)

        def dbg_dump(name, ap, bufs):
            if dbg and name in dbg_d:
                S.dma("sp", dbg_d[name], ap, reads=bufs, pwrites=[B_dbg])

        def make_xt(t):
            xb = sv(9, BF16, (t % 4) * 2048, [D])
            cp("act", xb.ap, X[t].t[:, :], reads=[X[t]], writes=xb.b)
            p = ps_next()
            pb = p.t[:, :].bitcast(BF16)
            for kc in range(8):
                S.op("pe", lambda e, kc=kc, pb=pb, xb=xb: e.transpose(pb[:, kc * 128:(kc + 1) * 128], xb.ap[:, kc * 128:(kc + 1) * 128], ident),
                     reads=xb.b + [CB] if kc == 0 else (), writes=[p] if kc == 0 else (), inc=(kc == 7))
            cp("act", XTt[:, :, t * 128:(t + 1) * 128], pb.rearrange("p (k t) -> p k t", k=8), reads=[p], pw=[XTb[t // 4]])

        def layernorm(li, l, g, last):
            S.dma("sp", LNG.t[:, 0, :], ln_g_d[li][l].partition_broadcast(128), writes=[LNG])
            S.dma("sp", LNG.t[:, 1, :], ln_b_d[li][l].partition_broadcast(128), pwrites=[LNG])
            smv = SM.t[:, 0:48].rearrange("p (a b c) -> p a b c", a=4, b=2)
            mv = SM2.t[:, 0:8].rearrange("p (a b) -> p a b", a=4)
            sd = SM2.t[:, 8:12]
            rs = SM2.t[:, 12:16]
            for hf in range(2):
                for t4 in range(4):
                    t = hf * 4 + t4
                    for c in range(2):
                        S.op("dve", lambda e, t=t, t4=t4, c=c: e.bn_stats(smv[:, t4, c, :], X[t].t[:, c * 512:(c + 1) * 512]),
                             reads=[X[t]], writes=[SM] if (t4 == 0 and c == 0) else (), pwrites=() if (t4 == 0 and c == 0) else [SM])
                    S.op("dve", lambda e, t4=t4: e.bn_aggr(mv[:, t4, :], smv[:, t4, :, :]), reads=[SM], writes=[SM2] if t4 == 0 else (), pwrites=() if t4 == 0 else [SM2])
                act(sd, mv[:, :, 1], AF.Sqrt, reads=[SM2, EPS], pw=[SM2], bias=EPS.t[:, 0:1])
                S.op("dve", lambda e: e.reciprocal(rs, sd), reads=[SM2], pwrites=[SM2])
                for t4 in range(4):
                    t = hf * 4 + t4
                    xa = X[t].t[:, :]
                    stt(xa, xa, mv[:, t4, 0:1], LNG.t[:, 0, :], ALU.subtract, ALU.mult, reads=[X[t], SM2, LNG], writes=[X[t]])
                    stt(xa, xa, rs[:, t4:t4 + 1], LNG.t[:, 1, :], ALU.mult, ALU.add, reads=[X[t], SM2, LNG], writes=[X[t]])
                    if last:
                        S.dma("sp", out_d[g * G + t * 128: g * G + (t + 1) * 128, :], xa, reads=[X[t]], pwrites=[B_out])
                    else:
                        make_xt(t)
                    if dbg and g == 0 and l == 0:
                        S.dma("sp", dbg_d[f"d_x{li + 1}"][t * 128:(t + 1) * 128, :], xa, reads=[X[t]], pwrites=[B_dbg])

        def out_proj(src_of_k, src_bufs, w_d2, l, li, g, last, filler=None):
            WU = []
            for hf in range(2):
                r = ring_next()
                wload(r, rv(r, [8, 512]), kview(w_d2)[:, :, hf * 512:(hf + 1) * 512])
                WU.append(r)
            for t in range(NT):
                for hf in range(2):
                    p = ps_next()
                    w = rv(WU[hf], [8, 512])
                    S.mm(p, p.t[:, :], [(src_of_k(k)[:, t * 128:(t + 1) * 128], w[:, k, :]) for k in range(8)],
                         reads=src_bufs + [WU[hf]])
                    xa = X[t].t[:, hf * 512:(hf + 1) * 512]
                    stt(xa, xa, ALPHA, p.t[:, :], ALU.mult, ALU.add, reads=[X[t], p], pw=[X[t]])
            if filler is not None:
                filler()
            layernorm(li, l, g, last)

        def phase_attn(g, l):
            Wq = ring_next()
            wq = rv(Wq, [8, 512])
            wload(Wq, wq, kview(w_in_d[l])[:, :, C_Q:C_Q + 512])
            Wkv = ring_next()
            wkv = rv(Wkv, [8, 256])
            wload(Wkv, wkv, kview(w_in_d[l])[:, :, C_K:C_K + 256])
            rope = sv(5, F32, 0, [2, G])
            S.dma("sp", rope.ap[0:32], c_rope_d[:, :, g * G:(g + 1) * G], writes=rope.b)
            wrot = sv(6, BF16, 0, [8, 10, 32])
            memset("dve", wrot.ap, 0.0, writes=wrot.b)
            wq4 = wq.rearrange("p k (h d) -> p k h d", h=8)
            wk4 = wkv[:, :, 0:128].rearrange("p k (h d) -> p k h d", h=2)
            cp("dve", wrot.ap[:, :, 0:8, 0:8], wq4[:, :, :, 8:16], reads=[Wq], pw=wrot.b)
            cp("dve", wrot.ap[:, :, 0:8, 8:16], wq4[:, :, :, 0:8], reads=[Wq], pw=wrot.b)
            cp("dve", wrot.ap[:, :, 8:10, 0:8], wk4[:, :, :, 8:16], reads=[Wkv], pw=wrot.b)
            cp("dve", wrot.ap[:, :, 8:10, 8:16], wk4[:, :, :, 0:8], reads=[Wkv], pw=wrot.b)
            QT = [sv(0, BF16, 0, [4, G]), sv(1, BF16, 0, [4, G])]
            AOT = [sv(2, BF16, 0, [4, G]), sv(3, BF16, 0, [4, G])]
            KT = sv(4, BF16, 0, [2, 1152])
            Vv = sv(4, BF16, 4608, [9, 128])
            cp("dve", KT.ap[0:64, :, 0:128], KTC.t[:, l, :, :], reads=[KTC], writes=KT.b)
            cp("dve", Vv.ap[:, 0, :], VC.t[:, l, :], reads=[VC], pw=Vv.b)
            for h in range(10):
                for s_ in range(2):
                    pq = ps_next()
                    pr = ps_next()
                    if h < 8:
                        lq = [wq[:, k, h * 64:(h + 1) * 64] for k in range(8)]
                        wb = Wq
                    else:
                        lq = [wkv[:, k, (h - 8) * 64:(h - 7) * 64] for k in range(8)]
                        wb = Wkv
                    S.mm(pq, pq.t[0:64, :], [(lq[k], xt(k, s_ * 512, (s_ + 1) * 512)) for k in range(8)], reads=[wb, XTb[s_]])
                    S.mm(pr, pr.t[0:32, :], [(wrot.ap[:, k, h, :], xt(k, s_ * 512, (s_ + 1) * 512)) for k in range(8)],
                         reads=wrot.b + [XTb[s_]])
                    par = (h * 2 + s_) % 2
                    T1 = sv(7, F32, par * 4096, [512])
                    T2 = sv(7, F32, par * 4096 + 2048, [512])
                    cs_ = rope.ap[0:32, 0, s_ * 512:(s_ + 1) * 512]
                    sn_ = rope.ap[0:32, 1, s_ * 512:(s_ + 1) * 512]
                    tt("dve", T1.ap[0:32], pq.t[0:32, :], cs_, ALU.mult, reads=[pq] + rope.b, writes=T1.b)
                    tt("dve", T2.ap[0:32], pr.t[0:32, :], sn_, ALU.mult, reads=[pr] + rope.b, writes=T2.b)
                    if h < 8:
                        dst = QT[h // 4].ap[:, h % 4, s_ * 512:(s_ + 1) * 512]
                        db = [QT[h // 4].b[h % 4]]
                    else:
                        dst = KT.ap[:, h - 8, 128 + s_ * 512:128 + (s_ + 1) * 512]
                        db = KT.b
                    tt("dve", dst[0:32], T1.ap[0:32], T2.ap[0:32], ALU.add, reads=T1.b + T2.b, pw=db)
                    cp("act", dst[32:64], pq.t[32:64, :], reads=[pq], pw=db)
            for t in range(NT):
                pv_ = ps_next()
                S.mm(pv_, pv_.t[:, 0:128], [(xt(k, t * 128, (t + 1) * 128), wkv[:, k, 128:256]) for k in range(8)],
                     reads=[XTb[t // 4], Wkv])
                cp("act", Vv.ap[:, 1 + t, :], pv_.t[:, 0:128], reads=[pv_], pw=Vv.b)
            cp("dve", KTC.t[:, l, :, :], KT.ap[0:64, :, G:G + 128], reads=KT.b, writes=[KTC])
            cp("dve", VC.t[:, l, :], Vv.ap[:, NT, :], reads=Vv.b, writes=[VC])
            it = 0
            for i in range(NT):
                for kv in range(2):
                    has_prev = not (g == 0 and i == 0)
                    qr = QT[kv].ap[0:64, :, i * 128:(i + 1) * 128]
                    PTc = sv(8, BF16, (it % 2) * 2048, [512])
                    PTp = sv(8, BF16, (it % 2) * 2048 + 1024, [512])
                    d1 = sv(8, F32, 4096 + (it % 2) * 2048, [512])
                    it += 1
                    pc = ps_next()
                    o3 = pc.t[:, :].rearrange("p (h q) -> p h q", h=4)
                    S.mm(pc, o3, [(KT.ap[0:64, kv, 128 + i * 128:128 + (i + 1) * 128], qr), (ident, mcur)],
                         reads=KT.b + QT[kv].b + [CB])
                    act(PTc.ap, pc.t[:, :], AF.Exp, reads=[pc], writes=PTc.b, scale=0.125)
                    if has_prev:
                        pp = ps_next()
                        o3p = pp.t[:, :].rearrange("p (h q) -> p h q", h=4)
                        S.mm(pp, o3p, [(KT.ap[0:64, kv, i * 128:(i + 1) * 128], qr), (ident, mprev)],
                             reads=KT.b + QT[kv].b + [CB])
                        act(PTp.ap, pp.t[:, :], AF.Exp, reads=[pp], pw=PTp.b, scale=0.125)
                    po = ps_next()
                    pd = ps_next()
                    tv = [(Vv.ap[:, 1 + i, kv * 64:(kv + 1) * 64], PTc.ap)]
                    td = [(ones[:, 0:64], PTc.ap)]
                    if has_prev:
                        tv.append((Vv.ap[:, i, kv * 64:(kv + 1) * 64], PTp.ap))
                        td.append((ones[:, 0:64], PTp.ap))
                    S.mm(po, po.t[0:64, :], tv, reads=Vv.b + PTc.b)
                    S.mm(pd, pd.t[0:64, :], td, reads=PTc.b + [CB])
                    esk = ESK.t[:, l, 4 * kv:4 * kv + 4].unsqueeze(2).to_broadcast([64, 4, 128])
                    d13 = d1.ap[0:64].rearrange("p (h q) -> p h q", h=4)
                    tt("dve", d13, pd.t[0:64, :].rearrange("p (h q) -> p h q", h=4), esk, ALU.add, reads=[pd, ESK], writes=d1.b)
                    S.op("dve", lambda e, d13=d13: e.reciprocal(d13, d13), reads=d1.b, writes=d1.b)
                    tt("dve", AOT[kv].ap[0:64, :, i * 128:(i + 1) * 128], po.t[0:64, :].rearrange("p (h q) -> p h q", h=4), d13,
                       ALU.mult, reads=[po] + d1.b, pw=AOT[kv].b)
            if g == 0 and l == 0:
                dbg_dump("d_aot0", SLt[2][:, :], AOT[0].b)
                dbg_dump("d_aot1", SLt[3][:, :], AOT[1].b)
            return AOT

        def phase_ssm(g, l):
            Wss = ring_next()
            wss = rv(Wss, [8, 512])
            wload(Wss, wss, kview(w_in_d[l])[:, :, C_SSM:C_SSM + 512])
            UT = sv(0, BF16, 0, [4, G])
            YG = UT
            GLUT = sv(10, BF16, 0, [4, G])
            dsk = sv(6, BF16, 4096, [4, 128])
            S.dma("sp", dsk.ap, ssmd_d[l], reads=[B_ssmd], writes=dsk.b)
            for j in range(4):
                for s_ in range(2):
                    p = ps_next()
                    S.mm(p, p.t[:, :], [(wss[:, k, j * 128:(j + 1) * 128], xt(k, s_ * 512, (s_ + 1) * 512)) for k in range(8)],
                         reads=[Wss, XTb[s_]])
                    cp("act", UT.ap[:, j, s_ * 512:(s_ + 1) * 512], p.t[:, :], reads=[p], pw=[UT.b[j]])
            t1 = sv(7, F32, 0, [G]); t2 = sv(7, F32, 4096, [G])
            t3 = sv(8, F32, 0, [G]); t4 = sv(8, F32, 4096, [G])
            gre = sv(9, F32, 0, [G]); gim = sv(9, F32, 4096, [G])
            pr = [sv(1, BF16, i * 2048, [G]) for i in range(4)]
            if g > 0:
                ca = SM.t[:, 0:16]
                cb = SM.t[:, 16:32]
                glr, gli = GL.t[:, l, :, 0], GL.t[:, l, :, 1]
                tlc, tls = TL.t[:, l, :, 0], TL.t[:, l, :, 1]
                RB_ = [GL, TL]
                tt("dve", ca, glr, tlc, ALU.mult, reads=RB_, writes=[SM])
                tt("dve", cb, gli, tls, ALU.mult, reads=RB_, pw=[SM])
                tt("dve", SSMC.t[:, l, :, 0], ca, cb, ALU.subtract, reads=[SM], writes=[SSMC])
                tt("dve", ca, glr, tls, ALU.mult, reads=RB_ + [SSMC], writes=[SM])
                tt("dve", cb, gli, tlc, ALU.mult, reads=RB_, pw=[SM])
                tt("dve", SSMC.t[:, l, :, 1], ca, cb, ALU.add, reads=[SM], pw=[SSMC])
            BRb = [PS[4], PS[5]]
            BIb = [PS[6], PS[7]]
            bre_ps = PSD[2][:, :]
            bim_ps = PSD[3][:, :]
            for j in range(4):
                yp = [PS[(j % 2) * 2], PS[(j % 2) * 2 + 1]]
                for s_ in range(2):
                    S.op("pe", lambda e, j=j, s_=s_, yp=yp: e.matmul(yp[s_].t[:, :], dsk.ap[:, j, :], UT.ap[:, j, s_ * 512:(s_ + 1) * 512], start=True, stop=False),
                         reads=dsk.b + [UT.b[j]], writes=[yp[s_]], inc=False)
                for qq in range(4):
                    q = 4 * j + qq
                    tb = sv(4 + q % 2, F32, 0, [2, G])
                    tbw = sv(6, BF16, (q % 2) * 2048, [4, 128])
                    S.dma("sp", tb.ap, ssmt_d[l, q], reads=[B_ssmt], writes=tb.b)
                    S.dma("sp", tbw.ap, ssmw_d[l, q], reads=[B_ssmw], writes=tbw.b)
                    rb = SSMR.t[:, l, q:q + 1].to_broadcast([128, G])
                    for s_ in range(2):
                        c0, c1 = s_ * 512, (s_ + 1) * 512
                        S.mm(BRb[s_], BRb[s_].t[:, :], [(tbw.ap[:, 0, :], UT.ap[:, j, c0:c1])], reads=tbw.b + [UT.b[j]])
                        S.mm(BIb[s_], BIb[s_].t[:, :], [(tbw.ap[:, 1, :], UT.ap[:, j, c0:c1])], reads=tbw.b + [UT.b[j]])
                    Tc = tb.ap[:, 0, :]
                    Ts = tb.ap[:, 1, :]
                    tt("dve", t1.ap, bre_ps, Tc, ALU.mult, reads=BRb + tb.b, writes=t1.b)
                    tt("dve", t2.ap, bim_ps, Ts, ALU.mult, reads=BIb + tb.b, writes=t2.b)
                    tt("dve", t3.ap, bim_ps, Tc, ALU.mult, reads=BIb + tb.b, writes=t3.b)
                    tt("dve", t4.ap, bre_ps, Ts, ALU.mult, reads=BRb + tb.b, writes=t4.b)
                    tt("dve", t1.ap, t1.ap, t2.ap, ALU.add, reads=t1.b + t2.b, writes=t1.b)
                    tt("dve", t3.ap, t3.ap, t4.ap, ALU.subtract, reads=t3.b + t4.b, writes=t3.b)
                    S.op("dve", lambda e, rb=rb, q=q: e.tensor_tensor_scan(gre.ap, rb, t1.ap, SSMC.t[:, l, q, 0:1], ALU.mult, ALU.add),
                         reads=t1.b + [SSMR, SSMC], writes=gre.b)
                    S.op("dve", lambda e, rb=rb, q=q: e.tensor_tensor_scan(gim.ap, rb, t3.ap, SSMC.t[:, l, q, 1:2], ALU.mult, ALU.add),
                         reads=t3.b + [SSMR, SSMC], writes=gim.b)
                    GB = gre.b + gim.b
                    tt("dve", pr[0].ap, gre.ap, Tc, ALU.mult, reads=GB + tb.b, writes=pr[0].b)
                    stt(pr[1].ap, gim.ap, -1.0, Ts, ALU.mult, ALU.mult, reads=GB + tb.b, writes=pr[1].b)
                    tt("dve", pr[2].ap, gre.ap, Ts, ALU.mult, reads=GB + tb.b, writes=pr[2].b)
                    tt("dve", pr[3].ap, gim.ap, Tc, ALU.mult, reads=GB + tb.b, writes=pr[3].b)
                    lastq = qq == 3
                    for s_ in range(2):
                        c0, c1 = s_ * 512, (s_ + 1) * 512
                        for i in range(4):
                            fin = lastq and i == 3
                            S.op("pe", lambda e, s_=s_, yp=yp, tbw=tbw, i=i, c0=c0, c1=c1, fin=fin:
                                 e.matmul(yp[s_].t[:, :], tbw.ap[:, 2 + i // 2, :], pr[i].ap[:, c0:c1], start=False, stop=fin),
                                 reads=tbw.b + pr[i].b, pwrites=[yp[s_]], inc=(i == 3))
                    cp("act", GL.t[:, l, q, 0:1], gre.ap[:, G - 1:G], reads=gre.b, pw=[GL])
                    cp("act", GL.t[:, l, q, 1:2], gim.ap[:, G - 1:G], reads=gim.b, pw=[GL])
                for s_ in range(2):
                    act(YG.ap[:, j, s_ * 512:(s_ + 1) * 512], yp[s_].t[:, :], AF.Gelu_apprx_tanh, reads=[yp[s_]], pw=[YG.b[j]])
            Wgl = ring_next()
            wgl = rv(Wgl, [4, 1024])
            wload(Wgl, wgl, kview(w_glu_d[l]))
            for c in range(4):
                for s_ in range(2):
                    phv = ps_next()
                    phg = ps_next()
                    S.mm(phv, phv.t[:, :], [(wgl[:, k, c * 128:(c + 1) * 128], YG.ap[:, k, s_ * 512:(s_ + 1) * 512]) for k in range(4)],
                         reads=[Wgl] + YG.b)
                    S.mm(phg, phg.t[:, :], [(wgl[:, k, 512 + c * 128:512 + (c + 1) * 128], YG.ap[:, k, s_ * 512:(s_ + 1) * 512]) for k in range(4)],
                         reads=[Wgl] + YG.b)
                    sg = sv(7, F32, ((c * 2 + s_) % 2) * 2048, [512])
                    act(sg.ap, phg.t[:, :], AF.Sigmoid, reads=[phg], writes=sg.b)
                    tt("dve", GLUT.ap[:, c, s_ * 512:(s_ + 1) * 512], phv.t[:, :], sg.ap, ALU.mult, reads=[phv] + sg.b, pw=[GLUT.b[c]])
            if g == 0 and l == 0:
                dbg_dump("d_glut", SLt[10][:, :], GLUT.b)
            return GLUT

        def phase_pool(g, l):
            Wpo = ring_next()
            wpo = rv(Wpo, [8, 512])
            wload(Wpo, wpo, kview(w_in_d[l])[:, :, C_POOL:C_POOL + 512])
            Wpw = ring_next()
            wpw = rv(Wpw, [4, 128])
            wload(Wpw, wpw, pool_w_d[l].rearrange("g c d -> c g d"))
            PL = sv(5, BF16, 0, [4, G])
            PMT = sv(6, BF16, 0, [4, G])
            L = G + 16
            for j in range(4):
                UP = sv(0, F32, 0, [L])
                SA = sv(1, F32, 0, [L])
                SB_ = sv(4, F32, 0, [L])
                cp("dve", UP.ap[:, 0:16], UPC.t[:, l, j, :], reads=[UPC], writes=UP.b)
                for s_ in range(2):
                    p = ps_next()
                    S.mm(p, p.t[:, :], [(wpo[:, k, j * 128:(j + 1) * 128], xt(k, s_ * 512, (s_ + 1) * 512)) for k in range(8)],
                         reads=[Wpo, XTb[s_]])
                    cp("act", UP.ap[:, 16 + s_ * 512:16 + (s_ + 1) * 512], p.t[:, :], reads=[p], pw=UP.b)
                cp("dve", UPC.t[:, l, j, :], UP.ap[:, G:G + 16], reads=UP.b, writes=[UPC])
                cur, nxt, other = UP, SA, SB_
                for s in range(1, j + 2):
                    sh = 1 << (s - 1)
                    lo = (1 << s) - 1
                    tt("dve", nxt.ap[:, lo:L], cur.ap[:, lo:L], cur.ap[:, lo - sh:L - sh], ALU.add, reads=cur.b, writes=nxt.b)
                    cur, nxt = nxt, (other if nxt is SA else SA)
                w = 1 << (j + 1)
                stt(PL.ap[:, j, :], cur.ap[:, 16:L], 1.0 / w, UP.ap[:, 16:L], ALU.mult, ALU.subtract, reads=cur.b + UP.b, writes=[PL.b[j]])
                if g == 0:
                    tmp = SM.t[:, 48:64]
                    tt("dve", tmp, cur.ap[:, 16:32], INVC[:, j, :], ALU.mult, reads=cur.b + [CF], pw=[SM])
                    tt("dve", PL.ap[:, j, 0:16], tmp, UP.ap[:, 16:32], ALU.subtract, reads=[SM] + UP.b, pw=[PL.b[j]])
                for s_ in range(2):
                    pm = ps_next()
                    S.mm(pm, pm.t[:, :], [(wpw[:, j, :], PL.ap[:, j, s_ * 512:(s_ + 1) * 512])], reads=[Wpw, PL.b[j]])
                    act(PMT.ap[:, j, s_ * 512:(s_ + 1) * 512], pm.t[:, :], AF.Identity, reads=[pm, PSC], pw=[PMT.b[j]],
                        scale=PSC.t[:, l, j:j + 1])
            if g == 0 and l == 0:
                dbg_dump("d_pmt", SLt[6][:, :], PMT.b)
            return PMT

        def phase_merge(g, l, AOT, GLUT, PMT):
            WA = [sv(4, BF16, 0, [4, D]), sv(5, BF16, 0, [4, D])]
            for hf in range(2):
                S.dma("pool", WA[hf].ap[0:64], w_bra_d[l].rearrange("(h p) d -> p h d", p=64)[:, hf * 4:(hf + 1) * 4, :], writes=WA[hf].b)
            WSs = sv(9, BF16, 0, [4, D])
            S.dma("pool", WSs.ap, kview(w_brs_d[l]), writes=WSs.b)
            r = [ring_next() for _ in range(4)]
            wbp = rv(r[0], [4, D])
            wload(r[0], wbp, kview(w_brp_d[l]))
            MT = [sv(7, BF16, 0, [4, G]), sv(8, BF16, 0, [4, G])]
            for hf in range(2):
                gw = []
                for i in range(3):
                    gv = rv(r[1 + i], [8, 512])
                    c0 = C_GATE + i * D + hf * 512
                    wload(r[1 + i], gv, kview(w_in_d[l])[:, :, c0:c0 + 512])
                    gw.append(gv)
                for d4 in range(4):
                    dc = hf * 4 + d4
                    for s_ in range(2):
                        c0, c1 = s_ * 512, (s_ + 1) * 512
                        pg = [ps_next() for _ in range(3)]
                        po = [ps_next() for _ in range(3)]
                        for i in range(3):
                            S.mm(pg[i], pg[i].t[:, :], [(gw[i][:, k, d4 * 128:(d4 + 1) * 128], xt(k, c0, c1)) for k in range(8)],
                                 reads=[r[1 + i], XTb[s_]])
                        S.mm(po[0], po[0].t[:, :],
                             [(WA[h // 4].ap[0:64, h % 4, dc * 128:(dc + 1) * 128], AOT[h // 4].ap[0:64, h % 4, c0:c1]) for h in range(8)],
                             reads=WA[0].b + WA[1].b + AOT[0].b + AOT[1].b)
                        S.mm(po[1], po[1].t[:, :], [(WSs.ap[:, k, dc * 128:(dc + 1) * 128], GLUT.ap[:, k, c0:c1]) for k in range(4)],
                             reads=WSs.b + GLUT.b)
                        S.mm(po[2], po[2].t[:, :], [(wbp[:, k, dc * 128:(dc + 1) * 128], PMT.ap[:, k, c0:c1]) for k in range(4)],
                             reads=[r[0]] + PMT.b)
                        sg = [sv(0, F32, i * 2048, [512]) for i in range(3)]
                        for i in range(3):
                            act(sg[i].ap, pg[i].t[:, :], AF.Sigmoid, reads=[pg[i], BG], writes=sg[i].b,
                                bias=BG.t[:, l, i * 8 + dc:i * 8 + dc + 1])
                        m1 = sv(1, F32, 0, [512])
                        m2 = sv(1, F32, 2048, [512])
                        tt("dve", m1.ap, po[0].t[:, :], sg[0].ap, ALU.mult, reads=[po[0]] + sg[0].b, writes=m1.b)
                        tt("dve", m2.ap, po[1].t[:, :], sg[1].ap, ALU.mult, reads=[po[1]] + sg[1].b, writes=m2.b)
                        tt("dve", m1.ap, m1.ap, m2.ap, ALU.add, reads=m1.b + m2.b, writes=m1.b)
                        tt("dve", m2.ap, po[2].t[:, :], sg[2].ap, ALU.mult, reads=[po[2]] + sg[2].b, writes=m2.b)
                        tt("dve", MT[hf].ap[:, d4, c0:c1], m1.ap, m2.ap, ALU.add, reads=m1.b + m2.b, pw=[MT[hf].b[d4]])
            if g == 0 and l == 0:
                dbg_dump("d_mt0", SLt[7][:, :], MT[0].b)
                dbg_dump("d_mt1", SLt[8][:, :], MT[1].b)
            return MT

        def phase_xattn_kv(g, l):
            KKT = sv(0, BF16, 0, [8, NMEM])
            VV = sv(0, BF16, 4096, [2, D])
            WK = []
            for u in range(4):
                rr_ = ring_next()
                wload(rr_, rv(rr_, [8, 512]), kview(xa_wkv_d[l])[:, :, u * 512:(u + 1) * 512])
                WK.append(rr_)
            for c in range(8):
                p = ps_next()
                w = rv(WK[c // 4], [8, 512])
                S.mm(p, p.t[:, 0:NMEM], [(w[:, k, (c % 4) * 128:(c % 4 + 1) * 128], MEMT.t[:, k, :]) for k in range(8)],
                     reads=[WK[c // 4], MEMT])
                cp("act", KKT.ap[:, c, :], p.t[:, 0:NMEM], reads=[p], pw=KKT.b)
            for mt in range(2):
                for hf in range(2):
                    p = ps_next()
                    w = rv(WK[2 + hf], [8, 512])
                    S.mm(p, p.t[:, :], [(MEMT.t[:, k, mt * 128:(mt + 1) * 128], w[:, k, :]) for k in range(8)], reads=[WK[2 + hf], MEMT])
                    cp("act", VV.ap[:, mt, hf * 512:(hf + 1) * 512], p.t[:, :], reads=[p], pw=VV.b)

        def phase_xattn(g, l):
            KKT = sv(0, BF16, 0, [8, NMEM])
            VV = sv(0, BF16, 4096, [2, D])
            QXT = [sv(1, BF16, 0, [4, G]), sv(2, BF16, 0, [4, G])]
            OXT = [sv(4, BF16, 0, [4, G]), sv(5, BF16, 0, [4, G])]
            WQ = []
            for u in range(2):
                rr_ = ring_next()
                wload(rr_, rv(rr_, [8, 512]), kview(xa_wq_d[l])[:, :, u * 512:(u + 1) * 512])
                WQ.append(rr_)
            for c in range(8):
                for s_ in range(2):
                    p = ps_next()
                    w = rv(WQ[c // 4], [8, 512])
                    S.mm(p, p.t[:, :], [(w[:, k, (c % 4) * 128:(c % 4 + 1) * 128], xt(k, s_ * 512, (s_ + 1) * 512)) for k in range(8)],
                         reads=[WQ[c // 4], XTb[s_]])
                    cp("act", QXT[c // 4].ap[:, c % 4, s_ * 512:(s_ + 1) * 512], p.t[:, :], reads=[p], pw=[QXT[c // 4].b[c % 4]])
            it = 0
            for h in range(4):
                for s_ in range(2):
                    c0, c1 = s_ * 512, (s_ + 1) * 512
                    PT = [sv(3, BF16, (it % 2) * 2048 + mt * 1024, [512]) for mt in range(2)]
                    rden = sv(3, F32, 4096 + (it % 2) * 2048, [512])
                    it += 1
                    for mt in range(2):
                        p = ps_next()
                        S.mm(p, p.t[:, :],
                             [(KKT.ap[:, 2 * h + dcc, mt * 128:(mt + 1) * 128], QXT[(2 * h + dcc) // 4].ap[:, (2 * h + dcc) % 4, c0:c1]) for dcc in range(2)],
                             reads=KKT.b + QXT[(2 * h) // 4].b)
                        if mt == 0:
                            act(PT[mt].ap, p.t[:, :], AF.Exp, reads=[p], writes=PT[mt].b, scale=1.0 / 16.0)
                        else:
                            act(PT[mt].ap, p.t[:, :], AF.Exp, reads=[p], pw=PT[mt].b, scale=1.0 / 16.0)
                    pd = ps_next()
                    S.mm(pd, pd.t[:, :], [(ones, PT[mt].ap) for mt in range(2)], reads=PT[0].b + [CB])
                    S.op("dve", lambda e, rden=rden, pd=pd: e.reciprocal(rden.ap, pd.t[:, :]), reads=[pd], writes=rden.b)
                    for dcc in range(2):
                        po = ps_next()
                        c = 2 * h + dcc
                        S.mm(po, po.t[:, :], [(VV.ap[:, mt, c * 128:(c + 1) * 128], PT[mt].ap) for mt in range(2)], reads=VV.b + PT[0].b)
                        tt("dve", OXT[c // 4].ap[:, c % 4, c0:c1], po.t[:, :], rden.ap, ALU.mult, reads=[po] + rden.b, pw=[OXT[c // 4].b[c % 4]])
            return OXT

        def phase_ffn(g, l):
            moe = (l % 2 == 1)
            jj = l // 2
            for t in range(NT):
                act(X[t].t[:, :], X[t].t[:, :], AF.Identity, reads=[X[t]], writes=[X[t]], scale=ALPHA)
            if moe:
                Wr = ring_next()
                wr = rv(Wr, [8, NEXP])
                S.dma("pool", wr, moe_wr_d[jj].rearrange("(k p) e -> p k e", p=128), writes=[Wr], **NCD)
                S.dma("sp", BR.t[:, :], moe_br_d[jj].partition_broadcast(128), writes=[BR], **NCD)
                pl = ps_next()
                for t in range(NT):
                    S.mm(pl, pl.t[:, t * 8:(t + 1) * 8], [(xt(k, t * 128, (t + 1) * 128), wr[:, k, :]) for k in range(8)],
                         reads=[Wr, XTb[t // 4]])
                lg = RT.t[:, :, 0:8]
                eq = RT.t[:, :, 8:16]
                l2 = RT.t[:, :, 16:24]
                ex = RT.t[:, :, 24:32]
                m1 = RT.t[:, :, 32:33]
                m2 = RT.t[:, :, 33:34]
                dn = RT.t[:, :, 34:35]
                R_ = [RT]
                tt("dve", lg, pl.t[:, 0:64].rearrange("p (t e) -> p t e", t=NT), BR.t[:, :].unsqueeze(1).to_broadcast([128, NT, NEXP]),
                   ALU.add, reads=[pl, BR], writes=R_)
                S.op("dve", lambda e: e.tensor_reduce(m1, lg, AX.X, ALU.max), reads=R_, writes=R_)
                tt("dve", eq, lg, m1.to_broadcast([128, NT, NEXP]), ALU.is_equal, reads=R_, writes=R_)
                S.op("dve", lambda e: e.scalar_tensor_tensor(l2, eq, -1e30, lg, ALU.mult, ALU.add), reads=R_, writes=R_)
                S.op("dve", lambda e: e.tensor_reduce(m2, l2, AX.X, ALU.max), reads=R_, writes=R_)
                tt("dve", eq, lg, m2.to_broadcast([128, NT, NEXP]), ALU.is_ge, reads=R_, writes=R_)
                tt("dve", l2, lg, m1.to_broadcast([128, NT, NEXP]), ALU.subtract, reads=R_, writes=R_)
                act(ex, l2, AF.Exp, reads=R_, writes=R_)
                tt("dve", ex, ex, eq, ALU.mult, reads=R_, writes=R_)
                S.op("dve", lambda e: e.tensor_reduce(dn, ex, AX.X, ALU.add), reads=R_, writes=R_)
                S.op("dve", lambda e: e.reciprocal(dn, dn), reads=R_, writes=R_)
                tt("dve", CW.t[:, :, :], ex, dn.to_broadcast([128, NT, NEXP]), ALU.mult, reads=R_, writes=[CW])
                nexp, F, gu_of, dn_of = NEXP, DFFE, (lambda e_: moe_gu_d[jj, e_]), (lambda e_: moe_dn_d[jj, e_])
            else:
                nexp, F, gu_of, dn_of = 1, DFF, (lambda e_: ffn_gu_d[jj]), (lambda e_: ffn_dn_d[jj])
            nch = F // 128
            fgs = []
            c = 0
            while c < nch:
                n = min(4, nch - c)
                fgs.append((c, n))
                c += n
            fi = 0
            for e_ in range(nexp):
                gu = kview(gu_of(e_))
                dnw = dn_of(e_)
                for (c0, n) in fgs:
                    Wg = ring_next(); Wu = ring_next(); Wd = ring_next()
                    wg = rv(Wg, [8, n * 128]); wu = rv(Wu, [8, n * 128]); wd = rv(Wd, [n, D])
                    wload(Wg, wg, gu[:, :, c0 * 128:(c0 + n) * 128])
                    wload(Wu, wu, gu[:, :, F + c0 * 128:F + (c0 + n) * 128])
                    wload(Wd, wd, dnw[c0 * 128:(c0 + n) * 128, :].rearrange("(c p) d -> p c d", p=128))
                    HT = sv(fi % 2, BF16, 0, [4, G])
                    fi += 1
                    for s_ in range(2):
                        a0, a1 = s_ * 512, (s_ + 1) * 512
                        for fc in range(n):
                            pg = ps_next()
                            pu = ps_next()
                            S.mm(pg, pg.t[:, :], [(wg[:, k, fc * 128:(fc + 1) * 128], xt(k, a0, a1)) for k in range(8)], reads=[Wg, XTb[s_]])
                            S.mm(pu, pu.t[:, :], [(wu[:, k, fc * 128:(fc + 1) * 128], xt(k, a0, a1)) for k in range(8)], reads=[Wu, XTb[s_]])
                            sg = sv(2, F32, ((fc + s_ * n) % 3) * 2048, [512])
                            act(sg.ap, pg.t[:, :], AF.Silu, reads=[pg], writes=sg.b)
                            tt("dve", HT.ap[:, fc, a0:a1], pu.t[:, :], sg.ap, ALU.mult, reads=[pu] + sg.b, pw=[HT.b[fc]])
                    for s_ in range(2):
                        for t4 in range(4):
                            t = s_ * 4 + t4
                            for hf in range(2):
                                p = ps_next()
                                S.mm(p, p.t[:, :], [(HT.ap[:, fc, t * 128:(t + 1) * 128], wd[:, fc, hf * 512:(hf + 1) * 512]) for fc in range(n)],
                                     reads=HT.b[0:n] + [Wd])
                                xa = X[t].t[:, hf * 512:(hf + 1) * 512]
                                if moe:
                                    stt(xa, p.t[:, :], CW.t[:, t, e_:e_ + 1], xa, ALU.mult, ALU.add, reads=[p, CW, X[t]], pw=[X[t]])
                                else:
                                    tt("dve", xa, xa, p.t[:, :], ALU.add, reads=[p, X[t]], pw=[X[t]])

        for g in range(ng):
            for t in range(NT):
                S.dma("sp", X[t].t[:, :], x_d[g * G + t * 128:g * G + (t + 1) * 128, :], writes=[X[t]])
                make_xt(t)
            PH = phases if phases is not None else {"attn", "ssm", "pool", "merge", "wo", "xattn", "xo", "ffn"}
            for l in range(depth):
                AOT = [sv(2, BF16, 0, [4, G]), sv(3, BF16, 0, [4, G])]
                GLUT = sv(10, BF16, 0, [4, G])
                PMT = sv(6, BF16, 0, [4, G])
                MT = [sv(7, BF16, 0, [4, G]), sv(8, BF16, 0, [4, G])]
                OXT = [sv(4, BF16, 0, [4, G]), sv(5, BF16, 0, [4, G])]
                if "attn" in PH:
                    AOT = phase_attn(g, l)
                if "ssm" in PH:
                    GLUT = phase_ssm(g, l)
                if "pool" in PH:
                    PMT = phase_pool(g, l)
                if "merge" in PH:
                    MT = phase_merge(g, l, AOT, GLUT, PMT)
                if "wo" in PH:
                    out_proj(lambda k: MT[k // 4].ap[:, k % 4, :], MT[0].b + MT[1].b, w_o_d[l], l, 0, g, False,
                             filler=(lambda g=g, l=l: phase_xattn_kv(g, l)) if "xattn" in PH else None)
                if "xattn" in PH:
                    OXT = phase_xattn(g, l)
                if "xo" in PH:
                    out_proj(lambda k: OXT[k // 4].ap[:, k % 4, :], OXT[0].b + OXT[1].b, xa_wo_d[l], l, 1, g, False)
                if "ffn" in PH:
                    phase_ffn(g, l)
                layernorm(2, l, g, l == depth - 1)
        fin = [B_out] + ([B_dbg] if dbg else [])
        S.finish(fin)
        S.emit()
        build.stats = (S.ninst, S.nsem, {e: len(v) for e, v in S.ops.items()})
    return nc


def make_consts(seq):
    bf = ml_dtypes.bfloat16
    c_bf = np.zeros((128, 1280), np.float32)
    c_bf[:, 0:128] = np.eye(128)
    c_bf[:, 128:256] = 1.0
    k = np.arange(128)[:, None]
    q = np.arange(128)[None, :]
    mc = np.where(k <= q, 0.0, -30000.0)
    mp = np.where(k > q, 0.0, -30000.0)
    c_bf[:, 256:768] = np.tile(mc, (1, 4))
    c_bf[:, 768:1280] = np.tile(mp, (1, 4))
    c_f = np.zeros((128, 72), np.float32)
    for j, w in enumerate((2, 4, 8, 16)):
        c_f[:, j * 16:(j + 1) * 16] = 1.0 / np.minimum(np.arange(1, 17), w)
    p = np.arange(128)
    for qq in range(4):
        c_f[:, 64 + qq] = (p // 32 == qq)
    c_f[:, 68] = ((p // 16) % 2 == 0)
    c_f[:, 69] = ((p // 16) % 2 == 1)
    half = 8
    inv_freq = np.power(np.float32(500000.0), -np.arange(half, dtype=np.float32) / half).astype(np.float32)
    ang = np.arange(seq, dtype=np.float32)[None, :] * inv_freq[:, None]
    c_rope = np.zeros((32, 2, seq), np.float32)
    c_rope[:, 0, :] = 1.0
    c_rope[0:8, 0, :] = np.cos(ang)
    c_rope[8:16, 0, :] = np.cos(ang)
    c_rope[0:8, 1, :] = -np.sin(ang)
    c_rope[8:16, 1, :] = np.sin(ang)
    return c_bf.astype(bf), c_f, c_rope


_NC_CACHE = {}


def kernel(**inputs):
    x = np.asarray(inputs["x"], np.float32)
    B, SEQ, _ = x.shape
    ng = SEQ // G
    key = (ng, DEPTH)
    if key not in _NC_CACHE:
        _NC_CACHE[key] = build(ng, DEPTH)
    nc = _NC_CACHE[key]
    c_bf, c_f, c_rope = make_consts(SEQ)
    shared = {k: np.ascontiguousarray(np.asarray(v, np.float32)) for k, v in inputs.items() if k not in ("x", "mem")}
    shared.update(c_bf=c_bf, c_f=c_f, c_rope=c_rope)
    in_maps = []
    for b in range(B):
        m = dict(shared)
        m["x"] = np.ascontiguousarray(x[b])
        m["mem"] = np.ascontiguousarray(np.asarray(inputs["mem"], np.float32)[b])
        in_maps.append(m)
    res = run_bass_kernel_spmd(nc, in_maps, core_ids=list(range(B)))
    return np.stack([np.asarray(r["out"], np.float32) for r in res.results], axis=0)
```
